# Optimizing a Trainium2 kernel written in Bass

```python
import math
import jax
import jax.numpy as jnp
from jax import lax
import numpy as np

D_MODEL = 2048
BATCH = 4
SEQ = 2048
DEPTH = 4

CHUNK = 64
N_BRANCH = 4
BRANCH = D_MODEL // N_BRANCH
HG_DK = 128
HG_HEADS = BRANCH // HG_DK
HG_DV = BRANCH // HG_HEADS
S5_CH = 16
S5_GROUPS = BRANCH // S5_CH
S5_STATE = 64
M2_HEADDIM = 64
M2_HEADS = BRANCH // M2_HEADDIM
M2_GROUPS = 2
M2_STATE = 128
M2_CONV = 4
M2_XBC = BRANCH + 2 * M2_GROUPS * M2_STATE
RK_HEADSIZE = 64
RK_HEADS = BRANCH // RK_HEADSIZE
RK_DECAY_LORA = 64
RK_A_LORA = 64
RK_GATE_LORA = 128
RK_WIDTH = 3 * BRANCH + RK_DECAY_LORA + RK_A_LORA + RK_GATE_LORA
RK_GN_EPS = 64e-5
GATE_IN = N_BRANCH * D_MODEL
HG_IN = 4 * BRANCH
S5_IN = BRANCH
M2_IN = BRANCH + M2_XBC + M2_HEADS
N_IN = GATE_IN + HG_IN + S5_IN + M2_IN + RK_WIDTH
N_EXPERTS = 32
TOP_K = 4
D_EXPERT = 3 * D_MODEL // 8
SWIGLU_LIMIT = 7.0
SWIGLU_ALPHA = 1.702
MOE_BLOCK = 128
NORM_EPS = 1e-6

kernel_name = 'hybrid_streaming_encoder_block'


def rmsnorm(x, g):
    xf = x.astype(jnp.float32)
    y = xf * lax.rsqrt(jnp.mean(xf * xf, axis=-1, keepdims=True) + NORM_EPS)
    return (y * g.astype(jnp.float32)).astype(x.dtype)


def causal_shift(x):
    return jnp.pad(x, ((0, 0), (1, 0), (0, 0)))[:, :-1]


def to_chunks(t, heads):
    b, s, _ = t.shape
    return t.reshape(b, s // CHUNK, CHUNK, heads, -1).transpose(1, 0, 3, 2, 4)


def hgrn2_mixer(p, lb, onorm):
    b = p.shape[0]
    q, f, i, g = jnp.split(p.astype(jnp.float32), 4, axis=-1)
    lb = lb.astype(jnp.float32)
    forget = lb + (1.0 - lb) * jax.nn.sigmoid(f)
    log_f = jnp.log(forget)
    k = 1.0 - forget
    q = jax.nn.silu(q)
    qc, kc, vc, lc = [to_chunks(t, HG_HEADS) for t in (q, k, i, log_f)]
    mask = jnp.tril(jnp.ones((CHUNK, CHUNK), bool))[:, :, None]

    def step(state, inp):
        qb, kb, vb, lfb = inp
        cum = jnp.cumsum(lfb, axis=2)
        o_inter = jnp.einsum('bhtk,bhkv->bhtv', qb * jnp.exp(cum), state)
        diff = cum[:, :, :, None, :] - cum[:, :, None, :, :]
        decay = jnp.exp(jnp.where(mask, diff, -jnp.inf))
        scores = jnp.einsum('bhtk,bhtsk,bhsk->bhts', qb, decay, kb)
        o = o_inter + jnp.einsum('bhts,bhsv->bhtv', scores, vb)
        last = cum[:, :, -1:, :]
        state = jnp.exp(last[:, :, 0, :])[..., None] * state + jnp.einsum(
            'bhsk,bhsv->bhkv', kb * jnp.exp(last - cum), vb)
        return state, o

    s0 = jnp.zeros((b, HG_HEADS, HG_DK, HG_DV), jnp.float32)
    _, o = lax.scan(step, s0, (qc, kc, vc, lc))
    o = o.transpose(1, 0, 3, 2, 4).reshape(b, -1, HG_HEADS, HG_DV)
    o = rmsnorm(o, onorm).reshape(b, -1, BRANCH)
    return o * jax.nn.silu(g)


def s5_mixer(u, lam_re, lam_im, log_dt, b_re, b_im, c_re, c_im, d_skip, w_glu, b_glu):
    u = u.astype(jnp.float32)
    b, s, _ = u.shape
    ug = u.reshape(b, s, S5_GROUPS, S5_CH)
    lr = lam_re.astype(jnp.float32)
    li = lam_im.astype(jnp.float32)
    dt = jnp.exp(log_dt.astype(jnp.float32))[:, None]
    mag = jnp.exp(lr * dt)
    ab_re = mag * jnp.cos(li * dt)
    ab_im = mag * jnp.sin(li * dt)
    den = lr * lr + li * li
    nr = ab_re - 1.0
    coef_re = (nr * lr + ab_im * li) / den
    coef_im = (ab_im * lr - nr * li) / den
    bu_r = jnp.einsum('gpi,bsgi->bsgp', b_re, ug)
    bu_i = jnp.einsum('gpi,bsgi->bsgp', b_im, ug)
    bu_re = coef_re * bu_r - coef_im * bu_i
    bu_im = coef_re * bu_i + coef_im * bu_r
    a_re = jnp.broadcast_to(ab_re, bu_re.shape)
    a_im = jnp.broadcast_to(ab_im, bu_im.shape)

    def combine(e1, e2):
        a1r, a1i, b1r, b1i = e1
        a2r, a2i, b2r, b2i = e2
        return (a2r * a1r - a2i * a1i,
                a2r * a1i + a2i * a1r,
                a2r * b1r - a2i * b1i + b2r,
                a2r * b1i + a2i * b1r + b2i)

    _, _, x_re, x_im = lax.associative_scan(combine, (a_re, a_im, bu_re, bu_im), axis=1)
    y = jnp.einsum('gip,bsgp->bsgi', c_re, x_re) - jnp.einsum('gip,bsgp->bsgi', c_im, x_im)
    y = y.reshape(b, s, BRANCH) + d_skip * u
    y = jax.nn.gelu(y)
    return y * jax.nn.sigmoid(jnp.einsum('bsw,wv->bsv', y, w_glu) + b_glu)


def causal_depthwise_conv(x, w, bias):
    ch = x.shape[-1]
    y = lax.conv_general_dilated(x, w.astype(x.dtype)[:, None, :], window_strides=(1,),
                                 padding=((M2_CONV - 1, 0),),
                                 dimension_numbers=('NWC', 'WIO', 'NWC'),
                                 feature_group_count=ch)
    return y + bias


def segsum(x):
    t = x.shape[-1]
    xr = jnp.broadcast_to(x[..., :, None], x.shape + (t,))
    xr = jnp.where(jnp.tril(jnp.ones((t, t), bool), -1), xr, 0.0)
    cs = jnp.cumsum(xr, axis=-2)
    return jnp.where(jnp.tril(jnp.ones((t, t), bool)), cs, -jnp.inf)


def ssd_chunked(x, a, bm, cm):
    b, s, h, pd = x.shape
    nc = s // CHUNK
    x = x.reshape(b, nc, CHUNK, h, pd)
    bm = bm.reshape(b, nc, CHUNK, h, -1)
    cm = cm.reshape(b, nc, CHUNK, h, -1)
    a = a.reshape(b, nc, CHUNK, h).transpose(0, 3, 1, 2)
    a_cum = jnp.cumsum(a, axis=-1)
    lmat = jnp.exp(segsum(a))
    cb = jnp.einsum('bclhn,bcshn->bhcls', cm, bm)
    y_diag = jnp.einsum('bhcls,bcshp->bclhp', cb * lmat, x)
    decay_states = jnp.exp(a_cum[..., -1:] - a_cum)
    states = jnp.einsum('bclhn,bhcl,bclhp->bchpn', bm, decay_states, x)
    states = jnp.concatenate([jnp.zeros_like(states[:, :1]), states], axis=1)
    chunk_decay = jnp.exp(segsum(jnp.pad(a_cum[..., -1], ((0, 0), (0, 0), (1, 0)))))
    states = jnp.einsum('bhzc,bchpn->bzhpn', chunk_decay, states)[:, :-1]
    y_off = jnp.einsum('bclhn,bchpn,bhcl->bclhp', cm, states, jnp.exp(a_cum))
    return (y_diag + y_off).reshape(b, s, h, pd)


def mamba2_mixer(p, conv_w, conv_b, dt_bias, a_log, d_skip, norm_g):
    p = p.astype(jnp.float32)
    b, s, _ = p.shape
    z = p[..., :BRANCH]
    xbc = p[..., BRANCH:BRANCH + M2_XBC]
    dt_raw = p[..., BRANCH + M2_XBC:]
    xbc = jax.nn.silu(causal_depthwise_conv(xbc, conv_w, conv_b))
    xs = xbc[..., :BRANCH].reshape(b, s, M2_HEADS, M2_HEADDIM)
    gn = M2_GROUPS * M2_STATE
    rep = M2_HEADS // M2_GROUPS
    bm = jnp.repeat(xbc[..., BRANCH:BRANCH + gn].reshape(b, s, M2_GROUPS, M2_STATE), rep, axis=2)
    cm = jnp.repeat(xbc[..., BRANCH + gn:].reshape(b, s, M2_GROUPS, M2_STATE), rep, axis=2)
    dt = jax.nn.softplus(dt_raw + dt_bias)
    a = -jnp.exp(a_log.astype(jnp.float32))
    y = ssd_chunked(xs * dt[..., None], a * dt, bm, cm) + d_skip[:, None] * xs
    y = y.reshape(b, s, BRANCH) * jax.nn.silu(z)
    y = rmsnorm(y.reshape(b, s, M2_GROUPS, -1), norm_g.reshape(M2_GROUPS, -1))
    return y.reshape(b, s, BRANCH)


def rwkv7_mixer(p, mu, w0, w2, a0, a2, g2, k_k, k_a, r_k, ln_w, ln_b):
    p = p.astype(jnp.float32)
    b, s, _ = p.shape
    pm = p + (causal_shift(p) - p) * mu
    r = pm[..., :BRANCH]
    k = pm[..., BRANCH:2 * BRANCH]
    v = pm[..., 2 * BRANCH:3 * BRANCH]
    o = 3 * BRANCH
    w_lo = pm[..., o:o + RK_DECAY_LORA]
    a_lo = pm[..., o + RK_DECAY_LORA:o + RK_DECAY_LORA + RK_A_LORA]
    g_lo = pm[..., o + RK_DECAY_LORA + RK_A_LORA:]
    w = -jax.nn.softplus(-(w0 + jnp.tanh(w_lo) @ w2)) - 0.5
    decay = jnp.exp(-jnp.exp(w))
    a = jax.nn.sigmoid(a0 + a_lo @ a2)
    g = jax.nn.sigmoid(g_lo) @ g2
    hs = lambda t: t.reshape(b, s, RK_HEADS, RK_HEADSIZE)
    kk = hs(k * k_k)
    kk = kk / jnp.maximum(jnp.linalg.norm(kk, axis=-1, keepdims=True), 1e-12)
    k = k * (1.0 + (a - 1.0) * k_a)
    r_h, k_h, v_h, w_h, a_h = hs(r), hs(k), hs(v), hs(decay), hs(a)
    tm = lambda t: t.transpose(1, 0, 2, 3)

    def step(st, inp):
        w_t, r_t, k_t, v_t, kk_t, a_t = inp
        sa = jnp.einsum('bhvk,bhk->bhv', st, -kk_t)
        st = (st * w_t[:, :, None, :] + sa[..., None] * (kk_t * a_t)[:, :, None, :]
              + v_t[..., None] * k_t[:, :, None, :])
        return st, jnp.einsum('bhvk,bhk->bhv', st, r_t)

    s0 = jnp.zeros((b, RK_HEADS, RK_HEADSIZE, RK_HEADSIZE), jnp.float32)
    _, y = lax.scan(step, s0, (tm(w_h), tm(r_h), tm(k_h), tm(v_h), tm(kk), tm(a_h)))
    y = y.transpose(1, 0, 2, 3)
    mean = jnp.mean(y, axis=-1, keepdims=True)
    var = jnp.mean(jnp.square(y - mean), axis=-1, keepdims=True)
    y = ((y - mean) * lax.rsqrt(var + RK_GN_EPS)).reshape(b, s, BRANCH) * ln_w + ln_b
    bonus = jnp.sum(r_h * k_h * r_k, axis=-1, keepdims=True) * v_h
    return (y + bonus.reshape(b, s, BRANCH)) * g


def mixer_block(h, w_in, w_branch, w_out, hg, s5, m2, rk):
    proj = jnp.einsum('bsd,de->bse', h, w_in)
    o1 = GATE_IN
    o2 = o1 + HG_IN
    o3 = o2 + S5_IN
    o4 = o3 + M2_IN
    ya = hgrn2_mixer(proj[..., o1:o2], *hg)
    yb = s5_mixer(proj[..., o2:o3], *s5)
    yc = mamba2_mixer(proj[..., o3:o4], *m2)
    yd = rwkv7_mixer(proj[..., o4:], *rk)
    ys = jnp.stack([ya, yb, yc, yd], axis=2).astype(h.dtype)
    zs = jnp.einsum('bsnw,nwd->bsnd', ys, w_branch)
    gates = jax.nn.sigmoid(proj[..., :GATE_IN].astype(jnp.float32)).reshape(zs.shape)
    merged = jnp.einsum('bsnd,bsnd->bsd', gates, zs.astype(jnp.float32)).astype(h.dtype)
    return jnp.einsum('bsd,de->bse', merged, w_out)


def moe_ffn(h, w_router, b_router, w_gu, b_gu, w_down, b_down):
    b, s, d = h.shape
    n = b * s
    xt = h.reshape(n, d)
    logits = (xt @ w_router + b_router).astype(jnp.float32)
    top_vals, top_idx = lax.top_k(logits, TOP_K)
    top_w = jax.nn.softmax(top_vals, axis=-1)
    nk = n * TOP_K
    eid = top_idx.reshape(-1)
    tok = jnp.arange(nk, dtype=jnp.int32) // TOP_K
    wts = top_w.reshape(-1)
    order = jnp.argsort(eid, stable=True)
    e_sorted = eid[order]
    counts = jnp.bincount(eid, length=N_EXPERTS)
    padded = ((counts + MOE_BLOCK - 1) // MOE_BLOCK) * MOE_BLOCK
    ends_p = jnp.cumsum(padded)
    starts_p = ends_p - padded
    starts = jnp.cumsum(counts) - counts
    dest = starts_p[e_sorted] + (jnp.arange(nk, dtype=jnp.int32) - starts[e_sorted])
    n_slots = nk + N_EXPERTS * MOE_BLOCK
    n_blocks = n_slots // MOE_BLOCK
    slot_tok = jnp.zeros((n_slots,), jnp.int32).at[dest].set(tok[order])
    slot_w = jnp.zeros((n_slots,), jnp.float32).at[dest].set(wts[order])
    block_e = jnp.minimum(jnp.searchsorted(ends_p, jnp.arange(n_blocks) * MOE_BLOCK, side='right'),
                          N_EXPERTS - 1)
    xs = xt[slot_tok].reshape(n_blocks, MOE_BLOCK, d)

    def expert(args):
        xb, e = args
        gu = xb @ w_gu[e] + b_gu[e]
        gate = jnp.minimum(gu[:, :D_EXPERT], SWIGLU_LIMIT)
        up = jnp.clip(gu[:, D_EXPERT:], -SWIGLU_LIMIT, SWIGLU_LIMIT)
        act = (up + 1.0) * gate * jax.nn.sigmoid(SWIGLU_ALPHA * gate)
        return act @ w_down[e] + b_down[e]

    ys = lax.map(expert, (xs, block_e)).reshape(n_slots, d)
    ys = ys.astype(jnp.float32) * slot_w[:, None]
    out = jax.ops.segment_sum(ys, slot_tok, num_segments=n)
    return out.reshape(b, s, d).astype(h.dtype)


def setup_inputs(seed: int = 0) -> dict:
    key = jax.random.key(seed)
    ks = iter(jax.random.split(key, 64))
    f32 = jnp.float32
    L, D = DEPTH, D_MODEL

    def nrm(shape, scale):
        return scale * jax.random.normal(next(ks), shape, f32)

    def unif(shape, lo, hi):
        return jax.random.uniform(next(ks), shape, f32, lo, hi)

    n_idx = jnp.arange(S5_STATE, dtype=f32)
    dt0 = jnp.exp(unif((L, M2_HEADS), math.log(1e-3), math.log(1e-1)))
    return {
        'x': nrm((BATCH, SEQ, D), 1.0),
        'c': nrm((BATCH, D), 1.0),
        'w_mod': nrm((L, D, 6 * D), 0.5 * D ** -0.5),
        'b_mod': nrm((L, 6 * D), 0.02),
        'g_norm_mix': 1.0 + nrm((L, D), 0.05),
        'g_norm_ffn': 1.0 + nrm((L, D), 0.05),
        'w_in': nrm((L, D, N_IN), D ** -0.5),
        'hg_lower_bound': 1.0 + nrm((L, BRANCH), 0.1),
        'hg_onorm': 1.0 + nrm((L, HG_DV), 0.05),
        's5_lambda_re': -0.5 + nrm((L, S5_GROUPS, S5_STATE), 0.01),
        's5_lambda_im': jnp.pi * n_idx + nrm((L, S5_GROUPS, S5_STATE), 0.01),
        's5_log_dt': unif((L, S5_GROUPS), math.log(1e-3), math.log(1e-1)),
        's5_b_re': nrm((L, S5_GROUPS, S5_STATE, S5_CH), (2 * S5_CH) ** -0.5),
        's5_b_im': nrm((L, S5_GROUPS, S5_STATE, S5_CH), (2 * S5_CH) ** -0.5),
        's5_c_re': nrm((L, S5_GROUPS, S5_CH, S5_STATE), S5_STATE ** -0.5),
        's5_c_im': nrm((L, S5_GROUPS, S5_CH, S5_STATE), S5_STATE ** -0.5),
        's5_d': nrm((L, BRANCH), 1.0),
        's5_w_glu': nrm((L, BRANCH, BRANCH), BRANCH ** -0.5),
        's5_b_glu': nrm((L, BRANCH), 0.02),
        'm2_conv_w': nrm((L, M2_CONV, M2_XBC), M2_CONV ** -0.5),
        'm2_conv_b': nrm((L, M2_XBC), 0.02),
        'm2_dt_bias': dt0 + jnp.log(-jnp.expm1(-dt0)),
        'm2_a_log': jnp.log(unif((L, M2_HEADS), 1.0, 16.0)),
        'm2_d': 1.0 + nrm((L, M2_HEADS), 0.1),
        'm2_norm': 1.0 + nrm((L, BRANCH), 0.05),
        'rk_mu': unif((L, RK_WIDTH), 0.0, 1.0),
        'rk_w0': unif((L, BRANCH), -6.5, -1.5),
        'rk_w2': nrm((L, RK_DECAY_LORA, BRANCH), 0.5 * RK_DECAY_LORA ** -0.5),
        'rk_a0': nrm((L, BRANCH), 0.1),
        'rk_a2': nrm((L, RK_A_LORA, BRANCH), 0.5 * RK_A_LORA ** -0.5),
        'rk_g2': nrm((L, RK_GATE_LORA, BRANCH), RK_GATE_LORA ** -0.5),
        'rk_k_k': 0.85 + nrm((L, BRANCH), 0.02),
        'rk_k_a': 1.0 + nrm((L, BRANCH), 0.02),
        'rk_r_k': nrm((L, RK_HEADS, RK_HEADSIZE), 0.1),
        'rk_ln_w': 1.0 + nrm((L, BRANCH), 0.05),
        'rk_ln_b': nrm((L, BRANCH), 0.02),
        'w_branch': nrm((L, N_BRANCH, BRANCH, D), BRANCH ** -0.5),
        'w_out': nrm((L, D, D), D ** -0.5),
        'w_router': nrm((L, D, N_EXPERTS), D ** -0.5),
        'b_router': nrm((L, N_EXPERTS), 0.01),
        'w_gu': nrm((L, N_EXPERTS, D, 2 * D_EXPERT), D ** -0.5),
        'b_gu': nrm((L, N_EXPERTS, 2 * D_EXPERT), 0.01),
        'w_down': nrm((L, N_EXPERTS, D_EXPERT, D), D_EXPERT ** -0.5),
        'b_down': nrm((L, N_EXPERTS, D), 0.01),
        'g_final': 1.0 + nrm((D,), 0.05),
    }


def reference(x, c, w_mod, b_mod, g_norm_mix, g_norm_ffn, w_in, hg_lower_bound, hg_onorm,
              s5_lambda_re, s5_lambda_im, s5_log_dt, s5_b_re, s5_b_im, s5_c_re, s5_c_im, s5_d,
              s5_w_glu, s5_b_glu, m2_conv_w, m2_conv_b, m2_dt_bias, m2_a_log, m2_d, m2_norm,
              rk_mu, rk_w0, rk_w2, rk_a0, rk_a2, rk_g2, rk_k_k, rk_k_a, rk_r_k, rk_ln_w, rk_ln_b,
              w_branch, w_out, w_router, b_router, w_gu, b_gu, w_down, b_down, g_final):
    lbs = jax.nn.softmax(hg_lower_bound.astype(jnp.float32), axis=0)
    lbs = jnp.cumsum(lbs, axis=0) - lbs[0]
    cond = jax.nn.silu(c)
    for l in range(DEPTH):
        mod = cond @ w_mod[l] + b_mod[l]
        sh_a, sc_a, gt_a, sh_m, sc_m, gt_m = jnp.split(mod[:, None, :], 6, axis=-1)
        h = rmsnorm(x, g_norm_mix[l]) * (1.0 + sc_a) + sh_a
        mix = mixer_block(
            h, w_in[l], w_branch[l], w_out[l],
            (lbs[l], hg_onorm[l]),
            (s5_lambda_re[l], s5_lambda_im[l], s5_log_dt[l], s5_b_re[l], s5_b_im[l],
             s5_c_re[l], s5_c_im[l], s5_d[l], s5_w_glu[l], s5_b_glu[l]),
            (m2_conv_w[l], m2_conv_b[l], m2_dt_bias[l], m2_a_log[l], m2_d[l], m2_norm[l]),
            (rk_mu[l], rk_w0[l], rk_w2[l], rk_a0[l], rk_a2[l], rk_g2[l], rk_k_k[l], rk_k_a[l],
             rk_r_k[l], rk_ln_w[l], rk_ln_b[l]))
        x = x + gt_a * mix
        h = rmsnorm(x, g_norm_ffn[l]) * (1.0 + sc_m) + sh_m
        x = x + gt_m * moe_ffn(h, w_router[l], b_router[l], w_gu[l], b_gu[l], w_down[l], b_down[l])
    return rmsnorm(x, g_final)
```

```python
import contextlib
import numpy as np
import concourse.bass as bass
import concourse.mybir as mybir
from concourse.bass_utils import run_bass_kernel_spmd

F32 = mybir.dt.float32
BF16 = mybir.dt.bfloat16
U32 = mybir.dt.uint32
I32 = mybir.dt.int32
ALU = mybir.AluOpType
AF = mybir.ActivationFunctionType
AX = mybir.AxisListType

D = 2048
S = 2048
L = 4
NB = 4
BR = 512
N_IN = 14088
O1 = 8192
O_HG = 8192
O_S5 = 10240
O_M2 = 10752
O_RK = 12296
NE = 32
DE = 768
EPS = 1e-6


class Buf:
    __slots__ = ("w", "r", "name")

    def __init__(self, name=""):
        self.w = None
        self.r = {}
        self.name = name


class T:
    def __init__(self, t, name=""):
        self.t = t
        self.b = Buf(name)

    def __getitem__(self, k):
        return self.t[k]


def _bufs(lst):
    out = []
    for x in lst:
        if x is None:
            continue
        out.append(x.b if isinstance(x, T) else x)
    return out


class Prog:
    CE = ("pe", "act", "dve", "pool")
    ENG = ("pe", "act", "dve", "pool", "sp")

    def __init__(self, nc, stack, ndma=20):
        self.nc = nc
        self.stack = stack
        self.q = {e: [] for e in self.ENG}
        self.cnt = {e: 0 for e in self.CE}
        self.sems = []
        self.own = {}
        for e in self.CE:
            self.own[e] = self._newsem("own_" + e)
        self.known = {e: {} for e in self.ENG}
        self.dpool = {}
        self.drr = {}
        for qn in ("sp", "act", "pool"):
            self.dpool[qn] = [[self._newsem(f"d_{qn}{i}"), 0] for i in range(ndma)]
            self.drr[qn] = 0
        self.n_ops = 0

    def _newsem(self, name):
        s = self.stack.enter_context(self.nc.semaphore(name))
        self.sems.append(s)
        return len(self.sems) - 1

    def _deps(self, eng, reads, writes, relaxed=False):
        deps = {}
        own = self.own.get(eng, -1)

        def add(s, v):
            if deps.get(s, 0) < v:
                deps[s] = v
        for b in reads:
            if b.w is not None:
                add(*b.w)
        for b in writes:
            if b.w is not None:
                add(*b.w)
            for s, v in b.r.items():
                if relaxed and s == own:
                    continue
                add(s, v)
        waits = []
        kn = self.known[eng]
        for s, v in deps.items():
            if eng in self.own and s == self.own[eng]:
                if eng == "pe":
                    continue
                if relaxed and v < self.cnt[eng]:
                    continue
                if v < self.cnt[eng] - 1:
                    continue
            if kn.get(s, 0) >= v:
                continue
            kn[s] = v
            waits.append((s, v))
        return waits

    def _mark(self, ev, reads, writes):
        s, v = ev
        for b in reads:
            if b.r.get(s, 0) < v:
                b.r[s] = v
        for b in writes:
            b.w = ev
            b.r = {}

    def op(self, eng, fn, reads=(), writes=(), relaxed=False):
        reads = _bufs(reads)
        writes = _bufs(writes)
        waits = self._deps(eng, reads, writes, relaxed)
        self.cnt[eng] += 1
        ev = (self.own[eng], self.cnt[eng])
        self.q[eng].append((waits, fn, ev[0], 1))
        self._mark(ev, reads, writes)
        self.n_ops += 1
        return ev

    def _dmaev(self, qn, waits):
        pool = self.dpool[qn]
        i = self.drr[qn]
        self.drr[qn] = (i + 1) % len(pool)
        s, tot = pool[i]
        kn = self.known[qn]
        if kn.get(s, 0) < tot:
            kn[s] = tot
            waits.append((s, tot))
        pool[i][1] = tot + 16
        return (s, tot + 16)

    def dma(self, qn, out, in_, reads=(), writes=(), **kw):
        reads = _bufs(reads)
        writes = _bufs(writes)
        waits = self._deps(qn, reads, writes)
        ev = self._dmaev(qn, waits)

        def fn(e, out=out, in_=in_, kw=kw):
            return e.dma_start(out=out, in_=in_, **kw)
        self.q[qn].append((waits, fn, ev[0], 16))
        self._mark(ev, reads, writes)
        self.n_ops += 1
        return ev

    def custom(self, qn, fn, reads=(), writes=()):
        reads = _bufs(reads)
        writes = _bufs(writes)
        waits = self._deps(qn, reads, writes)
        ev = self._dmaev(qn, waits)
        self.q[qn].append((waits, fn, ev[0], 16))
        self._mark(ev, reads, writes)
        return ev

    def barrier(self):
        tot = []
        for qn, pool in self.dpool.items():
            for s, t in pool:
                if t > 0:
                    tot.append((s, t))
        for e in self.ENG:
            waits = []
            kn = self.known[e]
            for e2 in self.CE:
                if e2 != e and self.cnt[e2] > 0:
                    s, v = self.own[e2], self.cnt[e2]
                    if kn.get(s, 0) < v:
                        kn[s] = v
                        waits.append((s, v))
            for s, v in tot:
                if kn.get(s, 0) < v:
                    kn[s] = v
                    waits.append((s, v))
            if waits:
                self.q[e].append((waits, None, None, 0))

    def finish(self):
        waits = []
        for qn, pool in self.dpool.items():
            for s, tot in pool:
                if tot > 0:
                    waits.append((s, tot))
        self.q["sp"].append((waits, None, None, 0))
        w2 = [(self.own[e], self.cnt[e]) for e in self.CE if self.cnt[e] > 0]
        self.q["sp"].append((w2, None, None, 0))

    def emit(self):
        nc = self.nc
        sems = self.sems
        q = self.q
        with nc.Block() as block:
            def run(e, lst):
                for waits, fn, s, inc in lst:
                    for ws, wv in waits:
                        e.wait_ge(sems[ws], wv)
                    if fn is None:
                        continue
                    ins = fn(e)
                    ins.then_inc(sems[s], inc)

            @block.tensor
            def _(e):
                run(e, q["pe"])

            @block.scalar
            def _(e):
                run(e, q["act"])

            @block.vector
            def _(e):
                run(e, q["dve"])

            @block.gpsimd
            def _(e):
                run(e, q["pool"])

            @block.sync
            def _(e):
                run(e, q["sp"])


class KB:
    def __init__(self, nc, stack, cfg):
        self.nc = nc
        self.st = stack
        self.cfg = cfg
        self.P = Prog(nc, stack)
        self.inputs = {}
        self.outs = {}
        self.rr = 0
        self.dbg = cfg.get("dbg", ())

    def inp(self, name, shape, dtype=F32):
        if name not in self.inputs:
            self.inputs[name] = T(self.nc.dram_tensor(name, list(shape), dtype, kind="ExternalInput").ap(), name)
        return self.inputs[name]

    def dram(self, name, shape, dtype=F32):
        if name in self.cfg.get("scratch_in", ()):
            return self.inp(name, shape, dtype)
        kind = "ExternalOutput" if name in self.dbg else "Internal"
        t = T(self.nc.dram_tensor(name, list(shape), dtype, kind=kind).ap(), name)
        if name in self.dbg:
            self.outs[name] = t
        return t

    def sb(self, name, shape, dtype=F32, stack=None):
        st = stack or self.st
        return T(st.enter_context(self.nc.sbuf_tensor(name, list(shape), dtype)), name)

    def psum(self, name, shape, dtype=F32, stack=None):
        st = stack or self.st
        return T(st.enter_context(self.nc.psum_tensor(name, list(shape), dtype)), name)

    @contextlib.contextmanager
    def phase(self):
        with contextlib.ExitStack() as st:
            yield st
        self.P.barrier()

    def dump(self, name, tile, shape, dtype=F32, ap=None):
        if name not in self.dbg:
            return
        if name not in self.outs:
            self.outs[name] = T(self.nc.dram_tensor(name, list(shape), dtype, kind="ExternalOutput").ap(), name)
        d = self.outs[name]
        self.dma("sp", d[:], tile[:] if ap is None else ap, r=[tile], w=[d])

    def dq(self):
        self.rr += 1
        return ("sp", "act")[self.rr % 2]

    def mm(self, out, lhsT, rhs, start, stop, r=(), w=()):
        return self.P.op("pe", lambda e: e.matmul(out, lhsT=lhsT, rhs=rhs, start=start, stop=stop), reads=r, writes=w)

    def tr(self, out, in_, ident, r=(), w=()):
        return self.P.op("pe", lambda e: e.transpose(out=out, in_=in_, identity=ident), reads=r, writes=w)

    def act(self, out, in_, func, r=(), w=(), bias=None, scale=None, accum=None, eng="act"):
        kw = {}
        if bias is not None:
            kw["bias"] = bias
        if scale is not None:
            kw["scale"] = scale
        if accum is not None:
            kw["accum_out"] = accum
        return self.P.op("act", lambda e: e.activation(out=out, in_=in_, func=func, **kw), reads=r, writes=w)

    def ts(self, eng, out, in0, s1, s2, op0, op1=None, r=(), w=(), accum=None):
        kw = {}
        if accum is not None:
            kw["accum_out"] = accum
        if op1 is None:
            return self.P.op(eng, lambda e: e.tensor_scalar(out=out, in0=in0, scalar1=s1, scalar2=None, op0=op0, **kw), reads=r, writes=w)
        return self.P.op(eng, lambda e: e.tensor_scalar(out=out, in0=in0, scalar1=s1, scalar2=s2, op0=op0, op1=op1, **kw), reads=r, writes=w)

    def tt(self, eng, out, in0, in1, op, r=(), w=()):
        return self.P.op(eng, lambda e: e.tensor_tensor(out=out, in0=in0, in1=in1, op=op), reads=r, writes=w)

    def stt(self, out, in0, scalar, in1, op0, op1, r=(), w=(), accum=None):
        kw = {}
        if accum is not None:
            kw["accum_out"] = accum
        return self.P.op("dve", lambda e: e.scalar_tensor_tensor(out=out, in0=in0, scalar=scalar, in1=in1, op0=op0, op1=op1, **kw), reads=r, writes=w)

    def cp(self, eng, out, in_, r=(), w=()):
        if eng == "act":
            return self.P.op("act", lambda e: e.copy(out=out, in_=in_), reads=r, writes=w)
        return self.P.op(eng, lambda e: e.tensor_copy(out=out, in_=in_), reads=r, writes=w)

    def memset(self, eng, ap, val, w=()):
        return self.P.op(eng, lambda e: e.memset(ap, val), writes=w)

    def red(self, eng, out, in_, op, r=(), w=(), axis=AX.X):
        return self.P.op(eng, lambda e: e.tensor_reduce(out=out, in_=in_, axis=axis, op=op), reads=r, writes=w)

    def recip(self, out, in_, r=(), w=()):
        return self.P.op("dve", lambda e: e.reciprocal(out=out, in_=in_), reads=r, writes=w)

    def dma(self, q, out, in_, r=(), w=(), **kw):
        return self.P.dma(q, out, in_, reads=r, writes=w, **kw)


def build_consts(k):
    c = {}
    idf = k.sb("idf", [128, 128], F32)
    k.memset("pool", idf[:], 1.0, w=[idf])
    k.P.op("pool", lambda e: e.affine_select(out=idf[:], in_=idf[:], pattern=[[-1, 128]], compare_op=ALU.is_equal,
                                              fill=0.0, base=0, channel_multiplier=1), reads=[idf], writes=[idf])
    idb = k.sb("idb", [128, 128], BF16)
    k.cp("dve", idb[:], idf[:], r=[idf], w=[idb])
    c["idf"] = idf
    c["idb"] = idb
    triu = k.sb("triu", [128, 128], F32)
    k.memset("pool", triu[:], 1.0, w=[triu])
    k.P.op("pool", lambda e: e.affine_select(out=triu[:], in_=triu[:], pattern=[[1, 128]], compare_op=ALU.is_ge,
                                              fill=0.0, base=0, channel_multiplier=-1), reads=[triu], writes=[triu])
    c["triu"] = triu
    ones = k.sb("onesf", [128, 128], F32)
    k.memset("pool", ones[:], 1.0, w=[ones])
    c["ones"] = ones
    onesb = k.sb("onesb", [128, 128], BF16)
    k.memset("pool", onesb[:], 1.0, w=[onesb])
    c["onesb"] = onesb
    return c


def stage_mod(k, c, nl):
    nc = k.nc
    cin = k.inp("c_col", [128, 16])
    w_mod = k.inp("w_mod", [nl, D, 6 * D])
    b_mod = k.inp("b_mod_col", [nl, 128, 96])
    gmix = k.inp("g_mix_col", [nl, 128, 16])
    gffn = k.inp("g_ffn_col", [nl, 128, 16])
    condc = k.sb("condc", [128, 16], F32)
    k.dma("sp", condc[:], cin[:], w=[condc])
    k.act(condc[:], condc[:], AF.Silu, r=[condc], w=[condc])
    modc = k.sb("modc", [128, L, 96], F32)
    gsa = k.sb("gsa", [128, L, 16], F32)
    gsm = k.sb("gsm", [128, L, 16], F32)
    gt_scr = k.dram("gt_scr", [L, 2, 16, 128])
    with k.phase() as st:
        wb = [k.sb(f"wmodbuf{i}", [128, 16, 768], F32, st) for i in range(2)]
        pm = k.psum("ps_mod", [128, 512], F32, st)
        pt = k.psum("ps_modT", [128, 512], F32, st)
        bt = k.sb("bmodt", [128, 96], F32, st)
        gtmp = k.sb("gtmp", [128, 16], F32, st)
        rowt = k.sb("rowt", [16, 128], F32, st)
        it = 0
        for l in range(nl):
            for cb in range(16):
                w = wb[it % 2]
                it += 1
                k.dma(k.dq(), w[:], w_mod[l, :, cb * 768:(cb + 1) * 768].rearrange("(kc p) n -> p kc n", p=128), w=[w])
                for m in range(6):
                    col = cb * 6 + m
                    for kc in range(16):
                        k.mm(pm[:, col:col + 1], w[:, kc, m * 128:(m + 1) * 128], condc[:, kc:kc + 1],
                             kc == 0, kc == 15, r=[w, condc], w=[pm])
            k.dma("sp", bt[:], b_mod[l], w=[bt])
            k.tt("dve", modc[:, l, :], pm[:, 0:96], bt[:], ALU.add, r=[pm, bt], w=[modc])
            for (dst, gsrc, off) in ((gsa, gmix, 16), (gsm, gffn, 64)):
                k.dma("sp", gtmp[:], gsrc[l], w=[gtmp])
                k.stt(dst[:, l, :], modc[:, l, off:off + 16], 1.0, gtmp[:], ALU.add, ALU.mult, r=[modc, gtmp], w=[dst])
            for gi, off in enumerate((32, 80)):
                k.tr(pt[0:16, 0:128], modc[:, l, off:off + 16], c["idf"][:], r=[modc, c["idf"]], w=[pt])
                k.cp("dve", rowt[:], pt[0:16, 0:128], r=[pt], w=[rowt])
                k.dma("sp", gt_scr[l, gi], rowt[:], r=[rowt], w=[gt_scr])
    return dict(modc=modc, gsa=gsa, gsm=gsm, gt_scr=gt_scr)


def stage_norm(k, c, xres, hT, gsT, l, modc, shoff, tag):
    with k.phase() as st:
        xt = [k.sb(f"nx{tag}{i}", [128, D], F32, st) for i in range(2)]
        xn = [k.sb(f"nxn{tag}{i}", [128, D], BF16, st) for i in range(2)]
        sq = k.sb(f"nsq{tag}", [128, D], BF16, st)
        ss = [k.sb(f"nss{tag}{i}", [128, 4], F32, st) for i in range(2)]
        pt = [k.psum(f"npt{tag}{i}", [128, 4, 128], BF16, st) for i in range(2)]
        for tt in range(S // 128):
            x = xt[tt % 2]
            n = xn[tt % 2]
            s = ss[tt % 2]
            k.dma(k.dq(), x[:], xres[tt * 128:(tt + 1) * 128, :], r=[xres], w=[x])
            k.act(sq[:], x[:], AF.Square, r=[x], w=[sq, s], accum=s[:, 0:1])
            k.ts("dve", s[:, 1:2], s[:, 0:1], 1.0 / D, EPS, ALU.mult, ALU.add, r=[s], w=[s])
            k.act(s[:, 2:3], s[:, 1:2], AF.Sqrt, r=[s], w=[s])
            k.recip(s[:, 3:4], s[:, 2:3], r=[s], w=[s])
            k.act(n[:], x[:], AF.Copy, r=[x, s], w=[n], scale=s[:, 3:4])
            for j4 in range(4):
                p = pt[j4 % 2]
                for jj in range(4):
                    j = j4 * 4 + jj
                    k.tr(p[:, jj, :], n[:, j * 128:(j + 1) * 128], c["idb"][:], r=[n, c["idb"]], w=[p])
                for jj in range(4):
                    j = j4 * 4 + jj
                    eng = "dve" if jj % 2 == 0 else "pool"
                    if eng == "pool":
                        k.act(hT[:, j, tt * 128:(tt + 1) * 128], p[:, jj, :], AF.Identity, r=[p, gsT, modc], w=[hT],
                              scale=gsT[:, l, j:j + 1], bias=modc[:, l, shoff + j:shoff + j + 1])
                    else:
                        k.ts("dve", hT[:, j, tt * 128:(tt + 1) * 128], p[:, jj, :], gsT[:, l, j:j + 1],
                             modc[:, l, shoff + j:shoff + j + 1], ALU.mult, ALU.add, r=[p, gsT, modc], w=[hT])


def proj_tm(k, hT, wsrc, c0, ncols, dst, st, tag):
    wb = [k.sb(f"pw{tag}{i}", [128, 16, 512], BF16, st) for i in range(2)]
    ob = [k.sb(f"po{tag}{i}", [128, 512], F32, st) for i in range(3)]
    ps = [k.psum(f"pp{tag}{i}", [128, 512], F32, st) for i in range(2)]
    it = 0
    io = 0
    for b0 in range(0, ncols, 512):
        nb = min(512, ncols - b0)
        w = wb[it % 2]
        it += 1
        k.dma("pool", w[:, :, 0:nb], wsrc[:, c0 + b0:c0 + b0 + nb].rearrange("(kc p) n -> p kc n", p=128), w=[w])
        for tt in range(S // 128):
            p = ps[tt % 2]
            for kc in range(16):
                k.mm(p[:, 0:nb], hT[:, kc, tt * 128:(tt + 1) * 128], w[:, kc, 0:nb], kc == 0, kc == 15, r=[hT, w], w=[p])
            o = ob[io % 3]
            io += 1
            if io % 2 == 0:
                k.cp("dve", o[:, 0:nb], p[:, 0:nb], r=[p], w=[o])
            else:
                k.cp("act", o[:, 0:nb], p[:, 0:nb], r=[p], w=[o])
            k.dma(k.dq(), dst[tt * 128:(tt + 1) * 128, b0:b0 + nb], o[:, 0:nb], r=[o], w=[dst])


def proj_fm(k, hT, wsrc, c0, ncols, dst, st, tag):
    wb = [k.sb(f"fw{tag}{i}", [128, 16, 128], BF16, st) for i in range(2)]
    ob = [k.sb(f"fo{tag}{i}", [128, S], F32, st) for i in range(2)]
    ps = [k.psum(f"fp{tag}{i}", [128, 512], F32, st) for i in range(2)]
    assert ncols % 128 == 0
    ip = 0
    for m in range(ncols // 128):
        w = wb[m % 2]
        k.dma("pool", w[:], wsrc[:, c0 + m * 128:c0 + (m + 1) * 128].rearrange("(kc p) n -> p kc n", p=128), w=[w])
        o = ob[m % 2]
        for n in range(4):
            p = ps[ip % 2]
            ip += 1
            for kc in range(16):
                k.mm(p[:], w[:, kc, :], hT[:, kc, n * 512:(n + 1) * 512], kc == 0, kc == 15, r=[hT, w], w=[p])
            if ip % 2 == 0:
                k.cp("dve", o[:, n * 512:(n + 1) * 512], p[:], r=[p], w=[o])
            else:
                k.cp("act", o[:, n * 512:(n + 1) * 512], p[:], r=[p], w=[o])
        k.dma(k.dq(), dst[m * 128:(m + 1) * 128, :], o[:], r=[o], w=[dst])


def stage_merge(k, c, hT, l, w_in, w_branch, w_out, ys_fm, mods, y):
    mergedT = None
    with k.phase() as st0:
        mergedT = k.sb(f"mergedT{l}", [128, 16, S], BF16, st0)
        with k.phase() as st:
            wg = [k.sb(f"mwg{l}{i}", [128, 16, 128], BF16, st) for i in range(2)]
            wbr = [k.sb(f"mwb{l}{i}", [128, 4, 128], BF16, st) for i in range(2)]
            ysb = [k.sb(f"mys{l}{i}", [128, 4, S], BF16, st) for i in range(2)]
            macc = k.sb(f"macc{l}", [128, S], F32, st)
            sg = [k.sb(f"msg{l}{i}", [128, 512], F32, st) for i in range(2)]
            tmp = [k.sb(f"mtmp{l}{i}", [128, 512], F32, st) for i in range(2)]
            pg = [k.psum(f"mpg{l}{i}", [128, 512], F32, st) for i in range(2)]
            pz = [k.psum(f"mpz{l}{i}", [128, 512], F32, st) for i in range(2)]
            it = 0
            for j in range(16):
                for n in range(4):
                    w1 = wg[it % 2]
                    w2 = wbr[it % 2]
                    yb = ysb[it % 2]
                    k.dma("pool", w1[:], w_in[l, :, n * 2048 + j * 128:n * 2048 + (j + 1) * 128].rearrange("(kc p) n -> p kc n", p=128), w=[w1])
                    k.dma("pool", w2[:], w_branch[l, n, :, j * 128:(j + 1) * 128].rearrange("(kc p) n -> p kc n", p=128), w=[w2])
                    k.dma(k.dq(), yb[:], ys_fm[n].rearrange("(kc p) s -> p kc s", p=128), r=[ys_fm], w=[yb])
                    for t in range(4):
                        sl = slice(t * 512, (t + 1) * 512)
                        g = pg[it % 2]
                        z = pz[it % 2]
                        sgt = sg[it % 2]
                        tm = tmp[it % 2]
                        it += 1
                        for kc in range(16):
                            k.mm(g[:], w1[:, kc, :], hT[:, kc, sl], kc == 0, kc == 15, r=[w1, hT], w=[g])
                        for kc in range(4):
                            k.mm(z[:], w2[:, kc, :], yb[:, kc, sl], kc == 0, kc == 3, r=[w2, yb], w=[z])
                        k.act(sgt[:], g[:], AF.Sigmoid, r=[g], w=[sgt])
                        if n == 0:
                            k.tt("dve", macc[:, sl], sgt[:], z[:], ALU.mult, r=[sgt, z], w=[macc])
                        else:
                            k.tt("dve", tm[:], sgt[:], z[:], ALU.mult, r=[sgt, z], w=[tm])
                            k.tt("pool", macc[:, sl], macc[:, sl], tm[:], ALU.add, r=[macc, tm], w=[macc])
                k.cp("act", mergedT[:, j, :], macc[:], r=[macc], w=[mergedT])
        with k.phase() as st:
            wo = [k.sb(f"mwo{l}{i}", [128, 16, 512], BF16, st) for i in range(2)]
            gtb = k.sb(f"mgtb{l}", [128, D], F32, st)
            xt = [k.sb(f"mxt{l}{i}", [128, 512], F32, st) for i in range(3)]
            tm2 = [k.sb(f"mt2{l}{i}", [128, 512], F32, st) for i in range(2)]
            po = [k.psum(f"mpo{l}{i}", [128, 512], F32, st) for i in range(2)]
            k.dma("sp", gtb[:], mods["gt_scr"][l, 0].rearrange("a b -> (a b)").partition_broadcast(128), r=[mods["gt_scr"]], w=[gtb])
            it = 0
            for nd in range(4):
                w = wo[nd % 2]
                k.dma("pool", w[:], w_out[l, :, nd * 512:(nd + 1) * 512].rearrange("(kc p) n -> p kc n", p=128), w=[w])
                for tt in range(16):
                    p = po[it % 2]
                    x = xt[it % 3]
                    t2 = tm2[it % 2]
                    it += 1
                    k.dma(k.dq(), x[:], y[tt * 128:(tt + 1) * 128, nd * 512:(nd + 1) * 512], r=[y], w=[x])
                    for kc in range(16):
                        k.mm(p[:], mergedT[:, kc, tt * 128:(tt + 1) * 128], w[:, kc, :], kc == 0, kc == 15, r=[mergedT, w], w=[p])
                    k.tt("dve", t2[:], p[:], gtb[:, nd * 512:(nd + 1) * 512], ALU.mult, r=[p, gtb], w=[t2])
                    k.tt("pool", x[:], x[:], t2[:], ALU.add, r=[x, t2], w=[x])
                    k.dma(k.dq(), y[tt * 128:(tt + 1) * 128, nd * 512:(nd + 1) * 512], x[:], r=[x], w=[y])


def stage_moe(k, c, hT, l, mods, y, nexp=NE):
    w_router = k.inp("w_router", [k.nl, D, NE])
    b_router = k.inp("b_router", [k.nl, NE])
    w_gu = k.inp("w_gu", [k.nl, NE, D, 2 * DE])
    b_gu = k.inp("b_gu_col", [k.nl, 128, NE * 12])
    w_down = k.inp("w_down", [k.nl, NE, DE, D])
    b_down = k.inp("b_down", [k.nl, NE, D])
    with k.phase() as st0:
        wts = k.sb(f"wts{l}", [128, 16, NE], F32, st0)
        with k.phase() as st:
            wr = k.sb(f"wr{l}", [128, 16, NE], BF16, st)
            brb = k.sb(f"brb{l}", [128, NE], F32, st)
            lg = [k.sb(f"lg{l}{i}", [128, NE], F32, st) for i in range(2)]
            ex = [k.sb(f"ex{l}{i}", [128, NE], F32, st) for i in range(2)]
            mk = [k.sb(f"mk{l}{i}", [128, NE], F32, st) for i in range(2)]
            m8 = [k.sb(f"m8{l}{i}", [128, 8], F32, st) for i in range(2)]
            sm = [k.sb(f"sm{l}{i}", [128, 4], F32, st) for i in range(2)]
            pr = [k.psum(f"pr{l}{i}", [128, 512], F32, st) for i in range(2)]
            k.dma("pool", wr[:], w_router[l].rearrange("(kc p) n -> p kc n", p=128), w=[wr])
            k.dma("sp", brb[:], b_router[l].partition_broadcast(128), w=[brb])
            for tt in range(16):
                i = tt % 2
                for kc in range(16):
                    k.mm(pr[i][:, 0:NE], hT[:, kc, tt * 128:(tt + 1) * 128], wr[:, kc, :], kc == 0, kc == 15, r=[hT, wr], w=[pr[i]])
                k.tt("dve", lg[i][:], pr[i][:, 0:NE], brb[:], ALU.add, r=[pr[i], brb], w=[lg[i]])
                k.P.op("dve", lambda e, a=m8[i], b=lg[i]: e.max(out=a[:], in_=b[:]), reads=[lg[i]], writes=[m8[i]])
                k.ts("dve", mk[i][:], lg[i][:], m8[i][:, 3:4], None, ALU.is_ge, r=[lg[i], m8[i]], w=[mk[i]])
                k.ts("dve", sm[i][:, 0:1], m8[i][:, 0:1], -1.0, None, ALU.mult, r=[m8[i]], w=[sm[i]])
                k.act(ex[i][:], lg[i][:], AF.Exp, r=[lg[i], sm[i]], w=[ex[i]], bias=sm[i][:, 0:1])
                k.stt(ex[i][:], ex[i][:], 1.0, mk[i][:], ALU.mult, ALU.mult, r=[ex[i], mk[i]], w=[ex[i], sm[i]], accum=sm[i][:, 1:2])
                k.recip(sm[i][:, 2:3], sm[i][:, 1:2], r=[sm[i]], w=[sm[i]])
                k.ts("dve", wts[:, tt, :], ex[i][:], sm[i][:, 2:3], None, ALU.mult, r=[ex[i], sm[i]], w=[wts])
        if "wtsd" in k.dbg:
            wd_ = k.dram("wtsd", [128, 16, NE])
            k.dma("sp", wd_[:], wts[:], r=[wts], w=[wd_])
        with k.phase() as st:
            acc = k.sb(f"acc{l}", [128, 8, D], F32, st)
            actT = k.sb(f"actT{l}", [128, 6, 1024], BF16, st)
            wgu = [k.sb(f"wgu{l}{i}", [128, 16, 2, 128], BF16, st) for i in range(2)]
            wdn = k.sb(f"wdn{l}", [128, 6, D], BF16, st)
            bgu = k.sb(f"bgu{l}", [128, NE * 12], F32, st)
            bdn = [k.sb(f"bdn{l}{i}", [1, D], BF16, st) for i in range(1)]
            gt_ = [k.sb(f"eg{l}{i}", [128, 512], F32, st) for i in range(2)]
            st_ = [k.sb(f"es{l}{i}", [128, 512], F32, st) for i in range(1)]
            ut_ = [k.sb(f"eu{l}{i}", [128, 512], F32, st) for i in range(2)]
            pgs = [k.psum(f"epg{l}{i}", [128, 512], F32, st) for i in range(2)]
            pus = [k.psum(f"epu{l}{i}", [128, 512], F32, st) for i in range(2)]
            pys = [k.psum(f"epy{l}{i}", [128, 512], F32, st) for i in range(3)]
            k.dma("sp", bgu[:], b_gu[l], w=[bgu])
            iw = 0
            ie = 0
            iy = 0
            for hf in range(2):
                for e in range(nexp):
                    for m in range(6):
                        w = wgu[iw % 2]
                        iw += 1
                        if not (k.cfg.get("moe_nodma") and (e > 0 or hf > 0)):
                            k.dma("pool", w[:, :, 0, :], w_gu[l, e, :, m * 128:(m + 1) * 128].rearrange("(kc p) n -> p kc n", p=128), w=[w])
                            k.dma("pool", w[:, :, 1, :], w_gu[l, e, :, DE + m * 128:DE + (m + 1) * 128].rearrange("(kc p) n -> p kc n", p=128), w=[w])
                        for n in range(2):
                            tok = slice(hf * 1024 + n * 512, hf * 1024 + (n + 1) * 512)
                            g = pgs[ie % 2]
                            u = pus[ie % 2]
                            gs_ = gt_[ie % 2]
                            ss_ = st_[0]
                            us_ = ut_[ie % 2]
                            ie += 1
                            for kc in range(16):
                                k.mm(g[:], w[:, kc, 0, :], hT[:, kc, tok], kc == 0, kc == 15, r=[w, hT], w=[g])
                            for kc in range(16):
                                k.mm(u[:], w[:, kc, 1, :], hT[:, kc, tok], kc == 0, kc == 15, r=[w, hT], w=[u])
                            cg = e * 12 + m
                            cu = e * 12 + 6 + m
                            k.ts("dve", gs_[:], g[:], bgu[:, cg:cg + 1], 7.0, ALU.add, ALU.min, r=[g, bgu], w=[gs_])
                            k.act(ss_[:], gs_[:], AF.Sigmoid, r=[gs_], w=[ss_], scale=1.702)
                            k.ts("dve", us_[:], u[:], bgu[:, cu:cu + 1], 7.0, ALU.add, ALU.min, r=[u, bgu], w=[us_])
                            k.ts("pool", us_[:], us_[:], -7.0, 1.0, ALU.max, ALU.add, r=[us_], w=[us_])
                            k.tt("pool", gs_[:], gs_[:], ss_[:], ALU.mult, r=[gs_, ss_], w=[gs_])
                            k.tt("dve", actT[:, m, n * 512:(n + 1) * 512], us_[:], gs_[:], ALU.mult, r=[us_, gs_], w=[actT])
                    if not (k.cfg.get("moe_nodma") and (e > 0 or hf > 0)):
                        k.dma("pool", wdn[:], w_down[l, e].rearrange("(kc p) n -> p kc n", p=128), w=[wdn])
                    bd = bdn[0]
                    k.dma("pool", bd[:], b_down[l, e:e + 1, :], w=[bd])
                    for tt in range(8):
                        for nd in range(4):
                            p = pys[iy % 3]
                            iy += 1
                            for kc in range(6):
                                k.mm(p[:], actT[:, kc, tt * 128:(tt + 1) * 128], wdn[:, kc, nd * 512:(nd + 1) * 512], kc == 0, False, r=[actT, wdn], w=[p])
                            k.mm(p[:], c["onesb"][0:1, :], bd[0:1, nd * 512:(nd + 1) * 512], False, True, r=[c["onesb"], bd], w=[p])
                            wc = wts[:, hf * 8 + tt, e:e + 1]
                            a = acc[:, tt, nd * 512:(nd + 1) * 512]
                            if e == 0:
                                k.ts("dve", a, p[:], wc, None, ALU.mult, r=[p, wts], w=[acc])
                            else:
                                k.stt(a, p[:], wc, a, ALU.mult, ALU.add, r=[p, wts, acc], w=[acc])
                with k.phase() as st2:
                    xt = [k.sb(f"ext{l}{hf}{i}", [128, 512], F32, st2) for i in range(2)]
                    gtc = k.sb(f"egtc{l}{hf}", [128, 512], F32, st2)
                    ix = 0
                    for nd in range(4):
                        cs = slice(nd * 512, (nd + 1) * 512)
                        k.dma("sp", gtc[:], mods["gt_scr"][l, 1].rearrange("a b -> (a b)")[nd * 512:(nd + 1) * 512].partition_broadcast(128),
                              r=[mods["gt_scr"]], w=[gtc])
                        for tt in range(8):
                            x = xt[ix % 2]
                            ix += 1
                            rows = slice(hf * 1024 + tt * 128, hf * 1024 + (tt + 1) * 128)
                            k.dma(k.dq(), x[:], y[rows, cs], r=[y], w=[x])
                            k.tt("dve", acc[:, tt, cs], acc[:, tt, cs], gtc[:], ALU.mult, r=[acc, gtc], w=[acc])
                            k.tt("pool", x[:], x[:], acc[:, tt, cs], ALU.add, r=[x, acc], w=[x])
                            k.dma(k.dq(), y[rows, cs], x[:], r=[x], w=[y])


def stage_final(k, c, y):
    gf = k.inp("g_final", [D])
    with k.phase() as st:
        gb = k.sb("gfb", [128, D], F32, st)
        xt = [k.sb(f"fx{i}", [128, D], F32, st) for i in range(2)]
        sq = k.sb("fsq", [128, D], BF16, st)
        ss = [k.sb(f"fss{i}", [128, 4], F32, st) for i in range(2)]
        k.dma("sp", gb[:], gf[:].partition_broadcast(128), w=[gb])
        for tt in range(16):
            x = xt[tt % 2]
            s = ss[tt % 2]
            k.dma(k.dq(), x[:], y[tt * 128:(tt + 1) * 128, :], r=[y], w=[x])
            k.act(sq[:], x[:], AF.Square, r=[x], w=[sq, s], accum=s[:, 0:1])
            k.ts("dve", s[:, 1:2], s[:, 0:1], 1.0 / D, EPS, ALU.mult, ALU.add, r=[s], w=[s])
            k.act(s[:, 2:3], s[:, 1:2], AF.Sqrt, r=[s], w=[s])
            k.recip(s[:, 3:4], s[:, 2:3], r=[s], w=[s])
            k.stt(x[:], x[:], s[:, 3:4], gb[:], ALU.mult, ALU.mult, r=[x, s, gb], w=[x])
            k.dma(k.dq(), y[tt * 128:(tt + 1) * 128, :], x[:], r=[x], w=[y])


def build(nc, stack, cfg):
    k = KB(nc, stack, cfg)
    nl = cfg.get("nl", L)
    k.nl = nl
    stages = cfg.get("stages", "all")
    c = build_consts(k)
    x_in = k.inp("x", [S, D])
    y = T(nc.dram_tensor("y", [S, D], F32, kind="ExternalOutput").ap(), "y")
    k.outs["y"] = y
    w_in = k.inp("w_in", [nl, D, N_IN])
    k.dma("sp", y[0:1024, :], x_in[0:1024, :], w=[y])
    k.dma("act", y[1024:2048, :], x_in[1024:2048, :], w=[y])
    mods = stage_mod(k, c, nl) if (stages == "all" or "mod" in stages or "norm" in stages or "merge" in stages or "moe" in stages) else None
    hT = k.sb("hT", [128, 16, S], BF16)
    hg_tm = k.dram("hg_tm", [S, 2048])
    s5_fm = k.dram("s5_fm", [512, S])
    m2z_tm = k.dram("m2z_tm", [S, 512])
    m2x_fm = k.dram("m2x_fm", [1024, S])
    m2dt_tm = k.dram("m2dt_tm", [S, 8])
    rk_tm = k.dram("rk_tm", [S, 1792])
    if cfg.get("ys_input"):
        ys_fm = k.inp("ys_fm", [4, 512, S], BF16)
    else:
        ys_fm = k.dram("ys_fm", [4, 512, S], BF16)
    if not cfg.get("ys_input"):
        with k.phase() as st:
            zt_ = k.sb("yszero", [128, S], BF16, st)
            k.memset("dve", zt_[:], 0.0, w=[zt_])
            for n_ in range(4):
                for j_ in range(4):
                    k.dma(k.dq(), ys_fm[n_, j_ * 128:(j_ + 1) * 128, :], zt_[:], r=[zt_], w=[ys_fm])
    for l in range(nl):
        if mods is not None:
            stage_norm(k, c, y, hT, mods["gsa"], l, mods["modc"], 0, f"a{l}")
        if "hTd" in k.dbg:
            hdbg = k.dram("hTd", [128, 16, S], BF16) if l == 0 else k.outs["hTd"]
            k.dma("sp", hdbg[:], hT[:], r=[hT], w=[hdbg])
        if stages == "norm":
            continue
        if "proj" in stages or stages == "all":
            with k.phase() as st:
                proj_tm(k, hT, w_in[l], O_HG, 2048, hg_tm, st, f"hg{l}")
            with k.phase() as st:
                proj_fm(k, hT, w_in[l], O_S5, 512, s5_fm, st, f"s5{l}")
            with k.phase() as st:
                proj_tm(k, hT, w_in[l], O_M2, 512, m2z_tm, st, f"mz{l}")
            with k.phase() as st:
                proj_fm(k, hT, w_in[l], O_M2 + 512, 1024, m2x_fm, st, f"mx{l}")
            with k.phase() as st:
                proj_tm(k, hT, w_in[l], O_M2 + 1536, 8, m2dt_tm, st, f"md{l}")
            with k.phase() as st:
                proj_tm(k, hT, w_in[l], O_RK, 1792, rk_tm, st, f"rk{l}")
        if "mix" in stages or stages == "all":
            mixers(k, c, l, dict(hg_tm=hg_tm, s5_fm=s5_fm, m2z_tm=m2z_tm, m2x_fm=m2x_fm, m2dt_tm=m2dt_tm, rk_tm=rk_tm), ys_fm)
        if "merge" in stages or stages == "all":
            w_branch = k.inp("w_branch", [nl, 4, BR, D])
            w_out = k.inp("w_out", [nl, D, D])
            stage_merge(k, c, hT, l, w_in, w_branch, w_out, ys_fm, mods, y)
        if "moe" in stages or stages == "all":
            stage_norm(k, c, y, hT, mods["gsm"], l, mods["modc"], 48, f"m{l}")
            stage_moe(k, c, hT, l, mods, y, nexp=cfg.get("nexp", NE))
    if stages == "all" or "final" in stages:
        stage_final(k, c, y)
    k.P.finish()
    k.P.emit()
    return k


TWO_PI = 6.283185307179586
PI = 3.141592653589793


def mix_s5(k, c, l, s5_fm, ys_fm):
    nl = k.nl
    lr_i = k.inp("s5_lr_col", [nl, 128, 16])
    li_i = k.inp("s5_li_col", [nl, 128, 16])
    ldt_i = k.inp("s5_ldt_col", [nl, 128, 16])
    bTr_i = k.inp("s5_bT_re", [nl, 16, 128, 128])
    bTi_i = k.inp("s5_bT_im", [nl, 16, 128, 128])
    cTr_i = k.inp("s5_cT_re", [nl, 16, 128, 128])
    cTi_i = k.inp("s5_cT_im", [nl, 16, 128, 128])
    dsk_i = k.inp("s5_d_col", [nl, 128, 4])
    wgl_i = k.inp("s5_w_glu", [nl, 512, 512])
    bgl_i = k.inp("s5_bglu_col", [nl, 128, 4])
    with k.phase() as st0:
        sm = lambda n, sh=[128, 16]: k.sb(f"s5{n}{l}", sh, F32, st0)
        mag = sm("mag"); cs = sm("cs"); sn = sm("sn"); cre = sm("cre"); cim = sm("cim")
        pwc = k.sb(f"s5pwc{l}", [128, 11, 16], F32, st0)
        pws = k.sb(f"s5pws{l}", [128, 11, 16], F32, st0)
        bTr = k.sb(f"s5bTr{l}", [128, 16, 128], BF16, st0)
        bTi = k.sb(f"s5bTi{l}", [128, 16, 128], BF16, st0)
        cAr = k.sb(f"s5cAr{l}", [128, 16, 128], BF16, st0)
        cAi = k.sb(f"s5cAi{l}", [128, 16, 128], BF16, st0)
        k.dma("pool", bTr[:], bTr_i[l].rearrange("t p m -> p t m"), w=[bTr])
        k.dma("pool", bTi[:], bTi_i[l].rearrange("t p m -> p t m"), w=[bTi])
        with k.phase() as st:
            t_ = lambda n, sh=[128, 16]: k.sb(f"s5t{n}{l}", sh, F32, st)
            lr = t_("lr"); li = t_("li"); dt = t_("dt"); a1 = t_("a1"); a2 = t_("a2"); mk = t_("mk"); den = t_("den")
            nr = t_("nr"); abr = t_("abr"); abi = t_("abi"); t1 = t_("t1"); t2 = t_("t2")
            k.dma("sp", lr[:], lr_i[l], w=[lr])
            k.dma("sp", li[:], li_i[l], w=[li])
            k.dma("sp", dt[:], ldt_i[l], w=[dt])
            k.act(dt[:], dt[:], AF.Exp, r=[dt], w=[dt])
            k.tt("dve", t1[:], lr[:], dt[:], ALU.mult, r=[lr, dt], w=[t1])
            k.act(mag[:], t1[:], AF.Exp, r=[t1], w=[mag])
            k.tt("dve", a1[:], li[:], dt[:], ALU.mult, r=[li, dt], w=[a1])
            k.ts("dve", a2[:], a1[:], PI / 2, None, ALU.add, r=[a1], w=[a2])
            for a in (a1, a2):
                for _ in range(6):
                    k.ts("dve", mk[:], a[:], PI, TWO_PI, ALU.is_gt, ALU.mult, r=[a], w=[mk])
                    k.tt("dve", a[:], a[:], mk[:], ALU.subtract, r=[a, mk], w=[a])
            k.act(sn[:], a1[:], AF.Sin, r=[a1], w=[sn])
            k.act(cs[:], a2[:], AF.Sin, r=[a2], w=[cs])
            k.tt("dve", abr[:], mag[:], cs[:], ALU.mult, r=[mag, cs], w=[abr])
            k.tt("dve", abi[:], mag[:], sn[:], ALU.mult, r=[mag, sn], w=[abi])
            k.tt("dve", den[:], lr[:], lr[:], ALU.mult, r=[lr], w=[den])
            k.tt("dve", t1[:], li[:], li[:], ALU.mult, r=[li], w=[t1])
            k.tt("dve", den[:], den[:], t1[:], ALU.add, r=[den, t1], w=[den])
            k.recip(den[:], den[:], r=[den], w=[den])
            k.ts("dve", nr[:], abr[:], -1.0, None, ALU.add, r=[abr], w=[nr])
            k.tt("dve", t1[:], nr[:], lr[:], ALU.mult, r=[nr, lr], w=[t1])
            k.tt("dve", t2[:], abi[:], li[:], ALU.mult, r=[abi, li], w=[t2])
            k.tt("dve", t1[:], t1[:], t2[:], ALU.add, r=[t1, t2], w=[t1])
            k.tt("dve", cre[:], t1[:], den[:], ALU.mult, r=[t1, den], w=[cre])
            k.tt("dve", t1[:], abi[:], lr[:], ALU.mult, r=[abi, lr], w=[t1])
            k.tt("dve", t2[:], nr[:], li[:], ALU.mult, r=[nr, li], w=[t2])
            k.tt("dve", t1[:], t1[:], t2[:], ALU.subtract, r=[t1, t2], w=[t1])
            k.tt("dve", cim[:], t1[:], den[:], ALU.mult, r=[t1, den], w=[cim])
            k.cp("dve", pwc[:, 0, :], cs[:], r=[cs], w=[pwc])
            k.cp("dve", pws[:, 0, :], sn[:], r=[sn], w=[pws])
            for jj in range(1, 11):
                k.tt("dve", t1[:], pwc[:, jj - 1, :], pwc[:, jj - 1, :], ALU.mult, r=[pwc], w=[t1])
                k.tt("dve", t2[:], pws[:, jj - 1, :], pws[:, jj - 1, :], ALU.mult, r=[pws], w=[t2])
                k.tt("dve", pwc[:, jj, :], t1[:], t2[:], ALU.subtract, r=[t1, t2], w=[pwc])
                k.tt("dve", t1[:], pwc[:, jj - 1, :], pws[:, jj - 1, :], ALU.mult, r=[pwc, pws], w=[t1])
                k.ts("dve", pws[:, jj, :], t1[:], 2.0, None, ALU.mult, r=[t1], w=[pws])
            cr = k.sb(f"s5cr{l}", [128, 16, 128], F32, st)
            ci = k.sb(f"s5ci{l}", [128, 16, 128], F32, st)
            tm = [k.sb(f"s5tm{l}{i}", [128, 128], F32, st) for i in range(2)]
            k.dma("sp", cr[:], cTr_i[l].rearrange("t p m -> p t m"), w=[cr])
            k.dma("act", ci[:], cTi_i[l].rearrange("t p m -> p t m"), w=[ci])
            ncim = t_("ncim")
            k.ts("dve", ncim[:], cim[:], -1.0, None, ALU.mult, r=[cim], w=[ncim])
            for t in range(16):
                a = tm[t % 2]
                k.ts("dve", a[:], cr[:, t, :], cre[:, t:t + 1], None, ALU.mult, r=[cr, cre], w=[a])
                k.stt(cAr[:, t, :], ci[:, t, :], ncim[:, t:t + 1], a[:], ALU.mult, ALU.add, r=[ci, ncim, a], w=[cAr])
                k.ts("dve", a[:], cr[:, t, :], ncim[:, t:t + 1], None, ALU.mult, r=[cr, ncim], w=[a])
                k.ts("dve", ci[:, t, :], ci[:, t, :], cre[:, t:t + 1], -1.0, ALU.mult, ALU.mult, r=[ci, cre], w=[ci])
                k.tt("dve", cAi[:, t, :], ci[:, t, :], a[:], ALU.add, r=[ci, a], w=[cAi])
        with k.phase() as st:
            tabc = k.sb(f"s5tabc{l}", [128, S], F32, st)
            tabs = k.sb(f"s5tabs{l}", [128, S], F32, st)
            bur = k.sb(f"s5bur{l}", [128, S], F32, st)
            bui = k.sb(f"s5bui{l}", [128, S], F32, st)
            tA = k.sb(f"s5tA{l}", [128, S], F32, st)
            tB = k.sb(f"s5tB{l}", [128, S], F32, st)
            tC = k.sb(f"s5tC{l}", [128, S], F32, st)
            Rt = k.sb(f"s5R{l}", [128, S], F32, st)
            xr = k.sb(f"s5xr{l}", [128, S], BF16, st)
            xi = k.sb(f"s5xi{l}", [128, S], BF16, st)
            ub = k.sb(f"s5ub{l}", [128, S], BF16, st)
            uf = k.sb(f"s5uf{l}", [128, S], F32, st)
            ygb = k.sb(f"s5ygb{l}", [128, 4, S], BF16, st)
            dsk = k.sb(f"s5dsk{l}", [128, 4], F32, st)
            bgl = k.sb(f"s5bgl{l}", [128, 4], F32, st)
            wgl = k.sb(f"s5wgl{l}", [128, 4, 512], BF16, st)
            pb = [k.psum(f"s5pb{l}{i}", [128, 512], F32, st) for i in range(4)]
            py = [k.psum(f"s5py{l}{i}", [128, 512], F32, st) for i in range(4)]
            k.dma("sp", dsk[:], dsk_i[l], w=[dsk])
            k.dma("sp", bgl[:], bgl_i[l], w=[bgl])
            k.dma("pool", wgl[:], wgl_i[l].rearrange("(kc p) n -> p kc n", p=128), w=[wgl])
            for ct in range(4):
                k.dma("pool", ub[:], s5_fm[ct * 128:(ct + 1) * 128, :], r=[s5_fm], w=[ub])
                k.dma("sp", uf[:], s5_fm[ct * 128:(ct + 1) * 128, :], r=[s5_fm], w=[uf])
                for s4 in range(4):
                    stt_ = ct * 4 + s4
                    k.memset("pool", tabc[:, 0:1], 1.0, w=[tabc])
                    k.memset("pool", tabs[:, 0:1], 0.0, w=[tabs])
                    for jj in range(11):
                        n = 1 << jj
                        pc = pwc[:, jj, stt_:stt_ + 1]
                        ps_ = pws[:, jj, stt_:stt_ + 1]
                        k.ts("dve", tA[:, 0:n], tabs[:, 0:n], ps_, None, ALU.mult, r=[tabs, pws], w=[tA])
                        k.ts("dve", tB[:, 0:n], tabs[:, 0:n], pc, None, ALU.mult, r=[tabs, pwc], w=[tB])
                        k.stt(tC[:, 0:n], tabc[:, 0:n], pc, tA[:, 0:n], ALU.mult, ALU.subtract, r=[tabc, pwc, tA], w=[tC])
                        k.stt(tabs[:, n:2 * n], tabc[:, 0:n], ps_, tB[:, 0:n], ALU.mult, ALU.add, r=[tabc, pws, tB], w=[tabs])
                        k.cp("pool", tabc[:, n:2 * n], tC[:, 0:n], r=[tC], w=[tabc])
                    k.ts("dve", Rt[:], tabc[:], 0.0, mag[:, stt_:stt_ + 1], ALU.mult, ALU.add, r=[tabc, mag], w=[Rt])
                    for n4 in range(4):
                        sl = slice(n4 * 512, (n4 + 1) * 512)
                        p1 = pb[(n4 % 2) * 2]
                        p2 = pb[(n4 % 2) * 2 + 1]
                        k.mm(p1[:], bTr[:, stt_, :], ub[:, sl], True, True, r=[bTr, ub], w=[p1])
                        k.mm(p2[:], bTi[:, stt_, :], ub[:, sl], True, True, r=[bTi, ub], w=[p2])
                        k.cp("act", bur[:, sl], p1[:], r=[p1], w=[bur])
                        k.cp("act", bui[:, sl], p2[:], r=[p2], w=[bui])
                    k.tt("dve", tA[:], tabc[:], bur[:], ALU.mult, r=[tabc, bur], w=[tA])
                    k.tt("pool", tB[:], tabs[:], bui[:], ALU.mult, r=[tabs, bui], w=[tB])
                    k.tt("dve", tA[:], tA[:], tB[:], ALU.add, r=[tA, tB], w=[tA])
                    k.tt("pool", tB[:], tabc[:], bui[:], ALU.mult, r=[tabc, bui], w=[tB])
                    k.tt("dve", tC[:], tabs[:], bur[:], ALU.mult, r=[tabs, bur], w=[tC])
                    k.tt("pool", tB[:], tB[:], tC[:], ALU.subtract, r=[tB, tC], w=[tB])
                    k.P.op("dve", lambda e: e.tensor_tensor_scan(out=bur[:], data0=Rt[:], data1=tA[:], initial=0.0, op0=ALU.mult, op1=ALU.add),
                           reads=[Rt, tA], writes=[bur])
                    k.P.op("dve", lambda e: e.tensor_tensor_scan(out=bui[:], data0=Rt[:], data1=tB[:], initial=0.0, op0=ALU.mult, op1=ALU.add),
                           reads=[Rt, tB], writes=[bui])
                    k.tt("dve", tA[:], tabc[:], bur[:], ALU.mult, r=[tabc, bur], w=[tA])
                    k.tt("pool", tC[:], tabs[:], bui[:], ALU.mult, r=[tabs, bui], w=[tC])
                    k.tt("dve", xr[:], tA[:], tC[:], ALU.subtract, r=[tA, tC], w=[xr])
                    k.tt("pool", tB[:], tabs[:], bur[:], ALU.mult, r=[tabs, bur], w=[tB])
                    k.tt("dve", tC[:], tabc[:], bui[:], ALU.mult, r=[tabc, bui], w=[tC])
                    k.tt("pool", xi[:], tB[:], tC[:], ALU.add, r=[tB, tC], w=[xi])
                    for n4 in range(4):
                        sl = slice(n4 * 512, (n4 + 1) * 512)
                        k.mm(py[n4][:], cAr[:, stt_, :], xr[:, sl], s4 == 0, False, r=[cAr, xr], w=[py[n4]])
                        k.mm(py[n4][:], cAi[:, stt_, :], xi[:, sl], False, s4 == 3, r=[cAi, xi], w=[py[n4]])
                for n4 in range(4):
                    sl = slice(n4 * 512, (n4 + 1) * 512)
                    k.stt(tA[:, sl], uf[:, sl], dsk[:, ct:ct + 1], py[n4][:], ALU.mult, ALU.add, r=[uf, dsk, py[n4]], w=[tA])
                k.tt("pool", tB[:], tA[:], tA[:], ALU.mult, r=[tA], w=[tB])
                k.ts("dve", tB[:], tB[:], 0.044715, 1.0, ALU.mult, ALU.add, r=[tB], w=[tB])
                k.tt("pool", tB[:], tB[:], tA[:], ALU.mult, r=[tB, tA], w=[tB])
                k.act(tB[:], tB[:], AF.Tanh, r=[tB], w=[tB], scale=0.7978845608028654)
                k.ts("dve", tB[:], tB[:], 1.0, 0.5, ALU.add, ALU.mult, r=[tB], w=[tB])
                k.tt("dve", ygb[:, ct, :], tB[:], tA[:], ALU.mult, r=[tB, tA], w=[ygb])
            for ct in range(4):
                for n4 in range(4):
                    sl = slice(n4 * 512, (n4 + 1) * 512)
                    p = pb[n4]
                    for kc in range(4):
                        k.mm(p[:], wgl[:, kc, ct * 128:(ct + 1) * 128], ygb[:, kc, sl], kc == 0, kc == 3, r=[wgl, ygb], w=[p])
                    k.act(tA[:, sl], p[:], AF.Sigmoid, r=[p, bgl], w=[tA], bias=bgl[:, ct:ct + 1])
                k.tt("dve", xr[:], tA[:], ygb[:, ct, :], ALU.mult, r=[tA, ygb], w=[xr])
                k.dma("sp", ys_fm[1, ct * 128:(ct + 1) * 128, :], xr[:], r=[xr], w=[ys_fm])


def mix_ssd(k, c, l, m2z_tm, m2x_fm, m2dt_tm, ys_fm):
    nl = k.nl
    cw_i = k.inp("m2_convw_col", [nl, 128, 8, 4])
    cb_i = k.inp("m2_convb_col", [nl, 128, 8])
    dtb_i = k.inp("m2_dt_bias", [nl, 8])
    alog_i = k.inp("m2_a_log", [nl, 8])
    dsk_i = k.inp("m2_d", [nl, 8])
    nrm_i = k.inp("m2_norm", [nl, 512])
    triu = c["triu"]
    with k.phase() as st0:
        xcb = k.sb(f"mxcb{l}", [128, 8, S], BF16, st0)
        ysc = k.sb(f"mysc{l}", [128, 4, S], BF16, st0)
        with k.phase() as st:
            cw = k.sb(f"mcw{l}", [128, 8, 4], F32, st)
            cb = k.sb(f"mcb{l}", [128, 8], F32, st)
            xin = [k.sb(f"mxin{l}{i}", [128, S], F32, st) for i in range(2)]
            ac = [k.sb(f"mac{l}{i}", [128, S], F32, st) for i in range(2)]
            k.dma("sp", cw[:], cw_i[l], w=[cw])
            k.dma("sp", cb[:], cb_i[l], w=[cb])
            for ct in range(8):
                x = xin[ct % 2]
                a = ac[ct % 2]
                k.dma(k.dq(), x[:], m2x_fm[ct * 128:(ct + 1) * 128, :], r=[m2x_fm], w=[x])
                k.ts("dve", a[:], x[:], cw[:, ct, 3:4], cb[:, ct:ct + 1], ALU.mult, ALU.add, r=[x, cw, cb], w=[a])
                for sh in (1, 2, 3):
                    k.stt(a[:, sh:S], x[:, 0:S - sh], cw[:, ct, 3 - sh:4 - sh], a[:, sh:S], ALU.mult, ALU.add, r=[x, cw, a], w=[a])
                k.act(xcb[:, ct, :], a[:], AF.Silu, r=[a], w=[xcb])
        with k.phase() as st:
            f = lambda n, sh, dt=F32: k.sb(f"m{n}{l}", sh, dt, st)
            dtb = f("dtb", [128, 8]); aneg = f("aneg", [128, 8]); dskb = f("dskb", [128, 8]); nrmb = f("nrmb", [128, 512])
            ST = f("ST", [128, 8, 64]); STb = f("STb", [128, 8, 64], BF16)
            xs_t = f("xst", [128, 8, 64]); B_t = f("Bt", [128, 2, 128], BF16)
            dtt = f("dtt", [128, 8]); adt = f("adt", [128, 8]); acs = f("acs", [128, 8]); nacs = f("nacs", [128, 8])
            lastb = f("lastb", [128, 8]); elast = f("elast", [128, 8]); eac = f("eac", [128, 8]); dcs = f("dcs", [128, 8])
            rhsh = [f(f"rhsh{i}", [128, 128]) for i in range(2)]
            Ld = [f(f"Ld{i}", [128, 128]) for i in range(2)]
            Mh = [f(f"Mh{i}", [128, 128], BF16) for i in range(2)]
            xdt = f("xdt", [128, 8, 64], BF16); xdw = f("xdw", [128, 8, 64], BF16)
            ysb = f("ysb", [128, 512]); zt = f("zt", [128, 512]); sq = f("sq", [128, 512]); ynb = f("ynb", [128, 512], BF16)
            ss = f("ss", [128, 4])
            pT = k.psum(f"mpT{l}", [128, 8, 128], BF16, st)
            pac = k.psum(f"mpac{l}", [128, 512], F32, st)
            prow = [k.psum(f"mprow{l}{i}", [128, 512], F32, st) for i in range(2)]
            pcb = k.psum(f"mpcb{l}", [128, 4, 128], F32, st)
            pyd = k.psum(f"mpyd{l}", [128, 512], F32, st)
            pyo = k.psum(f"mpyo{l}", [128, 512], F32, st)
            pst = k.psum(f"mpst{l}", [128, 512], F32, st)
            k.dma("sp", dtb[:], dtb_i[l].partition_broadcast(128), w=[dtb])
            k.dma("sp", aneg[:], alog_i[l].partition_broadcast(128), w=[aneg])
            k.dma("sp", dskb[:], dsk_i[l].partition_broadcast(128), w=[dskb])
            k.dma("sp", nrmb[:], nrm_i[l].partition_broadcast(128), w=[nrmb])
            k.act(aneg[:], aneg[:], AF.Exp, r=[aneg], w=[aneg])
            k.ts("dve", aneg[:], aneg[:], -1.0, None, ALU.mult, r=[aneg], w=[aneg])
            k.memset("dve", ST[:], 0.0, w=[ST])
            k.memset("dve", STb[:], 0.0, w=[STb])
            idb = c["idb"]
            for ch in range(k.cfg.get("ssd_ch0", 0), k.cfg.get("ssd_ch0", 0) + k.cfg.get("ssd_chunks", 16)):
                tok = slice(ch * 128, (ch + 1) * 128)
                for j in range(4):
                    k.tr(pT[:, j, :], xcb[:, j, tok], idb[:], r=[xcb, idb], w=[pT])
                for g in range(2):
                    k.tr(pT[:, 4 + g, :], xcb[:, 4 + g, tok], idb[:], r=[xcb, idb], w=[pT])
                k.cp("act", xs_t[:].rearrange("p h d -> p (h d)"), pT[:, 0:4, :].rearrange("p a b -> p (a b)"), r=[pT], w=[xs_t])
                k.cp("act", B_t[:].rearrange("p g n -> p (g n)"), pT[:, 4:6, :].rearrange("p a b -> p (a b)"), r=[pT], w=[B_t])
                lvl = k.cfg.get("ssd_lvl", 9)
                if lvl >= 2:
                    k.dma("sp", dtt[:], m2dt_tm[tok, :], r=[m2dt_tm], w=[dtt])
                    k.tt("dve", dtt[:], dtt[:], dtb[:], ALU.add, r=[dtt, dtb], w=[dtt])
                    k.act(dtt[:], dtt[:], AF.Exp, r=[dtt], w=[dtt])
                    k.act(dtt[:], dtt[:], AF.Ln, r=[dtt], w=[dtt], bias=1.0)
                    k.tt("dve", adt[:], dtt[:], aneg[:], ALU.mult, r=[dtt, aneg], w=[adt])
                    k.mm(pac[:, 0:8], triu[:], adt[:], True, True, r=[triu, adt], w=[pac])
                    k.mm(pac[:, 8:16], c["ones"][:], adt[:], True, True, r=[c["ones"], adt], w=[pac])
                    k.cp("dve", acs[:], pac[:, 0:8], r=[pac], w=[acs])
                    k.ts("dve", nacs[:], pac[:, 0:8], -1.0, None, ALU.mult, r=[pac], w=[nacs])
                    k.cp("dve", lastb[:], pac[:, 8:16], r=[pac], w=[lastb])
                    k.act(elast[:], lastb[:], AF.Exp, r=[lastb], w=[elast])
                    k.act(eac[:], acs[:], AF.Exp, r=[acs], w=[eac])
                    k.tt("dve", dcs[:], lastb[:], acs[:], ALU.subtract, r=[lastb, acs], w=[dcs])
                    k.act(dcs[:], dcs[:], AF.Exp, r=[dcs], w=[dcs])
                if lvl >= 3:
                    for h in range(8):
                        k.ts("dve", xdt[:, h, :], xs_t[:, h, :], dtt[:, h:h + 1], None, ALU.mult, r=[xs_t, dtt], w=[xdt])
                        k.ts("dve", xdw[:, h, :], xs_t[:, h, :], dtt[:, h:h + 1], dcs[:, h:h + 1], ALU.mult, ALU.mult, r=[xs_t, dtt, dcs], w=[xdw])
                    for g in range(2):
                        k.mm(pcb[:, g, :], xcb[:, 4 + g, tok], xcb[:, 6 + g, tok], True, True, r=[xcb], w=[pcb])
                if lvl >= 4:
                    for h in range(8):
                        i2 = h % 2
                        k.ts("dve", rhsh[i2][:], triu[:], adt[:, h:h + 1], None, ALU.mult, r=[triu, adt], w=[rhsh[i2]])
                        k.mm(prow[i2][:, 0:128], c["ones"][:], rhsh[i2][:], True, True, r=[c["ones"], rhsh[i2]], w=[prow[i2]])
                        k.ts("dve", Ld[i2][:], prow[i2][:, 0:128], nacs[:, h:h + 1], 0.0, ALU.add, ALU.min, r=[prow[i2], nacs], w=[Ld[i2]])
                        k.act(Ld[i2][:], Ld[i2][:], AF.Exp, r=[Ld[i2]], w=[Ld[i2]])
                        k.tt("pool", Ld[i2][:], Ld[i2][:], triu[:], ALU.mult, r=[Ld[i2], triu], w=[Ld[i2]])
                        k.tt("dve", Mh[i2][:], pcb[:, h // 4, :], Ld[i2][:], ALU.mult, r=[pcb, Ld[i2]], w=[Mh[i2]])
                        k.mm(pyd[:, h * 64:(h + 1) * 64], Mh[i2][:], xdt[:, h, :], True, True, r=[Mh[i2], xdt], w=[pyd])
                        k.mm(pyo[:, h * 64:(h + 1) * 64], xcb[:, 6 + h // 4, tok], STb[:, h, :], True, True, r=[xcb, STb], w=[pyo])
                        k.mm(pst[:, h * 64:(h + 1) * 64], B_t[:, h // 4, :], xdw[:, h, :], True, True, r=[B_t, xdw], w=[pst])
                if lvl >= 5:
                    if ch == 0:
                        k.dump("d_xcb", xcb, [128, 8, S], BF16)
                        k.dump("d_dtt", dtt, [128, 8]); k.dump("d_acs", acs, [128, 8]); k.dump("d_lastb", lastb, [128, 8])
                        k.dump("d_Ld", Ld[1], [128, 128]); k.dump("d_xdt", xdt, [128, 8, 64], BF16); k.dump("d_xst", xs_t, [128, 8, 64])
                    k.cp("act", ysb[:], pyd[:], r=[pyd], w=[ysb])
                    if ch == 0:
                        k.dump("d_yd", ysb, [128, 512])
                    for h in range(8):
                        hs = slice(h * 64, (h + 1) * 64)
                        k.stt(ysb[:, hs], pyo[:, hs], eac[:, h:h + 1], ysb[:, hs], ALU.mult, ALU.add, r=[pyo, eac, ysb], w=[ysb])
                        k.stt(ysb[:, hs], xs_t[:, h, :], dskb[:, h:h + 1], ysb[:, hs], ALU.mult, ALU.add, r=[xs_t, dskb, ysb], w=[ysb])
                        k.stt(ST[:, h, :], ST[:, h, :], elast[:, h:h + 1], pst[:, hs], ALU.mult, ALU.add, r=[ST, elast, pst], w=[ST])
                    k.cp("pool", STb[:], ST[:], r=[ST], w=[STb])
                if lvl >= 6:
                    k.dma("act", zt[:], m2z_tm[tok, :], r=[m2z_tm], w=[zt])
                    k.act(zt[:], zt[:], AF.Silu, r=[zt], w=[zt])
                    k.tt("dve", ysb[:], ysb[:], zt[:], ALU.mult, r=[ysb, zt], w=[ysb])
                    if ch == 0:
                        k.dump("d_yg", ysb, [128, 512])
                    k.tt("pool", sq[:], ysb[:], ysb[:], ALU.mult, r=[ysb], w=[sq])
                    k.red("dve", ss[:, 0:2], sq[:].rearrange("p (g d) -> p g d", g=2), ALU.add, r=[sq], w=[ss])
                    k.ts("dve", ss[:, 0:2], ss[:, 0:2], 1.0 / 256, EPS, ALU.mult, ALU.add, r=[ss], w=[ss])
                    k.act(ss[:, 0:2], ss[:, 0:2], AF.Sqrt, r=[ss], w=[ss])
                    k.recip(ss[:, 2:4], ss[:, 0:2], r=[ss], w=[ss])
                    for g in range(2):
                        gs_ = slice(g * 256, (g + 1) * 256)
                        k.stt(ynb[:, gs_], ysb[:, gs_], ss[:, 2 + g:3 + g], nrmb[:, gs_], ALU.mult, ALU.mult, r=[ysb, ss, nrmb], w=[ynb])
                    if ch == 0:
                        k.dump("d_ss", ss, [128, 4]); k.dump("d_ynb", ynb, [128, 512], BF16)
                    for j in range(4):
                        k.tr(pT[:, j, :], ynb[:, j * 128:(j + 1) * 128], idb[:], r=[ynb, idb], w=[pT])
                    k.cp("act", ysc[:, :, tok], pT[:, 0:4, :], r=[pT], w=[ysc])
                if k.cfg.get("ssd_barrier", True):
                    k.P.barrier()
        for j in range(4):
            k.dma(k.dq(), ys_fm[2, j * 128:(j + 1) * 128, :], ysc[:, j, :], r=[ysc], w=[ys_fm])


def mix_hgrn2(k, c, l, hg_tm, ys_fm):
    nl = k.nl
    lb_i = k.inp("hg_lower_bound", [L, 512])
    on_i = k.inp("hg_onorm", [nl, 128])
    triu = c["triu"]
    T_ = 64
    with k.phase() as st0:
        ysc = k.sb(f"hysc{l}", [128, 4, S], BF16, st0)
        lbb = k.sb(f"hlbb{l}", [T_, 512], F32, st0)
        omlb = k.sb(f"homlb{l}", [T_, 512], F32, st0)
        onb = k.sb(f"honb{l}", [T_, 512], F32, st0)
        mmid = k.sb(f"hmmid{l}", [T_, T_], F32, st0)
        mask4 = k.sb(f"hmask4{l}", [T_, 4, T_], F32, st0)
        with k.phase() as st:
            raw = k.sb(f"hraw{l}", [T_, L, 512], F32, st)
            mx = k.sb(f"hmx{l}", [T_, 512], F32, st)
            sm = k.sb(f"hsm{l}", [T_, 512], F32, st)
            for j in range(L):
                k.dma(k.dq(), raw[:, j, :], lb_i[j].partition_broadcast(T_), w=[raw])
            k.tt("dve", mx[:], raw[:, 0, :], raw[:, 1, :], ALU.max, r=[raw], w=[mx])
            for j in range(2, L):
                k.tt("dve", mx[:], mx[:], raw[:, j, :], ALU.max, r=[raw, mx], w=[mx])
            for j in range(L):
                k.tt("dve", raw[:, j, :], raw[:, j, :], mx[:], ALU.subtract, r=[raw, mx], w=[raw])
                k.act(raw[:, j, :], raw[:, j, :], AF.Exp, r=[raw], w=[raw])
            k.tt("dve", sm[:], raw[:, 0, :], raw[:, 1, :], ALU.add, r=[raw], w=[sm])
            for j in range(2, L):
                k.tt("dve", sm[:], sm[:], raw[:, j, :], ALU.add, r=[raw, sm], w=[sm])
            k.recip(sm[:], sm[:], r=[sm], w=[sm])
            k.memset("dve", lbb[:], 0.0, w=[lbb])
            for j in range(1, l + 1):
                k.tt("dve", lbb[:], lbb[:], raw[:, j, :], ALU.add, r=[raw, lbb], w=[lbb])
            k.tt("dve", lbb[:], lbb[:], sm[:], ALU.mult, r=[lbb, sm], w=[lbb])
            k.ts("dve", omlb[:], lbb[:], -1.0, 1.0, ALU.mult, ALU.add, r=[lbb], w=[omlb])
            for h in range(4):
                k.dma(k.dq(), onb[:, h * 128:(h + 1) * 128], on_i[l].partition_broadcast(T_), w=[onb])
            k.memset("pool", mmid[:], 1.0, w=[mmid])
            k.P.op("pool", lambda e: e.affine_select(out=mmid[:], in_=mmid[:], pattern=[[0, T_]], compare_op=ALU.is_ge,
                                                      fill=0.0, base=T_ // 2 - 1, channel_multiplier=-1), reads=[mmid], writes=[mmid])
            for h in range(4):
                k.cp("pool", mask4[:, h, :], triu[0:T_, 0:T_], r=[triu], w=[mask4])
        with k.phase() as st:
            f = lambda n, sh, dt=F32: k.sb(f"h{n}{l}", sh, dt, st)
            pin = [f(f"pin{i}", [T_, 2048]) for i in range(2)]
            sf = f("sf", [T_, 512]); fg = f("fg", [T_, 512]); lf = f("lf", [T_, 512]); kk = f("kk", [T_, 512]); qs = f("qs", [T_, 512])
            cums = f("cums", [T_, 512]); d1 = f("d1", [T_, 512]); d4 = f("d4", [T_, 512]); ex = [f(f"ex{i}", [T_, 512]) for i in range(2)]
            qkb = f("qkb", [T_, 3, 512], BF16)
            ksb = f("ksb", [T_, 512], BF16); vb = f("vb", [T_, 512], BF16)
            qkT = f("qkT", [128, 12, T_], BF16)
            scb = f("scb", [T_, 4, T_], BF16); sct = f("sct", [T_, 4 * T_])
            state = f("state", [128, 4, 128]); stb = f("stb", [128, 4, 128], BF16)
            eL = f("eL", [128, 4])
            osb = f("osb", [T_, 512]); sq = f("sq", [T_, 512]); ss = f("ss", [T_, 8]); sg = f("sg", [T_, 512]); yb = f("yb", [T_, 512], BF16)
            pc = k.psum(f"hpc{l}", [128, 512], F32, st); pm = k.psum(f"hpm{l}", [128, 512], F32, st); pl = k.psum(f"hpl{l}", [128, 512], F32, st)
            pT = k.psum(f"hpT{l}", [128, 16, T_], BF16, st)
            psc = k.psum(f"hpsc{l}", [128, 512], F32, st); po = k.psum(f"hpo{l}", [128, 512], F32, st)
            pst = k.psum(f"hpst{l}", [128, 512], F32, st); pL = k.psum(f"hpL{l}", [128, 512], F32, st)
            idb = c["idb"]
            ones = c["ones"]
            k.memset("dve", state[:], 0.0, w=[state])
            k.memset("dve", stb[:], 0.0, w=[stb])
            nch = k.cfg.get("hg_chunks", S // T_)
            for ch in range(nch):
                tok = slice(ch * T_, (ch + 1) * T_)
                p = pin[ch % 2]
                k.dma(k.dq(), p[:], hg_tm[tok, :], r=[hg_tm], w=[p])
                q_ = p[:, 0:512]; f_ = p[:, 512:1024]; i_ = p[:, 1024:1536]; g_ = p[:, 1536:2048]
                k.act(sf[:], f_, AF.Sigmoid, r=[p], w=[sf])
                k.tt("dve", fg[:], sf[:], omlb[:], ALU.mult, r=[sf, omlb], w=[fg])
                k.tt("dve", fg[:], fg[:], lbb[:], ALU.add, r=[fg, lbb], w=[fg])
                k.act(lf[:], fg[:], AF.Ln, r=[fg], w=[lf])
                k.ts("pool", kk[:], fg[:], -1.0, 1.0, ALU.mult, ALU.add, r=[fg], w=[kk])
                k.act(qs[:], q_, AF.Silu, r=[p], w=[qs])
                k.cp("pool", vb[:], i_, r=[p], w=[vb])
                k.mm(pc[0:T_, :], triu[0:T_, 0:T_], lf[:], True, True, r=[triu, lf], w=[pc])
                k.mm(pm[0:T_, :], mmid[:], lf[:], True, True, r=[mmid, lf], w=[pm])
                k.mm(pl[0:T_, :], ones[0:T_, 0:T_], lf[:], True, True, r=[ones, lf], w=[pl])
                for h in range(4):
                    k.mm(pL[:, h:h + 1], lf[:, h * 128:(h + 1) * 128], ones[0:T_, 0:1], True, True, r=[lf, ones], w=[pL])
                k.cp("act", cums[:], pc[0:T_, :], r=[pc], w=[cums])
                k.tt("dve", d1[:], cums[:], pm[0:T_, :], ALU.subtract, r=[cums, pm], w=[d1])
                k.ts("dve", d1[:], d1[:], 85.0, -85.0, ALU.min, ALU.max, r=[d1], w=[d1])
                k.tt("dve", d4[:], pl[0:T_, :], cums[:], ALU.subtract, r=[pl, cums], w=[d4])
                k.act(ex[0][:], d1[:], AF.Exp, r=[d1], w=[ex[0]])
                k.tt("dve", qkb[:, 0, :], qs[:], ex[0][:], ALU.mult, r=[qs, ex[0]], w=[qkb])
                k.act(ex[1][:], d1[:], AF.Exp, r=[d1], w=[ex[1]], scale=-1.0)
                k.tt("pool", qkb[:, 1, :], kk[:], ex[1][:], ALU.mult, r=[kk, ex[1]], w=[qkb])
                k.act(ex[0][:], cums[:], AF.Exp, r=[cums], w=[ex[0]])
                k.tt("dve", qkb[:, 2, :], qs[:], ex[0][:], ALU.mult, r=[qs, ex[0]], w=[qkb])
                k.act(ex[1][:], d4[:], AF.Exp, r=[d4], w=[ex[1]])
                k.tt("pool", ksb[:], kk[:], ex[1][:], ALU.mult, r=[kk, ex[1]], w=[ksb])
                k.act(eL[:], pL[:, 0:4], AF.Exp, r=[pL], w=[eL])
                for a in range(3):
                    for h in range(4):
                        k.tr(pT[:, a * 4 + h, :], qkb[:, a, h * 128:(h + 1) * 128], idb[0:T_, 0:T_], r=[qkb, idb], w=[pT])
                k.cp("act", qkT[:], pT[:, 0:12, :], r=[pT], w=[qkT])
                for h in range(4):
                    k.mm(psc[0:T_, h * T_:(h + 1) * T_], qkT[:, 4 + h, :], qkT[:, h, :], True, True, r=[qkT], w=[psc])
                k.ts("dve", sct[:], psc[0:T_, 0:4 * T_], -3.0e38, 3.0e38, ALU.max, ALU.min, r=[psc], w=[sct])
                k.tt("dve", scb[:].rearrange("p h t -> p (h t)"), sct[:], mask4[:].rearrange("p h t -> p (h t)"), ALU.mult,
                     r=[sct, mask4], w=[scb])
                for h in range(4):
                    hs = slice(h * 128, (h + 1) * 128)
                    k.mm(po[0:T_, hs], scb[:, h, :], vb[:, hs], True, False, r=[scb, vb], w=[po])
                    k.mm(po[0:T_, hs], qkT[:, 8 + h, :], stb[:, h, :], False, True, r=[qkT, stb], w=[po])
                for h in range(4):
                    hs = slice(h * 128, (h + 1) * 128)
                    k.mm(pst[:, hs], ksb[:, hs], vb[:, hs], True, True, r=[ksb, vb], w=[pst])
                for h in range(4):
                    hs = slice(h * 128, (h + 1) * 128)
                    k.stt(state[:, h, :], state[:, h, :], eL[:, h:h + 1], pst[:, hs], ALU.mult, ALU.add, r=[state, eL, pst], w=[state])
                k.cp("pool", stb[:], state[:], r=[state], w=[stb])
                k.cp("act", osb[:], po[0:T_, :], r=[po], w=[osb])
                if ch == 1:
                    k.dump("h_lf", lf, [T_, 512]); k.dump("h_cums", cums, [T_, 512]); k.dump("h_d1", d1, [T_, 512]); k.dump("h_d4", d4, [T_, 512])
                    k.dump("h_qkb", qkb, [T_, 3, 512], BF16); k.dump("h_qkT", qkT, [128, 12, T_], BF16); k.dump("h_scb", scb, [T_, 4, T_], BF16)
                    k.dump("h_osb", osb, [T_, 512]); k.dump("h_eL", eL, [128, 4]); k.dump("h_state", state, [128, 4, 128]); k.dump("h_ksb", ksb, [T_, 512], BF16)
                k.tt("pool", sq[:], osb[:], osb[:], ALU.mult, r=[osb], w=[sq])
                k.red("dve", ss[:, 0:4], sq[:].rearrange("p (h d) -> p h d", h=4), ALU.add, r=[sq], w=[ss])
                k.ts("dve", ss[:, 0:4], ss[:, 0:4], 1.0 / 128, EPS, ALU.mult, ALU.add, r=[ss], w=[ss])
                k.act(ss[:, 0:4], ss[:, 0:4], AF.Sqrt, r=[ss], w=[ss])
                k.recip(ss[:, 4:8], ss[:, 0:4], r=[ss], w=[ss])
                k.act(sg[:], g_, AF.Silu, r=[p], w=[sg])
                k.tt("pool", sg[:], sg[:], onb[:], ALU.mult, r=[sg, onb], w=[sg])
                for h in range(4):
                    hs = slice(h * 128, (h + 1) * 128)
                    k.stt(yb[:, hs], osb[:, hs], ss[:, 4 + h:5 + h], sg[:, hs], ALU.mult, ALU.mult, r=[osb, ss, sg], w=[yb])
                if ch == 1:
                    k.dump("h_yb", yb, [T_, 512], BF16); k.dump("h_ss", ss, [T_, 8]); k.dump("h_sg", sg, [T_, 512])
                for j in range(4):
                    k.tr(pT[:, j, :], yb[:, j * 128:(j + 1) * 128], idb[0:T_, 0:T_], r=[yb, idb], w=[pT])
                k.cp("act", ysc[:, :, tok], pT[:, 0:4, :], r=[pT], w=[ysc])
        for j in range(4):
            k.dma(k.dq(), ys_fm[0, j * 128:(j + 1) * 128, :], ysc[:, j, :], r=[ysc], w=[ys_fm])


def mix_rwkv7(k, c, l, rk_tm, ys_fm):
    nl = k.nl
    mu_i = k.inp("rk_mu", [nl, 1792])
    w0_i = k.inp("rk_w0", [nl, 512]); a0_i = k.inp("rk_a0", [nl, 512])
    w2_i = k.inp("rk_w2pad", [nl, 128, 512]); a2_i = k.inp("rk_a2pad", [nl, 128, 512]); g2_i = k.inp("rk_g2", [nl, 128, 512])
    kk_i = k.inp("rk_k_k", [nl, 512]); ka_i = k.inp("rk_k_a", [nl, 512]); rk_i = k.inp("rk_r_k_flat", [nl, 512])
    lnw_i = k.inp("rk_ln_w", [nl, 512]); lnb_i = k.inp("rk_ln_b", [nl, 512])
    qW = k.dram(f"rkq_w", [8, S, 64], F32) if l == 0 else k.rkq["w"]
    if l == 0:
        k.rkq = dict(w=qW, nkk=k.dram("rkq_nkk", [8, S, 64], BF16), ka=k.dram("rkq_ka", [8, S, 64], BF16),
                     k2=k.dram("rkq_k2", [8, S, 64], BF16), r=k.dram("rkq_r", [8, S, 64], BF16),
                     g=k.dram("rkq_g", [S, 512], F32), bonus=k.dram("rkq_bonus", [S, 512], F32))
    Q = k.rkq
    idf = c["idf"]; idb = c["idb"]
    nsteps = k.cfg.get("rk_steps", S)
    with k.phase() as st0:
        vT = k.sb(f"rvT{l}", [128, 4, S], F32, st0)
        with k.phase() as st:
            f = lambda n, sh, dt=F32: k.sb(f"r{n}{l}", sh, dt, st)
            mub = f("mub", [128, 1792]); w0b = f("w0b", [128, 512]); a0b = f("a0b", [128, 512])
            kkb = f("kkb", [128, 512]); kab = f("kab", [128, 512]); rkb = f("rkb", [128, 512])
            w2b = f("w2b", [128, 512], BF16); a2b = f("a2b", [128, 512], BF16); g2b = f("g2b", [128, 512], BF16)
            pc = [f(f"pc{i}", [128, 1792]) for i in range(2)]
            pv = [f(f"pv{i}", [128, 1792]) for i in range(2)]
            lob = f("lob", [128, 256], BF16); loT = f("loT", [128, 2, 128], BF16)
            xw = f("xw", [128, 512]); dec = f("dec", [128, 512]); av = f("av", [128, 512]); gsb = f("gsb", [128, 512])
            kkt = f("kkt", [128, 512]); sq = f("sq", [128, 512]); k2 = f("k2", [128, 512]); bon = f("bon", [128, 512])
            ss = f("ss", [128, 24])
            o_nkk = f("onkk", [128, 512], BF16); o_ka = f("oka", [128, 512], BF16); o_k2 = f("ok2", [128, 512], BF16); o_r = f("or", [128, 512], BF16)
            pT = k.psum(f"rpT{l}", [128, 8, 128], BF16, st)
            pw = k.psum(f"rpw{l}", [128, 512], F32, st); pa = k.psum(f"rpa{l}", [128, 512], F32, st); pg = k.psum(f"rpg{l}", [128, 512], F32, st)
            pvt = k.psum(f"rpvt{l}", [128, 512], F32, st)
            for (t_, src_, n_) in ((mub, mu_i, 1792), (w0b, w0_i, 512), (a0b, a0_i, 512), (kkb, kk_i, 512), (kab, ka_i, 512), (rkb, rk_i, 512)):
                k.dma(k.dq(), t_[:], src_[l].partition_broadcast(128), w=[t_])
            k.dma("pool", w2b[:], w2_i[l], w=[w2b])
            k.dma("pool", a2b[:], a2_i[l], w=[a2b])
            k.dma("pool", g2b[:], g2_i[l], w=[g2b])
            for tt in range(16):
                tok = slice(tt * 128, (tt + 1) * 128)
                p = pc[tt % 2]; pr = pv[tt % 2]
                k.dma("sp", p[:], rk_tm[tok, :], r=[rk_tm], w=[p])
                if tt == 0:
                    k.memset("pool", pr[0:1, :], 0.0, w=[pr])
                    k.dma("act", pr[1:128, :], rk_tm[0:127, :], r=[rk_tm], w=[pr])
                else:
                    k.dma("act", pr[:], rk_tm[tt * 128 - 1:tt * 128 + 127, :], r=[rk_tm], w=[pr])
                k.tt("dve", pr[:], pr[:], p[:], ALU.subtract, r=[pr, p], w=[pr])
                k.tt("pool", pr[:], pr[:], mub[:], ALU.mult, r=[pr, mub], w=[pr])
                k.tt("dve", p[:], p[:], pr[:], ALU.add, r=[p, pr], w=[p])
                r_ = p[:, 0:512]; k_ = p[:, 512:1024]; v_ = p[:, 1024:1536]
                k.act(lob[:, 0:64], p[:, 1536:1600], AF.Tanh, r=[p], w=[lob])
                k.cp("act", lob[:, 64:128], p[:, 1600:1664], r=[p], w=[lob])
                k.act(lob[:, 128:256], p[:, 1664:1792], AF.Sigmoid, r=[p], w=[lob])
                k.tr(pT[:, 0, :], lob[:, 0:128], idb[:], r=[lob, idb], w=[pT])
                k.tr(pT[:, 1, :], lob[:, 128:256], idb[:], r=[lob, idb], w=[pT])
                k.cp("act", loT[:], pT[:, 0:2, :], r=[pT], w=[loT])
                k.mm(pw[:], loT[:, 0, :], w2b[:], True, True, r=[loT, w2b], w=[pw])
                k.mm(pa[:], loT[:, 0, :], a2b[:], True, True, r=[loT, a2b], w=[pa])
                k.mm(pg[:], loT[:, 1, :], g2b[:], True, True, r=[loT, g2b], w=[pg])
                k.tt("dve", xw[:], pw[:], w0b[:], ALU.add, r=[pw, w0b], w=[xw])
                k.act(xw[:], xw[:], AF.Exp, r=[xw], w=[xw], scale=-1.0)
                k.act(xw[:], xw[:], AF.Ln, r=[xw], w=[xw], bias=1.0)
                k.act(xw[:], xw[:], AF.Exp, r=[xw], w=[xw], scale=-1.0, bias=-0.5)
                k.act(dec[:], xw[:], AF.Exp, r=[xw], w=[dec], scale=-1.0)
                k.tt("dve", av[:], pa[:], a0b[:], ALU.add, r=[pa, a0b], w=[av])
                k.act(av[:], av[:], AF.Sigmoid, r=[av], w=[av])
                k.cp("act", gsb[:], pg[:], r=[pg], w=[gsb])
                k.tt("pool", kkt[:], k_, kkb[:], ALU.mult, r=[p, kkb], w=[kkt])
                k.tt("pool", sq[:], kkt[:], kkt[:], ALU.mult, r=[kkt], w=[sq])
                k.red("dve", ss[:, 0:8], sq[:].rearrange("p (h d) -> p h d", h=8), ALU.add, r=[sq], w=[ss])
                k.act(ss[:, 0:8], ss[:, 0:8], AF.Sqrt, r=[ss], w=[ss])
                k.ts("dve", ss[:, 0:8], ss[:, 0:8], 1e-12, None, ALU.max, r=[ss], w=[ss])
                k.recip(ss[:, 8:16], ss[:, 0:8], r=[ss], w=[ss])
                for h in range(8):
                    hs = slice(h * 64, (h + 1) * 64)
                    k.ts("dve", kkt[:, hs], kkt[:, hs], ss[:, 8 + h:9 + h], None, ALU.mult, r=[kkt, ss], w=[kkt])
                k.stt(k2[:], av[:], -1.0, kab[:], ALU.add, ALU.mult, r=[av, kab], w=[k2])
                k.stt(k2[:], k2[:], 1.0, k_, ALU.add, ALU.mult, r=[k2, p], w=[k2])
                k.ts("dve", o_nkk[:], kkt[:], -1.0, None, ALU.mult, r=[kkt], w=[o_nkk])
                k.tt("pool", o_ka[:], kkt[:], av[:], ALU.mult, r=[kkt, av], w=[o_ka])
                k.cp("pool", o_k2[:], k2[:], r=[k2], w=[o_k2])
                k.cp("act", o_r[:], r_, r=[p], w=[o_r])
                k.tt("pool", sq[:], r_, k2[:], ALU.mult, r=[p, k2], w=[sq])
                k.tt("pool", sq[:], sq[:], rkb[:], ALU.mult, r=[sq, rkb], w=[sq])
                k.red("dve", ss[:, 16:24], sq[:].rearrange("p (h d) -> p h d", h=8), ALU.add, r=[sq], w=[ss])
                for h in range(8):
                    hs = slice(h * 64, (h + 1) * 64)
                    k.ts("dve", bon[:, hs], p[:, 1024 + h * 64:1024 + (h + 1) * 64], ss[:, 16 + h:17 + h], None, ALU.mult, r=[p, ss], w=[bon])
                for j in range(4):
                    k.tr(pvt[:, j * 128:(j + 1) * 128], p[:, 1024 + j * 128:1024 + (j + 1) * 128], idf[:], r=[p, idf], w=[pvt])
                k.cp("act", vT[:, :, tok], pvt[:].rearrange("p (j t) -> p j t", j=4), r=[pvt], w=[vT])
                hm = lambda q: q.t.rearrange("h t k -> t h k")[tok]
                k.dma("sp", hm(Q["w"]), dec[:].rearrange("p (h d) -> p h d", h=8), r=[dec], w=[Q["w"]])
                k.dma("act", hm(Q["nkk"]), o_nkk[:].rearrange("p (h d) -> p h d", h=8), r=[o_nkk], w=[Q["nkk"]])
                k.dma("sp", hm(Q["ka"]), o_ka[:].rearrange("p (h d) -> p h d", h=8), r=[o_ka], w=[Q["ka"]])
                k.dma("act", hm(Q["k2"]), o_k2[:].rearrange("p (h d) -> p h d", h=8), r=[o_k2], w=[Q["k2"]])
                k.dma("sp", hm(Q["r"]), o_r[:].rearrange("p (h d) -> p h d", h=8), r=[o_r], w=[Q["r"]])
                k.dma("act", Q["g"][tok, :], gsb[:], r=[gsb], w=[Q["g"]])
                k.dma("sp", Q["bonus"][tok, :], bon[:], r=[bon], w=[Q["bonus"]])
        with k.phase() as st1:
            yT = k.sb(f"ryT{l}", [128, 4, S], F32, st1)
            with k.phase() as st:
                TS = 8
                Wb = [k.sb(f"rWb{l}{i}", [128, 4, TS, 64], F32, st) for i in range(2)]
                Nb = [k.sb(f"rNb{l}{i}", [128, 4, TS, 64], BF16, st) for i in range(2)]
                Ab = [k.sb(f"rAb{l}{i}", [128, 4, TS, 64], BF16, st) for i in range(2)]
                Kb = [k.sb(f"rKb{l}{i}", [128, 4, TS, 64], BF16, st) for i in range(2)]
                Rb = [k.sb(f"rRb{l}{i}", [128, 4, TS, 64], BF16, st) for i in range(2)]
                KV = [k.sb(f"rKV{l}{i}", [128, 4, TS, 64], F32, st) for i in range(2)]
                St = k.sb(f"rSt{l}", [128, 4, 64], F32, st)
                tmpA = k.sb(f"rtmpA{l}", [128, 4, 64], F32, st)
                tmpB = k.sb(f"rtmpB{l}", [128, 4, 64], F32, st)
                tmpC = k.sb(f"rtmpC{l}", [128, 4, 64], F32, st)
                pend = None
                sa = k.sb(f"rsa{l}", [128, 4], F32, st)
                k.memset("dve", St[:], 0.0, w=[St])
                if nsteps < S:
                    k.memset("pool", yT[:], 0.0, w=[yT])
                for cch in range(nsteps // TS):
                    t0 = cch * TS
                    b = cch % 2
                    for (buf, qn) in ((Wb, "w"), (Nb, "nkk"), (Ab, "ka"), (Kb, "k2"), (Rb, "r")):
                        if k.cfg.get("rk_nodma") and cch >= 2:
                            continue
                        for hp in range(2):
                            srcap = Q[qn].t.rearrange("(j hp) t k -> hp j t k", hp=2)[hp, :, t0:t0 + TS, :]
                            k.dma(k.dq(), buf[b][hp * 64:(hp + 1) * 64], srcap.partition_broadcast(64), r=[Q[qn]], w=[buf[b]])
                    k.tt("pool", KV[b][:], Kb[b][:], vT[:, :, t0:t0 + TS].unsqueeze(3).to_broadcast([128, 4, TS, 64]), ALU.mult,
                         r=[Kb[b], vT], w=[KV[b]])
                    for ti in range(TS):
                        t = t0 + ti
                        N_ = Nb[b]; W_ = Wb[b]; A_ = Ab[b]; KV_ = KV[b]; R_ = Rb[b]
                        RX = dict(relaxed=True)
                        opa = lambda N_=N_, ti=ti: k.P.op("dve", lambda e: e.tensor_tensor(out=tmpA[:], in0=St[:], in1=N_[:, :, ti, :], op=ALU.mult),
                                                           reads=[St.b, N_.b], writes=[tmpA.b], **RX)
                        opb = lambda W_=W_, ti=ti: k.P.op("dve", lambda e: e.tensor_tensor(out=St[:], in0=St[:], in1=W_[:, :, ti, :], op=ALU.mult),
                                                           reads=[St.b, W_.b], writes=[St.b], **RX)
                        opc = lambda: k.P.op("dve", lambda e: e.tensor_reduce(out=sa[:], in_=tmpA[:], axis=AX.X, op=ALU.add),
                                             reads=[tmpA.b], writes=[sa.b], **RX)
                        opd = lambda KV_=KV_, ti=ti: k.P.op("dve", lambda e: e.tensor_tensor(out=St[:], in0=St[:], in1=KV_[:, :, ti, :], op=ALU.add),
                                                             reads=[St.b, KV_.b], writes=[St.b], **RX)
                        ope = lambda A_=A_, ti=ti: k.P.op("dve", lambda e: e.tensor_tensor(out=tmpB[:], in0=A_[:, :, ti, :],
                                                                                          in1=sa[:].unsqueeze(2).to_broadcast([128, 4, 64]), op=ALU.mult),
                                                           reads=[A_.b, sa.b], writes=[tmpB.b], **RX)
                        opf = lambda: k.P.op("dve", lambda e: e.tensor_tensor(out=St[:], in0=St[:], in1=tmpB[:], op=ALU.add),
                                             reads=[St.b, tmpB.b], writes=[St.b], **RX)
                        opg = lambda R_=R_, ti=ti: k.P.op("dve", lambda e: e.tensor_tensor(out=tmpC[:], in0=St[:], in1=R_[:, :, ti, :], op=ALU.mult),
                                                           reads=[St.b, R_.b], writes=[tmpC.b], **RX)
                        oph = lambda t=t: k.P.op("dve", lambda e: e.tensor_reduce(out=yT[:, :, t], in_=tmpC[:], axis=AX.X, op=ALU.add),
                                                 reads=[tmpC.b], writes=[yT.b], **RX)
                        opa()
                        if pend is not None:
                            pend[0]()
                        opb(); opc(); opd(); ope()
                        if pend is not None:
                            pend[1]()
                        opf()
                        pend = (opg, oph)
                if pend is not None:
                    pend[0]()
                    pend[1]()
            with k.phase() as st:
                f = lambda n, sh, dt=F32: k.sb(f"q{n}{l}", sh, dt, st)
                lnw = f("lnw", [128, 512]); lnb = f("lnb", [128, 512])
                ysb = f("ysb", [128, 512]); sq = f("sq", [128, 512]); ss = f("ss", [128, 24]); gt = f("gt", [128, 512]); bt = f("bt", [128, 512])
                ynb = f("ynb", [128, 512], BF16)
                ysc = f("ysc", [128, 4, S], BF16)
                py = k.psum(f"qpy{l}", [128, 512], F32, st)
                pT = k.psum(f"qpT{l}", [128, 8, 128], BF16, st)
                k.dma("sp", lnw[:], lnw_i[l].partition_broadcast(128), w=[lnw])
                k.dma("act", lnb[:], lnb_i[l].partition_broadcast(128), w=[lnb])
                for tt in range(16):
                    tok = slice(tt * 128, (tt + 1) * 128)
                    for j in range(4):
                        k.tr(py[:, j * 128:(j + 1) * 128], yT[:, j, tok], idf[:], r=[yT, idf], w=[py])
                    k.cp("act", ysb[:], py[:], r=[py], w=[ysb])
                    k.dma("sp", gt[:], Q["g"][tok, :], r=[Q["g"]], w=[gt])
                    k.dma("act", bt[:], Q["bonus"][tok, :], r=[Q["bonus"]], w=[bt])
                    k.red("dve", ss[:, 0:8], ysb[:].rearrange("p (h d) -> p h d", h=8), ALU.add, r=[ysb], w=[ss])
                    k.ts("dve", ss[:, 0:8], ss[:, 0:8], -1.0 / 64, None, ALU.mult, r=[ss], w=[ss])
                    for h in range(8):
                        hs = slice(h * 64, (h + 1) * 64)
                        k.ts("dve", ysb[:, hs], ysb[:, hs], ss[:, h:h + 1], None, ALU.add, r=[ysb, ss], w=[ysb])
                    k.tt("pool", sq[:], ysb[:], ysb[:], ALU.mult, r=[ysb], w=[sq])
                    k.red("dve", ss[:, 8:16], sq[:].rearrange("p (h d) -> p h d", h=8), ALU.add, r=[sq], w=[ss])
                    k.ts("dve", ss[:, 8:16], ss[:, 8:16], 1.0 / 64, 64e-5, ALU.mult, ALU.add, r=[ss], w=[ss])
                    k.act(ss[:, 8:16], ss[:, 8:16], AF.Sqrt, r=[ss], w=[ss])
                    k.recip(ss[:, 16:24], ss[:, 8:16], r=[ss], w=[ss])
                    for h in range(8):
                        hs = slice(h * 64, (h + 1) * 64)
                        k.stt(ysb[:, hs], ysb[:, hs], ss[:, 16 + h:17 + h], lnw[:, hs], ALU.mult, ALU.mult, r=[ysb, ss, lnw], w=[ysb])
                    k.tt("pool", bt[:], bt[:], lnb[:], ALU.add, r=[bt, lnb], w=[bt])
                    k.tt("dve", ysb[:], ysb[:], bt[:], ALU.add, r=[ysb, bt], w=[ysb])
                    k.tt("dve", ynb[:], ysb[:], gt[:], ALU.mult, r=[ysb, gt], w=[ynb])
                    for j in range(4):
                        k.tr(pT[:, j, :], ynb[:, j * 128:(j + 1) * 128], idb[:], r=[ynb, idb], w=[pT])
                    k.cp("act", ysc[:, :, tok], pT[:, 0:4, :], r=[pT], w=[ysc])
                for j in range(4):
                    k.dma(k.dq(), ys_fm[3, j * 128:(j + 1) * 128, :], ysc[:, j, :], r=[ysc], w=[ys_fm])


def mixers(k, c, l, src, ys_fm):
    which = k.cfg.get("mixers", "abcd")
    if "a" in which:
        mix_hgrn2(k, c, l, src["hg_tm"], ys_fm)
    if "b" in which:
        mix_s5(k, c, l, src["s5_fm"], ys_fm)
    if "d" in which:
        mix_rwkv7(k, c, l, src["rk_tm"], ys_fm)
    if "c" in which:
        mix_ssd(k, c, l, src["m2z_tm"], src["m2x_fm"], src["m2dt_tm"], ys_fm)


def col_layout(v, ncol):
    sh = v.shape[:-1]
    return np.ascontiguousarray(v.reshape(*sh, ncol, 128).swapaxes(-1, -2))


def host_inputs(inp, b):
    m = {}
    m["x"] = np.ascontiguousarray(inp["x"][b])
    m["c_col"] = col_layout(inp["c"][b], 16)
    if "w_mod" in inp:
        m["w_mod"] = inp["w_mod"]
        m["b_mod_col"] = col_layout(inp["b_mod"], 96)
        m["g_mix_col"] = col_layout(inp["g_norm_mix"], 16)
        m["g_ffn_col"] = col_layout(inp["g_norm_ffn"], 16)
    if "w_in" in inp:
        m["w_in"] = inp["w_in"]
    for n in ("w_branch", "w_out", "w_router", "b_router", "w_gu", "w_down", "b_down", "g_final"):
        if n in inp:
            m[n] = inp[n]
    if "b_gu" in inp:
        bg = inp["b_gu"]
        m["b_gu_col"] = np.ascontiguousarray(bg.reshape(bg.shape[0], NE, 12, 128).transpose(0, 3, 1, 2).reshape(bg.shape[0], 128, NE * 12))
    if "ys_fm" in inp:
        m["ys_fm"] = inp["ys_fm"]
    if "rk_mu" in inp:
        nl_ = inp["rk_mu"].shape[0]
        for n in ("rk_mu", "rk_w0", "rk_a0", "rk_g2", "rk_k_k", "rk_k_a", "rk_ln_w", "rk_ln_b"):
            m[n] = inp[n]
        m["rk_r_k_flat"] = np.ascontiguousarray(inp["rk_r_k"].reshape(nl_, 512))
        w2p = np.zeros((nl_, 128, 512), np.float32); w2p[:, 0:64] = inp["rk_w2"]
        a2p = np.zeros((nl_, 128, 512), np.float32); a2p[:, 64:128] = inp["rk_a2"]
        m["rk_w2pad"] = w2p; m["rk_a2pad"] = a2p
    if "hg_lower_bound" in inp:
        m["hg_lower_bound"] = inp["hg_lower_bound"]
        m["hg_onorm"] = inp["hg_onorm"]
    if "m2_conv_w" in inp:
        cw = inp["m2_conv_w"]
        nl_ = cw.shape[0]
        m["m2_convw_col"] = np.ascontiguousarray(cw.reshape(nl_, 4, 8, 128).transpose(0, 3, 2, 1))
        m["m2_convb_col"] = col_layout(inp["m2_conv_b"], 8)
        for n in ("m2_dt_bias", "m2_a_log", "m2_d", "m2_norm"):
            m[n] = inp[n]
    if "s5_lambda_re" in inp:
        nl = inp["s5_lambda_re"].shape[0]
        def stcol(a):
            return np.ascontiguousarray(a.reshape(nl, 16, 2, 64).transpose(0, 2, 3, 1).reshape(nl, 128, 16))
        m["s5_lr_col"] = stcol(inp["s5_lambda_re"])
        m["s5_li_col"] = stcol(inp["s5_lambda_im"])
        m["s5_ldt_col"] = stcol(np.broadcast_to(inp["s5_log_dt"][:, :, None], (nl, 32, 64)))
        def bT(b):
            o = np.zeros((nl, 16, 128, 128), np.float32)
            for g in range(32):
                st_, g2 = g // 2, g % 2
                gl = g % 8
                o[:, st_, gl * 16:(gl + 1) * 16, g2 * 64:(g2 + 1) * 64] = b[:, g].transpose(0, 2, 1)
            return o
        def cT(cc):
            o = np.zeros((nl, 16, 128, 128), np.float32)
            for g in range(32):
                st_, g2 = g // 2, g % 2
                gl = g % 8
                o[:, st_, g2 * 64:(g2 + 1) * 64, gl * 16:(gl + 1) * 16] = cc[:, g].transpose(0, 2, 1)
            return o
        m["s5_bT_re"] = bT(inp["s5_b_re"]); m["s5_bT_im"] = bT(inp["s5_b_im"])
        m["s5_cT_re"] = cT(inp["s5_c_re"]); m["s5_cT_im"] = cT(inp["s5_c_im"])
        m["s5_d_col"] = col_layout(inp["s5_d"], 4)
        m["s5_w_glu"] = inp["s5_w_glu"]
        m["s5_bglu_col"] = col_layout(inp["s5_b_glu"], 4)
    return m


def run(inputs, cfg, cores):
    nc = bass.Bass("TRN2", target_bir_lowering=False)
    with contextlib.ExitStack() as stack:
        k = build(nc, stack, cfg)
    maps = []
    for ci in range(cores):
        full = host_inputs(inputs, ci % 4)
        nl = cfg.get("nl", L)
        maps.append({n: np.ascontiguousarray(full[n]) for n in k.inputs})
    res = run_bass_kernel_spmd(nc, maps, core_ids=list(range(cores)))
    return res, k


def kernel(**inputs):
    inputs = {n: np.asarray(v) for n, v in inputs.items()}
    res, k = run(inputs, {}, 4)
    out = np.stack([res.results[b]["y"] for b in range(4)], axis=0)
    return out.astype(np.float32)
```

```python
import contextlib
import numpy as np
import concourse.bass as bass
import concourse.mybir as mybir
from concourse.bass_utils import run_bass_kernel_spmd

F32 = mybir.dt.float32
BF16 = mybir.dt.bfloat16
U32 = mybir.dt.uint32
I32 = mybir.dt.int32
ALU = mybir.AluOpType
AF = mybir.ActivationFunctionType
AX = mybir.AxisListType

D = 2048
S = 2048
L = 4
NB = 4
BR = 512
N_IN = 14088
O1 = 8192
O_HG = 8192
O_S5 = 10240
O_M2 = 10752
O_RK = 12296
NE = 32
DE = 768
EPS = 1e-6


class Buf:
    __slots__ = ("w", "r", "name")

    def __init__(self, name=""):
        self.w = None
        self.r = {}
        self.name = name


class T:
    def __init__(self, t, name=""):
        self.t = t
        self.b = Buf(name)

    def __getitem__(self, k):
        return self.t[k]


def _bufs(lst):
    out = []
    for x in lst:
        if x is None:
            continue
        out.append(x.b if isinstance(x, T) else x)
    return out


class Prog:
    CE = ("pe", "act", "dve", "pool")
    ENG = ("pe", "act", "dve", "pool", "sp")

    def __init__(self, nc, stack, ndma=20):
        self.nc = nc
        self.stack = stack
        self.q = {e: [] for e in self.ENG}
        self.cnt = {e: 0 for e in self.CE}
        self.sems = []
        self.own = {}
        for e in self.CE:
            self.own[e] = self._newsem("own_" + e)
        self.known = {e: {} for e in self.ENG}
        self.dpool = {}
        self.drr = {}
        for qn in ("sp", "act", "pool"):
            self.dpool[qn] = [[self._newsem(f"d_{qn}{i}"), 0] for i in range(ndma)]
            self.drr[qn] = 0
        self.n_ops = 0

    def _newsem(self, name):
        s = self.stack.enter_context(self.nc.semaphore(name))
        self.sems.append(s)
        return len(self.sems) - 1

    def _deps(self, eng, reads, writes, relaxed=False):
        deps = {}
        own = self.own.get(eng, -1)

        def add(s, v):
            if deps.get(s, 0) < v:
                deps[s] = v
        for b in reads:
            if b.w is not None:
                add(*b.w)
        for b in writes:
            if b.w is not None:
                add(*b.w)
            for s, v in b.r.items():
                if relaxed and s == own:
                    continue
                add(s, v)
        waits = []
        kn = self.known[eng]
        for s, v in deps.items():
            if eng in self.own and s == self.own[eng]:
                if eng == "pe":
                    continue
                if relaxed and v < self.cnt[eng]:
                    continue
                if v < self.cnt[eng] - 1:
                    continue
            if kn.get(s, 0) >= v:
                continue
            kn[s] = v
            waits.append((s, v))
        return waits

    def _mark(self, ev, reads, writes):
        s, v = ev
        for b in reads:
            if b.r.get(s, 0) < v:
                b.r[s] = v
        for b in writes:
            b.w = ev
            b.r = {}

    def op(self, eng, fn, reads=(), writes=(), relaxed=False):
        reads = _bufs(reads)
        writes = _bufs(writes)
        waits = self._deps(eng, reads, writes, relaxed)
        self.cnt[eng] += 1
        ev = (self.own[eng], self.cnt[eng])
        self.q[eng].append((waits, fn, ev[0], 1))
        self._mark(ev, reads, writes)
        self.n_ops += 1
        return ev

    def _dmaev(self, qn, waits):
        pool = self.dpool[qn]
        i = self.drr[qn]
        self.drr[qn] = (i + 1) % len(pool)
        s, tot = pool[i]
        kn = self.known[qn]
        if kn.get(s, 0) < tot:
            kn[s] = tot
            waits.append((s, tot))
        pool[i][1] = tot + 16
        return (s, tot + 16)

    def dma(self, qn, out, in_, reads=(), writes=(), **kw):
        reads = _bufs(reads)
        writes = _bufs(writes)
        waits = self._deps(qn, reads, writes)
        ev = self._dmaev(qn, waits)

        def fn(e, out=out, in_=in_, kw=kw):
            return e.dma_start(out=out, in_=in_, **kw)
        self.q[qn].append((waits, fn, ev[0], 16))
        self._mark(ev, reads, writes)
        self.n_ops += 1
        return ev

    def custom(self, qn, fn, reads=(), writes=()):
        reads = _bufs(reads)
        writes = _bufs(writes)
        waits = self._deps(qn, reads, writes)
        ev = self._dmaev(qn, waits)
        self.q[qn].append((waits, fn, ev[0], 16))
        self._mark(ev, reads, writes)
        return ev

    def barrier(self):
        tot = []
        for qn, pool in self.dpool.items():
            for s, t in pool:
                if t > 0:
                    tot.append((s, t))
        for e in self.ENG:
            waits = []
            kn = self.known[e]
            for e2 in self.CE:
                if e2 != e and self.cnt[e2] > 0:
                    s, v = self.own[e2], self.cnt[e2]
                    if kn.get(s, 0) < v:
                        kn[s] = v
                        waits.append((s, v))
            for s, v in tot:
                if kn.get(s, 0) < v:
                    kn[s] = v
                    waits.append((s, v))
            if waits:
                self.q[e].append((waits, None, None, 0))

    def finish(self):
        waits = []
        for qn, pool in self.dpool.items():
            for s, tot in pool:
                if tot > 0:
                    waits.append((s, tot))
        self.q["sp"].append((waits, None, None, 0))
        w2 = [(self.own[e], self.cnt[e]) for e in self.CE if self.cnt[e] > 0]
        self.q["sp"].append((w2, None, None, 0))

    def emit(self):
        nc = self.nc
        sems = self.sems
        q = self.q
        with nc.Block() as block:
            def run(e, lst):
                for waits, fn, s, inc in lst:
                    for ws, wv in waits:
                        e.wait_ge(sems[ws], wv)
                    if fn is None:
                        continue
                    ins = fn(e)
                    ins.then_inc(sems[s], inc)

            @block.tensor
            def _(e):
                run(e, q["pe"])

            @block.scalar
            def _(e):
                run(e, q["act"])

            @block.vector
            def _(e):
                run(e, q["dve"])

            @block.gpsimd
            def _(e):
                run(e, q["pool"])

            @block.sync
            def _(e):
                run(e, q["sp"])


class KB:
    def __init__(self, nc, stack, cfg):
        self.nc = nc
        self.st = stack
        self.cfg = cfg
        self.P = Prog(nc, stack)
        self.inputs = {}
        self.outs = {}
        self.rr = 0
        self.dbg = cfg.get("dbg", ())

    def inp(self, name, shape, dtype=F32):
        if name not in self.inputs:
            self.inputs[name] = T(self.nc.dram_tensor(name, list(shape), dtype, kind="ExternalInput").ap(), name)
        return self.inputs[name]

    def dram(self, name, shape, dtype=F32):
        if name in self.cfg.get("scratch_in", ()):
            return self.inp(name, shape, dtype)
        kind = "ExternalOutput" if name in self.dbg else "Internal"
        t = T(self.nc.dram_tensor(name, list(shape), dtype, kind=kind).ap(), name)
        if name in self.dbg:
            self.outs[name] = t
        return t

    def sb(self, name, shape, dtype=F32, stack=None):
        st = stack or self.st
        return T(st.enter_context(self.nc.sbuf_tensor(name, list(shape), dtype)), name)

    def psum(self, name, shape, dtype=F32, stack=None):
        st = stack or self.st
        return T(st.enter_context(self.nc.psum_tensor(name, list(shape), dtype)), name)

    @contextlib.contextmanager
    def phase(self):
        with contextlib.ExitStack() as st:
            yield st
        self.P.barrier()

    def dump(self, name, tile, shape, dtype=F32, ap=None):
        if name not in self.dbg:
            return
        if name not in self.outs:
            self.outs[name] = T(self.nc.dram_tensor(name, list(shape), dtype, kind="ExternalOutput").ap(), name)
        d = self.outs[name]
        self.dma("sp", d[:], tile[:] if ap is None else ap, r=[tile], w=[d])

    def dq(self):
        self.rr += 1
        return ("sp", "act")[self.rr % 2]

    def mm(self, out, lhsT, rhs, start, stop, r=(), w=()):
        return self.P.op("pe", lambda e: e.matmul(out, lhsT=lhsT, rhs=rhs, start=start, stop=stop), reads=r, writes=w)

    def tr(self, out, in_, ident, r=(), w=()):
        return self.P.op("pe", lambda e: e.transpose(out=out, in_=in_, identity=ident), reads=r, writes=w)

    def act(self, out, in_, func, r=(), w=(), bias=None, scale=None, accum=None, eng="act"):
        kw = {}
        if bias is not None:
            kw["bias"] = bias
        if scale is not None:
            kw["scale"] = scale
        if accum is not None:
            kw["accum_out"] = accum
        return self.P.op("act", lambda e: e.activation(out=out, in_=in_, func=func, **kw), reads=r, writes=w)

    def ts(self, eng, out, in0, s1, s2, op0, op1=None, r=(), w=(), accum=None):
        kw = {}
        if accum is not None:
            kw["accum_out"] = accum
        if op1 is None:
            return self.P.op(eng, lambda e: e.tensor_scalar(out=out, in0=in0, scalar1=s1, scalar2=None, op0=op0, **kw), reads=r, writes=w)
        return self.P.op(eng, lambda e: e.tensor_scalar(out=out, in0=in0, scalar1=s1, scalar2=s2, op0=op0, op1=op1, **kw), reads=r, writes=w)

    def tt(self, eng, out, in0, in1, op, r=(), w=()):
        return self.P.op(eng, lambda e: e.tensor_tensor(out=out, in0=in0, in1=in1, op=op), reads=r, writes=w)

    def stt(self, out, in0, scalar, in1, op0, op1, r=(), w=(), accum=None):
        kw = {}
        if accum is not None:
            kw["accum_out"] = accum
        return self.P.op("dve", lambda e: e.scalar_tensor_tensor(out=out, in0=in0, scalar=scalar, in1=in1, op0=op0, op1=op1, **kw), reads=r, writes=w)

    def cp(self, eng, out, in_, r=(), w=()):
        if eng == "act":
            return self.P.op("act", lambda e: e.copy(out=out, in_=in_), reads=r, writes=w)
        return self.P.op(eng, lambda e: e.tensor_copy(out=out, in_=in_), reads=r, writes=w)

    def memset(self, eng, ap, val, w=()):
        return self.P.op(eng, lambda e: e.memset(ap, val), writes=w)

    def red(self, eng, out, in_, op, r=(), w=(), axis=AX.X):
        return self.P.op(eng, lambda e: e.tensor_reduce(out=out, in_=in_, axis=axis, op=op), reads=r, writes=w)

    def recip(self, out, in_, r=(), w=()):
        return self.P.op("dve", lambda e: e.reciprocal(out=out, in_=in_), reads=r, writes=w)

    def dma(self, q, out, in_, r=(), w=(), **kw):
        return self.P.dma(q, out, in_, reads=r, writes=w, **kw)


def build_consts(k):
    c = {}
    idf = k.sb("idf", [128, 128], F32)
    k.memset("pool", idf[:], 1.0, w=[idf])
    k.P.op("pool", lambda e: e.affine_select(out=idf[:], in_=idf[:], pattern=[[-1, 128]], compare_op=ALU.is_equal,
                                              fill=0.0, base=0, channel_multiplier=1), reads=[idf], writes=[idf])
    idb = k.sb("idb", [128, 128], BF16)
    k.cp("dve", idb[:], idf[:], r=[idf], w=[idb])
    c["idf"] = idf
    c["idb"] = idb
    triu = k.sb("triu", [128, 128], F32)
    k.memset("pool", triu[:], 1.0, w=[triu])
    k.P.op("pool", lambda e: e.affine_select(out=triu[:], in_=triu[:], pattern=[[1, 128]], compare_op=ALU.is_ge,
                                              fill=0.0, base=0, channel_multiplier=-1), reads=[triu], writes=[triu])
    c["triu"] = triu
    ones = k.sb("onesf", [128, 128], F32)
    k.memset("pool", ones[:], 1.0, w=[ones])
    c["ones"] = ones
    onesb = k.sb("onesb", [128, 128], BF16)
    k.memset("pool", onesb[:], 1.0, w=[onesb])
    c["onesb"] = onesb
    return c


def stage_mod(k, c, nl):
    nc = k.nc
    cin = k.inp("c_col", [128, 16])
    w_mod = k.inp("w_mod", [nl, D, 6 * D])
    b_mod = k.inp("b_mod_col", [nl, 128, 96])
    gmix = k.inp("g_mix_col", [nl, 128, 16])
    gffn = k.inp("g_ffn_col", [nl, 128, 16])
    condc = k.sb("condc", [128, 16], F32)
    k.dma("sp", condc[:], cin[:], w=[condc])
    k.act(condc[:], condc[:], AF.Silu, r=[condc], w=[condc])
    modc = k.sb("modc", [128, L, 96], F32)
    gsa = k.sb("gsa", [128, L, 16], F32)
    gsm = k.sb("gsm", [128, L, 16], F32)
    gt_scr = k.dram("gt_scr", [L, 2, 16, 128])
    with k.phase() as st:
        wb = [k.sb(f"wmodbuf{i}", [128, 16, 768], F32, st) for i in range(2)]
        pm = k.psum("ps_mod", [128, 512], F32, st)
        pt = k.psum("ps_modT", [128, 512], F32, st)
        bt = k.sb("bmodt", [128, 96], F32, st)
        gtmp = k.sb("gtmp", [128, 16], F32, st)
        rowt = k.sb("rowt", [16, 128], F32, st)
        it = 0
        for l in range(nl):
            for cb in range(16):
                w = wb[it % 2]
                it += 1
                k.dma(k.dq(), w[:], w_mod[l, :, cb * 768:(cb + 1) * 768].rearrange("(kc p) n -> p kc n", p=128), w=[w])
                for m in range(6):
                    col = cb * 6 + m
                    for kc in range(16):
                        k.mm(pm[:, col:col + 1], w[:, kc, m * 128:(m + 1) * 128], condc[:, kc:kc + 1],
                             kc == 0, kc == 15, r=[w, condc], w=[pm])
            k.dma("sp", bt[:], b_mod[l], w=[bt])
            k.tt("dve", modc[:, l, :], pm[:, 0:96], bt[:], ALU.add, r=[pm, bt], w=[modc])
            for (dst, gsrc, off) in ((gsa, gmix, 16), (gsm, gffn, 64)):
                k.dma("sp", gtmp[:], gsrc[l], w=[gtmp])
                k.stt(dst[:, l, :], modc[:, l, off:off + 16], 1.0, gtmp[:], ALU.add, ALU.mult, r=[modc, gtmp], w=[dst])
            for gi, off in enumerate((32, 80)):
                k.tr(pt[0:16, 0:128], modc[:, l, off:off + 16], c["idf"][:], r=[modc, c["idf"]], w=[pt])
                k.cp("dve", rowt[:], pt[0:16, 0:128], r=[pt], w=[rowt])
                k.dma("sp", gt_scr[l, gi], rowt[:], r=[rowt], w=[gt_scr])
    return dict(modc=modc, gsa=gsa, gsm=gsm, gt_scr=gt_scr)


def stage_norm(k, c, xres, hT, gsT, l, modc, shoff, tag):
    with k.phase() as st:
        xt = [k.sb(f"nx{tag}{i}", [128, D], F32, st) for i in range(2)]
        xn = [k.sb(f"nxn{tag}{i}", [128, D], BF16, st) for i in range(2)]
        sq = k.sb(f"nsq{tag}", [128, D], BF16, st)
        ss = [k.sb(f"nss{tag}{i}", [128, 4], F32, st) for i in range(2)]
        pt = [k.psum(f"npt{tag}{i}", [128, 4, 128], BF16, st) for i in range(2)]
        for tt in range(S // 128):
            x = xt[tt % 2]
            n = xn[tt % 2]
            s = ss[tt % 2]
            k.dma(k.dq(), x[:], xres[tt * 128:(tt + 1) * 128, :], r=[xres], w=[x])
            k.act(sq[:], x[:], AF.Square, r=[x], w=[sq, s], accum=s[:, 0:1])
            k.ts("dve", s[:, 1:2], s[:, 0:1], 1.0 / D, EPS, ALU.mult, ALU.add, r=[s], w=[s])
            k.act(s[:, 2:3], s[:, 1:2], AF.Sqrt, r=[s], w=[s])
            k.recip(s[:, 3:4], s[:, 2:3], r=[s], w=[s])
            k.act(n[:], x[:], AF.Copy, r=[x, s], w=[n], scale=s[:, 3:4])
            for j4 in range(4):
                p = pt[j4 % 2]
                for jj in range(4):
                    j = j4 * 4 + jj
                    k.tr(p[:, jj, :], n[:, j * 128:(j + 1) * 128], c["idb"][:], r=[n, c["idb"]], w=[p])
                for jj in range(4):
                    j = j4 * 4 + jj
                    eng = "dve" if jj % 2 == 0 else "pool"
                    if eng == "pool":
                        k.act(hT[:, j, tt * 128:(tt + 1) * 128], p[:, jj, :], AF.Identity, r=[p, gsT, modc], w=[hT],
                              scale=gsT[:, l, j:j + 1], bias=modc[:, l, shoff + j:shoff + j + 1])
                    else:
                        k.ts("dve", hT[:, j, tt * 128:(tt + 1) * 128], p[:, jj, :], gsT[:, l, j:j + 1],
                             modc[:, l, shoff + j:shoff + j + 1], ALU.mult, ALU.add, r=[p, gsT, modc], w=[hT])


def proj_tm(k, hT, wsrc, c0, ncols, dst, st, tag):
    wb = [k.sb(f"pw{tag}{i}", [128, 16, 512], BF16, st) for i in range(2)]
    ob = [k.sb(f"po{tag}{i}", [128, 512], F32, st) for i in range(3)]
    ps = [k.psum(f"pp{tag}{i}", [128, 512], F32, st) for i in range(2)]
    it = 0
    io = 0
    for b0 in range(0, ncols, 512):
        nb = min(512, ncols - b0)
        w = wb[it % 2]
        it += 1
        k.dma("pool", w[:, :, 0:nb], wsrc[:, c0 + b0:c0 + b0 + nb].rearrange("(kc p) n -> p kc n", p=128), w=[w])
        for tt in range(S // 128):
            p = ps[tt % 2]
            for kc in range(16):
                k.mm(p[:, 0:nb], hT[:, kc, tt * 128:(tt + 1) * 128], w[:, kc, 0:nb], kc == 0, kc == 15, r=[hT, w], w=[p])
            o = ob[io % 3]
            io += 1
            if io % 2 == 0:
                k.cp("dve", o[:, 0:nb], p[:, 0:nb], r=[p], w=[o])
            else:
                k.cp("act", o[:, 0:nb], p[:, 0:nb], r=[p], w=[o])
            k.dma(k.dq(), dst[tt * 128:(tt + 1) * 128, b0:b0 + nb], o[:, 0:nb], r=[o], w=[dst])


def proj_fm(k, hT, wsrc, c0, ncols, dst, st, tag):
    wb = [k.sb(f"fw{tag}{i}", [128, 16, 128], BF16, st) for i in range(2)]
    ob = [k.sb(f"fo{tag}{i}", [128, S], F32, st) for i in range(2)]
    ps = [k.psum(f"fp{tag}{i}", [128, 512], F32, st) for i in range(2)]
    assert ncols % 128 == 0
    ip = 0
    for m in range(ncols // 128):
        w = wb[m % 2]
        k.dma("pool", w[:], wsrc[:, c0 + m * 128:c0 + (m + 1) * 128].rearrange("(kc p) n -> p kc n", p=128), w=[w])
        o = ob[m % 2]
        for n in range(4):
            p = ps[ip % 2]
            ip += 1
            for kc in range(16):
                k.mm(p[:], w[:, kc, :], hT[:, kc, n * 512:(n + 1) * 512], kc == 0, kc == 15, r=[hT, w], w=[p])
            if ip % 2 == 0:
                k.cp("dve", o[:, n * 512:(n + 1) * 512], p[:], r=[p], w=[o])
            else:
                k.cp("act", o[:, n * 512:(n + 1) * 512], p[:], r=[p], w=[o])
        k.dma(k.dq(), dst[m * 128:(m + 1) * 128, :], o[:], r=[o], w=[dst])


def stage_merge(k, c, hT, l, w_in, w_branch, w_out, ys_fm, mods, y):
    mergedT = None
    with k.phase() as st0:
        mergedT = k.sb(f"mergedT{l}", [128, 16, S], BF16, st0)
        with k.phase() as st:
            wg = [k.sb(f"mwg{l}{i}", [128, 16, 128], BF16, st) for i in range(2)]
            wbr = [k.sb(f"mwb{l}{i}", [128, 4, 128], BF16, st) for i in range(2)]
            ysb = [k.sb(f"mys{l}{i}", [128, 4, S], BF16, st) for i in range(2)]
            macc = k.sb(f"macc{l}", [128, S], F32, st)
            sg = [k.sb(f"msg{l}{i}", [128, 512], F32, st) for i in range(2)]
            tmp = [k.sb(f"mtmp{l}{i}", [128, 512], F32, st) for i in range(2)]
            pg = [k.psum(f"mpg{l}{i}", [128, 512], F32, st) for i in range(2)]
            pz = [k.psum(f"mpz{l}{i}", [128, 512], F32, st) for i in range(2)]
            it = 0
            for j in range(16):
                for n in range(4):
                    w1 = wg[it % 2]
                    w2 = wbr[it % 2]
                    yb = ysb[it % 2]
                    k.dma("pool", w1[:], w_in[l, :, n * 2048 + j * 128:n * 2048 + (j + 1) * 128].rearrange("(kc p) n -> p kc n", p=128), w=[w1])
                    k.dma("pool", w2[:], w_branch[l, n, :, j * 128:(j + 1) * 128].rearrange("(kc p) n -> p kc n", p=128), w=[w2])
                    k.dma(k.dq(), yb[:], ys_fm[n].rearrange("(kc p) s -> p kc s", p=128), r=[ys_fm], w=[yb])
                    for t in range(4):
                        sl = slice(t * 512, (t + 1) * 512)
                        g = pg[it % 2]
                        z = pz[it % 2]
                        sgt = sg[it % 2]
                        tm = tmp[it % 2]
                        it += 1
                        for kc in range(16):
                            k.mm(g[:], w1[:, kc, :], hT[:, kc, sl], kc == 0, kc == 15, r=[w1, hT], w=[g])
                        for kc in range(4):
                            k.mm(z[:], w2[:, kc, :], yb[:, kc, sl], kc == 0, kc == 3, r=[w2, yb], w=[z])
                        k.act(sgt[:], g[:], AF.Sigmoid, r=[g], w=[sgt])
                        if n == 0:
                            k.tt("dve", macc[:, sl], sgt[:], z[:], ALU.mult, r=[sgt, z], w=[macc])
                        else:
                            k.tt("dve", tm[:], sgt[:], z[:], ALU.mult, r=[sgt, z], w=[tm])
                            k.tt("pool", macc[:, sl], macc[:, sl], tm[:], ALU.add, r=[macc, tm], w=[macc])
                k.cp("act", mergedT[:, j, :], macc[:], r=[macc], w=[mergedT])
        with k.phase() as st:
            wo = [k.sb(f"mwo{l}{i}", [128, 16, 512], BF16, st) for i in range(2)]
            gtb = k.sb(f"mgtb{l}", [128, D], F32, st)
            xt = [k.sb(f"mxt{l}{i}", [128, 512], F32, st) for i in range(3)]
            tm2 = [k.sb(f"mt2{l}{i}", [128, 512], F32, st) for i in range(2)]
            po = [k.psum(f"mpo{l}{i}", [128, 512], F32, st) for i in range(2)]
            k.dma("sp", gtb[:], mods["gt_scr"][l, 0].rearrange("a b -> (a b)").partition_broadcast(128), r=[mods["gt_scr"]], w=[gtb])
            it = 0
            for nd in range(4):
                w = wo[nd % 2]
                k.dma("pool", w[:], w_out[l, :, nd * 512:(nd + 1) * 512].rearrange("(kc p) n -> p kc n", p=128), w=[w])
                for tt in range(16):
                    p = po[it % 2]
                    x = xt[it % 3]
                    t2 = tm2[it % 2]
                    it += 1
                    k.dma(k.dq(), x[:], y[tt * 128:(tt + 1) * 128, nd * 512:(nd + 1) * 512], r=[y], w=[x])
                    for kc in range(16):
                        k.mm(p[:], mergedT[:, kc, tt * 128:(tt + 1) * 128], w[:, kc, :], kc == 0, kc == 15, r=[mergedT, w], w=[p])
                    k.tt("dve", t2[:], p[:], gtb[:, nd * 512:(nd + 1) * 512], ALU.mult, r=[p, gtb], w=[t2])
                    k.tt("pool", x[:], x[:], t2[:], ALU.add, r=[x, t2], w=[x])
                    k.dma(k.dq(), y[tt * 128:(tt + 1) * 128, nd * 512:(nd + 1) * 512], x[:], r=[x], w=[y])


def stage_moe(k, c, hT, l, mods, y, nexp=NE):
    w_router = k.inp("w_router", [k.nl, D, NE])
    b_router = k.inp("b_router", [k.nl, NE])
    w_gu = k.inp("w_gu", [k.nl, NE, D, 2 * DE])
    b_gu = k.inp("b_gu_col", [k.nl, 128, NE * 12])
    w_down = k.inp("w_down", [k.nl, NE, DE, D])
    b_down = k.inp("b_down", [k.nl, NE, D])
    with k.phase() as st0:
        wts = k.sb(f"wts{l}", [128, 16, NE], F32, st0)
        with k.phase() as st:
            wr = k.sb(f"wr{l}", [128, 16, NE], BF16, st)
            brb = k.sb(f"brb{l}", [128, NE], F32, st)
            lg = [k.sb(f"lg{l}{i}", [128, NE], F32, st) for i in range(2)]
            ex = [k.sb(f"ex{l}{i}", [128, NE], F32, st) for i in range(2)]
            mk = [k.sb(f"mk{l}{i}", [128, NE], F32, st) for i in range(2)]
            m8 = [k.sb(f"m8{l}{i}", [128, 8], F32, st) for i in range(2)]
            sm = [k.sb(f"sm{l}{i}", [128, 4], F32, st) for i in range(2)]
            pr = [k.psum(f"pr{l}{i}", [128, 512], F32, st) for i in range(2)]
            k.dma("pool", wr[:], w_router[l].rearrange("(kc p) n -> p kc n", p=128), w=[wr])
            k.dma("sp", brb[:], b_router[l].partition_broadcast(128), w=[brb])
            for tt in range(16):
                i = tt % 2
                for kc in range(16):
                    k.mm(pr[i][:, 0:NE], hT[:, kc, tt * 128:(tt + 1) * 128], wr[:, kc, :], kc == 0, kc == 15, r=[hT, wr], w=[pr[i]])
                k.tt("dve", lg[i][:], pr[i][:, 0:NE], brb[:], ALU.add, r=[pr[i], brb], w=[lg[i]])
                k.P.op("dve", lambda e, a=m8[i], b=lg[i]: e.max(out=a[:], in_=b[:]), reads=[lg[i]], writes=[m8[i]])
                k.ts("dve", mk[i][:], lg[i][:], m8[i][:, 3:4], None, ALU.is_ge, r=[lg[i], m8[i]], w=[mk[i]])
                k.ts("dve", sm[i][:, 0:1], m8[i][:, 0:1], -1.0, None, ALU.mult, r=[m8[i]], w=[sm[i]])
                k.act(ex[i][:], lg[i][:], AF.Exp, r=[lg[i], sm[i]], w=[ex[i]], bias=sm[i][:, 0:1])
                k.stt(ex[i][:], ex[i][:], 1.0, mk[i][:], ALU.mult, ALU.mult, r=[ex[i], mk[i]], w=[ex[i], sm[i]], accum=sm[i][:, 1:2])
                k.recip(sm[i][:, 2:3], sm[i][:, 1:2], r=[sm[i]], w=[sm[i]])
                k.ts("dve", wts[:, tt, :], ex[i][:], sm[i][:, 2:3], None, ALU.mult, r=[ex[i], sm[i]], w=[wts])
        if "wtsd" in k.dbg:
            wd_ = k.dram("wtsd", [128, 16, NE])
            k.dma("sp", wd_[:], wts[:], r=[wts], w=[wd_])
        if not hasattr(k, "moe_act"):
            k.moe_act = k.dram("moe_act", [NE, 6, 128, S], BF16)
        act_scr = k.moe_act
        with k.phase() as st:
            acc = k.sb(f"acc{l}", [128, 16, 1024], F32, st)
            actT = k.sb(f"actT{l}", [128, 6, S], BF16, st)
            wgu = [k.sb(f"wgu{l}{i}", [128, 16, 2, 128], BF16, st) for i in range(2)]
            wdh = [k.sb(f"wdh{l}{i}", [128, 6, 512], BF16, st) for i in range(2)]
            bgu = k.sb(f"bgu{l}", [128, NE * 12], F32, st)
            bdn = k.sb(f"bdn{l}", [1, 1024], BF16, st)
            gt_ = [k.sb(f"eg{l}{i}", [128, 512], F32, st) for i in range(2)]
            st_ = [k.sb(f"es{l}{i}", [128, 512], F32, st) for i in range(1)]
            ut_ = [k.sb(f"eu{l}{i}", [128, 512], F32, st) for i in range(2)]
            xt = [k.sb(f"ext{l}{i}", [128, 512], F32, st) for i in range(2)]
            gtc = k.sb(f"egtc{l}", [128, 512], F32, st)
            pgs = [k.psum(f"epg{l}{i}", [128, 512], F32, st) for i in range(2)]
            pus = [k.psum(f"epu{l}{i}", [128, 512], F32, st) for i in range(2)]
            pys = [k.psum(f"epy{l}{i}", [128, 512], F32, st) for i in range(3)]
            k.dma("sp", bgu[:], b_gu[l], w=[bgu])
            iw = 0
            ie = 0
            iy = 0
            ix = 0
            for half in range(2):
                for e in range(nexp):
                    for nd in range(2):
                        c0 = half * 1024 + nd * 512
                        k.dma("pool", wdh[nd][:], w_down[l, e, :, c0:c0 + 512].rearrange("(kc p) n -> p kc n", p=128), w=[wdh[nd]])
                    k.dma("pool", bdn[:], b_down[l, e:e + 1, half * 1024:(half + 1) * 1024], w=[bdn])
                    if half == 0:
                        abuf = actT
                        aview = lambda kc, tt, ab=actT: ab[:, kc, tt * 128:(tt + 1) * 128]
                        for m in range(6):
                            w = wgu[iw % 2]
                            iw += 1
                            k.dma("pool", w[:, :, 0, :], w_gu[l, e, :, m * 128:(m + 1) * 128].rearrange("(kc p) n -> p kc n", p=128), w=[w])
                            k.dma("pool", w[:, :, 1, :], w_gu[l, e, :, DE + m * 128:DE + (m + 1) * 128].rearrange("(kc p) n -> p kc n", p=128), w=[w])
                            for n in range(4):
                                tok = slice(n * 512, (n + 1) * 512)
                                g = pgs[ie % 2]
                                u = pus[ie % 2]
                                gs_ = gt_[ie % 2]
                                ss_ = st_[0]
                                us_ = ut_[ie % 2]
                                ie += 1
                                for kc in range(16):
                                    k.mm(g[:], w[:, kc, 0, :], hT[:, kc, tok], kc == 0, kc == 15, r=[w, hT], w=[g])
                                for kc in range(16):
                                    k.mm(u[:], w[:, kc, 1, :], hT[:, kc, tok], kc == 0, kc == 15, r=[w, hT], w=[u])
                                cg = e * 12 + m
                                cu = e * 12 + 6 + m
                                k.ts("dve", gs_[:], g[:], bgu[:, cg:cg + 1], 7.0, ALU.add, ALU.min, r=[g, bgu], w=[gs_])
                                k.act(ss_[:], gs_[:], AF.Sigmoid, r=[gs_], w=[ss_], scale=1.702)
                                k.ts("dve", us_[:], u[:], bgu[:, cu:cu + 1], 7.0, ALU.add, ALU.min, r=[u, bgu], w=[us_])
                                k.ts("pool", us_[:], us_[:], -7.0, 1.0, ALU.max, ALU.add, r=[us_], w=[us_])
                                k.tt("pool", gs_[:], gs_[:], ss_[:], ALU.mult, r=[gs_, ss_], w=[gs_])
                                k.tt("dve", actT[:, m, tok], us_[:], gs_[:], ALU.mult, r=[us_, gs_], w=[actT])
                            k.dma(k.dq(), act_scr[e, m], actT[:, m, :], r=[actT], w=[act_scr])
                    else:
                        if e % 2 == 0:
                            abuf = actT
                            k.dma(k.dq(), actT[:], act_scr[e].rearrange("m p s -> p m s"), r=[act_scr], w=[actT])
                            aview = lambda kc, tt, ab=actT: ab[:, kc, tt * 128:(tt + 1) * 128]
                        else:
                            abuf = hT
                            k.dma(k.dq(), hT[:, 0:6, :], act_scr[e].rearrange("m p s -> p m s"), r=[act_scr], w=[hT])
                            aview = lambda kc, tt, ab=hT: ab[:, kc, tt * 128:(tt + 1) * 128]
                    for nd in range(2):
                        for tt in range(16):
                            p = pys[iy % 3]
                            iy += 1
                            for kc in range(6):
                                k.mm(p[:], aview(kc, tt), wdh[nd][:, kc, :], kc == 0, False, r=[abuf, wdh[nd]], w=[p])
                            k.mm(p[:], c["onesb"][0:1, :], bdn[0:1, nd * 512:(nd + 1) * 512], False, True, r=[c["onesb"], bdn], w=[p])
                            wc = wts[:, tt, e:e + 1]
                            a_ = acc[:, tt, nd * 512:(nd + 1) * 512]
                            if e == 0:
                                k.ts("dve", a_, p[:], wc, None, ALU.mult, r=[p, wts], w=[acc])
                            else:
                                k.stt(a_, p[:], wc, a_, ALU.mult, ALU.add, r=[p, wts, acc], w=[acc])
                for nd in range(2):
                    c0 = half * 1024 + nd * 512
                    cs = slice(c0, c0 + 512)
                    k.dma("sp", gtc[:], mods["gt_scr"][l, 1].rearrange("a b -> (a b)")[c0:c0 + 512].partition_broadcast(128),
                          r=[mods["gt_scr"]], w=[gtc])
                    for tt in range(16):
                        x = xt[ix % 2]
                        ix += 1
                        rows = slice(tt * 128, (tt + 1) * 128)
                        a_ = acc[:, tt, nd * 512:(nd + 1) * 512]
                        k.dma(k.dq(), x[:], y[rows, cs], r=[y], w=[x])
                        k.tt("dve", a_, a_, gtc[:], ALU.mult, r=[acc, gtc], w=[acc])
                        k.tt("pool", x[:], x[:], a_, ALU.add, r=[x, acc], w=[x])
                        k.dma(k.dq(), y[rows, cs], x[:], r=[x], w=[y])


def stage_final(k, c, y):
    gf = k.inp("g_final", [D])
    with k.phase() as st:
        gb = k.sb("gfb", [128, D], F32, st)
        xt = [k.sb(f"fx{i}", [128, D], F32, st) for i in range(2)]
        sq = k.sb("fsq", [128, D], BF16, st)
        ss = [k.sb(f"fss{i}", [128, 4], F32, st) for i in range(2)]
        k.dma("sp", gb[:], gf[:].partition_broadcast(128), w=[gb])
        for tt in range(16):
            x = xt[tt % 2]
            s = ss[tt % 2]
            k.dma(k.dq(), x[:], y[tt * 128:(tt + 1) * 128, :], r=[y], w=[x])
            k.act(sq[:], x[:], AF.Square, r=[x], w=[sq, s], accum=s[:, 0:1])
            k.ts("dve", s[:, 1:2], s[:, 0:1], 1.0 / D, EPS, ALU.mult, ALU.add, r=[s], w=[s])
            k.act(s[:, 2:3], s[:, 1:2], AF.Sqrt, r=[s], w=[s])
            k.recip(s[:, 3:4], s[:, 2:3], r=[s], w=[s])
            k.stt(x[:], x[:], s[:, 3:4], gb[:], ALU.mult, ALU.mult, r=[x, s, gb], w=[x])
            k.dma(k.dq(), y[tt * 128:(tt + 1) * 128, :], x[:], r=[x], w=[y])


def build(nc, stack, cfg):
    k = KB(nc, stack, cfg)
    nl = cfg.get("nl", L)
    k.nl = nl
    stages = cfg.get("stages", "all")
    c = build_consts(k)
    x_in = k.inp("x", [S, D])
    y = T(nc.dram_tensor("y", [S, D], F32, kind="ExternalOutput").ap(), "y")
    k.outs["y"] = y
    w_in = k.inp("w_in", [nl, D, N_IN])
    k.dma("sp", y[0:1024, :], x_in[0:1024, :], w=[y])
    k.dma("act", y[1024:2048, :], x_in[1024:2048, :], w=[y])
    mods = stage_mod(k, c, nl) if (stages == "all" or "mod" in stages or "norm" in stages or "merge" in stages or "moe" in stages) else None
    hT = k.sb("hT", [128, 16, S], BF16)
    hg_tm = k.dram("hg_tm", [S, 2048])
    s5_fm = k.dram("s5_fm", [512, S])
    m2z_tm = k.dram("m2z_tm", [S, 512])
    m2x_fm = k.dram("m2x_fm", [1024, S])
    m2dt_tm = k.dram("m2dt_tm", [S, 8])
    rk_tm = k.dram("rk_tm", [S, 1792])
    if cfg.get("ys_input"):
        ys_fm = k.inp("ys_fm", [4, 512, S], BF16)
    else:
        ys_fm = k.dram("ys_fm", [4, 512, S], BF16)
    if not cfg.get("ys_input"):
        with k.phase() as st:
            zt_ = k.sb("yszero", [128, S], BF16, st)
            k.memset("dve", zt_[:], 0.0, w=[zt_])
            for n_ in range(4):
                for j_ in range(4):
                    k.dma(k.dq(), ys_fm[n_, j_ * 128:(j_ + 1) * 128, :], zt_[:], r=[zt_], w=[ys_fm])
    for l in range(nl):
        if mods is not None:
            stage_norm(k, c, y, hT, mods["gsa"], l, mods["modc"], 0, f"a{l}")
        if "hTd" in k.dbg:
            hdbg = k.dram("hTd", [128, 16, S], BF16) if l == 0 else k.outs["hTd"]
            k.dma("sp", hdbg[:], hT[:], r=[hT], w=[hdbg])
        if stages == "norm":
            continue
        if "proj" in stages or stages == "all":
            with k.phase() as st:
                proj_tm(k, hT, w_in[l], O_HG, 2048, hg_tm, st, f"hg{l}")
            with k.phase() as st:
                proj_fm(k, hT, w_in[l], O_S5, 512, s5_fm, st, f"s5{l}")
            with k.phase() as st:
                proj_tm(k, hT, w_in[l], O_M2, 512, m2z_tm, st, f"mz{l}")
            with k.phase() as st:
                proj_fm(k, hT, w_in[l], O_M2 + 512, 1024, m2x_fm, st, f"mx{l}")
            with k.phase() as st:
                proj_tm(k, hT, w_in[l], O_M2 + 1536, 8, m2dt_tm, st, f"md{l}")
            with k.phase() as st:
                proj_tm(k, hT, w_in[l], O_RK, 1792, rk_tm, st, f"rk{l}")
        if "mix" in stages or stages == "all":
            mixers(k, c, l, dict(hg_tm=hg_tm, s5_fm=s5_fm, m2z_tm=m2z_tm, m2x_fm=m2x_fm, m2dt_tm=m2dt_tm, rk_tm=rk_tm), ys_fm)
        if "merge" in stages or stages == "all":
            w_branch = k.inp("w_branch", [nl, 4, BR, D])
            w_out = k.inp("w_out", [nl, D, D])
            stage_merge(k, c, hT, l, w_in, w_branch, w_out, ys_fm, mods, y)
        if "moe" in stages or stages == "all":
            stage_norm(k, c, y, hT, mods["gsm"], l, mods["modc"], 48, f"m{l}")
            stage_moe(k, c, hT, l, mods, y, nexp=cfg.get("nexp", NE))
    if stages == "all" or "final" in stages:
        stage_final(k, c, y)
    k.P.finish()
    k.P.emit()
    return k


TWO_PI = 6.283185307179586
PI = 3.141592653589793


def mix_s5(k, c, l, s5_fm, ys_fm):
    nl = k.nl
    lr_i = k.inp("s5_lr_col", [nl, 128, 16])
    li_i = k.inp("s5_li_col", [nl, 128, 16])
    ldt_i = k.inp("s5_ldt_col", [nl, 128, 16])
    bTr_i = k.inp("s5_bT_re", [nl, 16, 128, 128])
    bTi_i = k.inp("s5_bT_im", [nl, 16, 128, 128])
    cTr_i = k.inp("s5_cT_re", [nl, 16, 128, 128])
    cTi_i = k.inp("s5_cT_im", [nl, 16, 128, 128])
    dsk_i = k.inp("s5_d_col", [nl, 128, 4])
    wgl_i = k.inp("s5_w_glu", [nl, 512, 512])
    bgl_i = k.inp("s5_bglu_col", [nl, 128, 4])
    with k.phase() as st0:
        sm = lambda n, sh=[128, 16]: k.sb(f"s5{n}{l}", sh, F32, st0)
        mag = sm("mag"); cs = sm("cs"); sn = sm("sn"); cre = sm("cre"); cim = sm("cim")
        pwc = k.sb(f"s5pwc{l}", [128, 11, 16], F32, st0)
        pws = k.sb(f"s5pws{l}", [128, 11, 16], F32, st0)
        bTr = k.sb(f"s5bTr{l}", [128, 16, 128], BF16, st0)
        bTi = k.sb(f"s5bTi{l}", [128, 16, 128], BF16, st0)
        cAr = k.sb(f"s5cAr{l}", [128, 16, 128], BF16, st0)
        cAi = k.sb(f"s5cAi{l}", [128, 16, 128], BF16, st0)
        k.dma("pool", bTr[:], bTr_i[l].rearrange("t p m -> p t m"), w=[bTr])
        k.dma("pool", bTi[:], bTi_i[l].rearrange("t p m -> p t m"), w=[bTi])
        with k.phase() as st:
            t_ = lambda n, sh=[128, 16]: k.sb(f"s5t{n}{l}", sh, F32, st)
            lr = t_("lr"); li = t_("li"); dt = t_("dt"); a1 = t_("a1"); a2 = t_("a2"); mk = t_("mk"); den = t_("den")
            nr = t_("nr"); abr = t_("abr"); abi = t_("abi"); t1 = t_("t1"); t2 = t_("t2")
            k.dma("sp", lr[:], lr_i[l], w=[lr])
            k.dma("sp", li[:], li_i[l], w=[li])
            k.dma("sp", dt[:], ldt_i[l], w=[dt])
            k.act(dt[:], dt[:], AF.Exp, r=[dt], w=[dt])
            k.tt("dve", t1[:], lr[:], dt[:], ALU.mult, r=[lr, dt], w=[t1])
            k.act(mag[:], t1[:], AF.Exp, r=[t1], w=[mag])
            k.tt("dve", a1[:], li[:], dt[:], ALU.mult, r=[li, dt], w=[a1])
            k.ts("dve", a2[:], a1[:], PI / 2, None, ALU.add, r=[a1], w=[a2])
            for a in (a1, a2):
                for _ in range(6):
                    k.ts("dve", mk[:], a[:], PI, TWO_PI, ALU.is_gt, ALU.mult, r=[a], w=[mk])
                    k.tt("dve", a[:], a[:], mk[:], ALU.subtract, r=[a, mk], w=[a])
            k.act(sn[:], a1[:], AF.Sin, r=[a1], w=[sn])
            k.act(cs[:], a2[:], AF.Sin, r=[a2], w=[cs])
            k.tt("dve", abr[:], mag[:], cs[:], ALU.mult, r=[mag, cs], w=[abr])
            k.tt("dve", abi[:], mag[:], sn[:], ALU.mult, r=[mag, sn], w=[abi])
            k.tt("dve", den[:], lr[:], lr[:], ALU.mult, r=[lr], w=[den])
            k.tt("dve", t1[:], li[:], li[:], ALU.mult, r=[li], w=[t1])
            k.tt("dve", den[:], den[:], t1[:], ALU.add, r=[den, t1], w=[den])
            k.recip(den[:], den[:], r=[den], w=[den])
            k.ts("dve", nr[:], abr[:], -1.0, None, ALU.add, r=[abr], w=[nr])
            k.tt("dve", t1[:], nr[:], lr[:], ALU.mult, r=[nr, lr], w=[t1])
            k.tt("dve", t2[:], abi[:], li[:], ALU.mult, r=[abi, li], w=[t2])
            k.tt("dve", t1[:], t1[:], t2[:], ALU.add, r=[t1, t2], w=[t1])
            k.tt("dve", cre[:], t1[:], den[:], ALU.mult, r=[t1, den], w=[cre])
            k.tt("dve", t1[:], abi[:], lr[:], ALU.mult, r=[abi, lr], w=[t1])
            k.tt("dve", t2[:], nr[:], li[:], ALU.mult, r=[nr, li], w=[t2])
            k.tt("dve", t1[:], t1[:], t2[:], ALU.subtract, r=[t1, t2], w=[t1])
            k.tt("dve", cim[:], t1[:], den[:], ALU.mult, r=[t1, den], w=[cim])
            k.cp("dve", pwc[:, 0, :], cs[:], r=[cs], w=[pwc])
            k.cp("dve", pws[:, 0, :], sn[:], r=[sn], w=[pws])
            for jj in range(1, 11):
                k.tt("dve", t1[:], pwc[:, jj - 1, :], pwc[:, jj - 1, :], ALU.mult, r=[pwc], w=[t1])
                k.tt("dve", t2[:], pws[:, jj - 1, :], pws[:, jj - 1, :], ALU.mult, r=[pws], w=[t2])
                k.tt("dve", pwc[:, jj, :], t1[:], t2[:], ALU.subtract, r=[t1, t2], w=[pwc])
                k.tt("dve", t1[:], pwc[:, jj - 1, :], pws[:, jj - 1, :], ALU.mult, r=[pwc, pws], w=[t1])
                k.ts("dve", pws[:, jj, :], t1[:], 2.0, None, ALU.mult, r=[t1], w=[pws])
            cr = k.sb(f"s5cr{l}", [128, 16, 128], F32, st)
            ci = k.sb(f"s5ci{l}", [128, 16, 128], F32, st)
            tm = [k.sb(f"s5tm{l}{i}", [128, 128], F32, st) for i in range(2)]
            k.dma("sp", cr[:], cTr_i[l].rearrange("t p m -> p t m"), w=[cr])
            k.dma("act", ci[:], cTi_i[l].rearrange("t p m -> p t m"), w=[ci])
            ncim = t_("ncim")
            k.ts("dve", ncim[:], cim[:], -1.0, None, ALU.mult, r=[cim], w=[ncim])
            for t in range(16):
                a = tm[t % 2]
                k.ts("dve", a[:], cr[:, t, :], cre[:, t:t + 1], None, ALU.mult, r=[cr, cre], w=[a])
                k.stt(cAr[:, t, :], ci[:, t, :], ncim[:, t:t + 1], a[:], ALU.mult, ALU.add, r=[ci, ncim, a], w=[cAr])
                k.ts("dve", a[:], cr[:, t, :], ncim[:, t:t + 1], None, ALU.mult, r=[cr, ncim], w=[a])
                k.ts("dve", ci[:, t, :], ci[:, t, :], cre[:, t:t + 1], -1.0, ALU.mult, ALU.mult, r=[ci, cre], w=[ci])
                k.tt("dve", cAi[:, t, :], ci[:, t, :], a[:], ALU.add, r=[ci, a], w=[cAi])
        with k.phase() as st:
            tabc = k.sb(f"s5tabc{l}", [128, S], F32, st)
            tabs = k.sb(f"s5tabs{l}", [128, S], F32, st)
            bur = k.sb(f"s5bur{l}", [128, S], F32, st)
            bui = k.sb(f"s5bui{l}", [128, S], F32, st)
            tA = k.sb(f"s5tA{l}", [128, S], F32, st)
            tB = k.sb(f"s5tB{l}", [128, S], F32, st)
            tC = k.sb(f"s5tC{l}", [128, S], F32, st)
            Rt = k.sb(f"s5R{l}", [128, S], F32, st)
            xr = k.sb(f"s5xr{l}", [128, S], BF16, st)
            xi = k.sb(f"s5xi{l}", [128, S], BF16, st)
            ub = k.sb(f"s5ub{l}", [128, S], BF16, st)
            uf = k.sb(f"s5uf{l}", [128, S], F32, st)
            ygb = k.sb(f"s5ygb{l}", [128, 4, S], BF16, st)
            dsk = k.sb(f"s5dsk{l}", [128, 4], F32, st)
            bgl = k.sb(f"s5bgl{l}", [128, 4], F32, st)
            wgl = k.sb(f"s5wgl{l}", [128, 4, 512], BF16, st)
            pb = [k.psum(f"s5pb{l}{i}", [128, 512], F32, st) for i in range(4)]
            py = [k.psum(f"s5py{l}{i}", [128, 512], F32, st) for i in range(4)]
            k.dma("sp", dsk[:], dsk_i[l], w=[dsk])
            k.dma("sp", bgl[:], bgl_i[l], w=[bgl])
            k.dma("pool", wgl[:], wgl_i[l].rearrange("(kc p) n -> p kc n", p=128), w=[wgl])
            for ct in range(4):
                k.dma("pool", ub[:], s5_fm[ct * 128:(ct + 1) * 128, :], r=[s5_fm], w=[ub])
                k.dma("sp", uf[:], s5_fm[ct * 128:(ct + 1) * 128, :], r=[s5_fm], w=[uf])
                for s4 in range(4):
                    stt_ = ct * 4 + s4
                    k.memset("pool", tabc[:, 0:1], 1.0, w=[tabc])
                    k.memset("pool", tabs[:, 0:1], 0.0, w=[tabs])
                    for jj in range(11):
                        n = 1 << jj
                        pc = pwc[:, jj, stt_:stt_ + 1]
                        ps_ = pws[:, jj, stt_:stt_ + 1]
                        k.ts("dve", tA[:, 0:n], tabs[:, 0:n], ps_, None, ALU.mult, r=[tabs, pws], w=[tA])
                        k.ts("dve", tB[:, 0:n], tabs[:, 0:n], pc, None, ALU.mult, r=[tabs, pwc], w=[tB])
                        k.stt(tC[:, 0:n], tabc[:, 0:n], pc, tA[:, 0:n], ALU.mult, ALU.subtract, r=[tabc, pwc, tA], w=[tC])
                        k.stt(tabs[:, n:2 * n], tabc[:, 0:n], ps_, tB[:, 0:n], ALU.mult, ALU.add, r=[tabc, pws, tB], w=[tabs])
                        k.cp("pool", tabc[:, n:2 * n], tC[:, 0:n], r=[tC], w=[tabc])
                    k.ts("dve", Rt[:], tabc[:], 0.0, mag[:, stt_:stt_ + 1], ALU.mult, ALU.add, r=[tabc, mag], w=[Rt])
                    for n4 in range(4):
                        sl = slice(n4 * 512, (n4 + 1) * 512)
                        p1 = pb[(n4 % 2) * 2]
                        p2 = pb[(n4 % 2) * 2 + 1]
                        k.mm(p1[:], bTr[:, stt_, :], ub[:, sl], True, True, r=[bTr, ub], w=[p1])
                        k.mm(p2[:], bTi[:, stt_, :], ub[:, sl], True, True, r=[bTi, ub], w=[p2])
                        k.cp("act", bur[:, sl], p1[:], r=[p1], w=[bur])
                        k.cp("act", bui[:, sl], p2[:], r=[p2], w=[bui])
                    k.tt("dve", tA[:], tabc[:], bur[:], ALU.mult, r=[tabc, bur], w=[tA])
                    k.tt("pool", tB[:], tabs[:], bui[:], ALU.mult, r=[tabs, bui], w=[tB])
                    k.tt("dve", tA[:], tA[:], tB[:], ALU.add, r=[tA, tB], w=[tA])
                    k.tt("pool", tB[:], tabc[:], bui[:], ALU.mult, r=[tabc, bui], w=[tB])
                    k.tt("dve", tC[:], tabs[:], bur[:], ALU.mult, r=[tabs, bur], w=[tC])
                    k.tt("pool", tB[:], tB[:], tC[:], ALU.subtract, r=[tB, tC], w=[tB])
                    k.P.op("dve", lambda e: e.tensor_tensor_scan(out=bur[:], data0=Rt[:], data1=tA[:], initial=0.0, op0=ALU.mult, op1=ALU.add),
                           reads=[Rt, tA], writes=[bur])
                    k.P.op("dve", lambda e: e.tensor_tensor_scan(out=bui[:], data0=Rt[:], data1=tB[:], initial=0.0, op0=ALU.mult, op1=ALU.add),
                           reads=[Rt, tB], writes=[bui])
                    k.tt("dve", tA[:], tabc[:], bur[:], ALU.mult, r=[tabc, bur], w=[tA])
                    k.tt("pool", tC[:], tabs[:], bui[:], ALU.mult, r=[tabs, bui], w=[tC])
                    k.tt("dve", xr[:], tA[:], tC[:], ALU.subtract, r=[tA, tC], w=[xr])
                    k.tt("pool", tB[:], tabs[:], bur[:], ALU.mult, r=[tabs, bur], w=[tB])
                    k.tt("dve", tC[:], tabc[:], bui[:], ALU.mult, r=[tabc, bui], w=[tC])
                    k.tt("pool", xi[:], tB[:], tC[:], ALU.add, r=[tB, tC], w=[xi])
                    for n4 in range(4):
                        sl = slice(n4 * 512, (n4 + 1) * 512)
                        k.mm(py[n4][:], cAr[:, stt_, :], xr[:, sl], s4 == 0, False, r=[cAr, xr], w=[py[n4]])
                        k.mm(py[n4][:], cAi[:, stt_, :], xi[:, sl], False, s4 == 3, r=[cAi, xi], w=[py[n4]])
                for n4 in range(4):
                    sl = slice(n4 * 512, (n4 + 1) * 512)
                    k.stt(tA[:, sl], uf[:, sl], dsk[:, ct:ct + 1], py[n4][:], ALU.mult, ALU.add, r=[uf, dsk, py[n4]], w=[tA])
                k.tt("pool", tB[:], tA[:], tA[:], ALU.mult, r=[tA], w=[tB])
                k.ts("dve", tB[:], tB[:], 0.044715, 1.0, ALU.mult, ALU.add, r=[tB], w=[tB])
                k.tt("pool", tB[:], tB[:], tA[:], ALU.mult, r=[tB, tA], w=[tB])
                k.act(tB[:], tB[:], AF.Tanh, r=[tB], w=[tB], scale=0.7978845608028654)
                k.ts("dve", tB[:], tB[:], 1.0, 0.5, ALU.add, ALU.mult, r=[tB], w=[tB])
                k.tt("dve", ygb[:, ct, :], tB[:], tA[:], ALU.mult, r=[tB, tA], w=[ygb])
            for ct in range(4):
                for n4 in range(4):
                    sl = slice(n4 * 512, (n4 + 1) * 512)
                    p = pb[n4]
                    for kc in range(4):
                        k.mm(p[:], wgl[:, kc, ct * 128:(ct + 1) * 128], ygb[:, kc, sl], kc == 0, kc == 3, r=[wgl, ygb], w=[p])
                    k.act(tA[:, sl], p[:], AF.Sigmoid, r=[p, bgl], w=[tA], bias=bgl[:, ct:ct + 1])
                k.tt("dve", xr[:], tA[:], ygb[:, ct, :], ALU.mult, r=[tA, ygb], w=[xr])
                k.dma("sp", ys_fm[1, ct * 128:(ct + 1) * 128, :], xr[:], r=[xr], w=[ys_fm])


def mix_ssd(k, c, l, m2z_tm, m2x_fm, m2dt_tm, ys_fm):
    nl = k.nl
    cw_i = k.inp("m2_convw_col", [nl, 128, 8, 4])
    cb_i = k.inp("m2_convb_col", [nl, 128, 8])
    dtb_i = k.inp("m2_dt_bias", [nl, 8])
    alog_i = k.inp("m2_a_log", [nl, 8])
    dsk_i = k.inp("m2_d", [nl, 8])
    nrm_i = k.inp("m2_norm", [nl, 512])
    triu = c["triu"]
    with k.phase() as st0:
        xcb = k.sb(f"mxcb{l}", [128, 8, S], BF16, st0)
        ysc = k.sb(f"mysc{l}", [128, 4, S], BF16, st0)
        with k.phase() as st:
            cw = k.sb(f"mcw{l}", [128, 8, 4], F32, st)
            cb = k.sb(f"mcb{l}", [128, 8], F32, st)
            xin = [k.sb(f"mxin{l}{i}", [128, S], F32, st) for i in range(2)]
            ac = [k.sb(f"mac{l}{i}", [128, S], F32, st) for i in range(2)]
            k.dma("sp", cw[:], cw_i[l], w=[cw])
            k.dma("sp", cb[:], cb_i[l], w=[cb])
            for ct in range(8):
                x = xin[ct % 2]
                a = ac[ct % 2]
                k.dma(k.dq(), x[:], m2x_fm[ct * 128:(ct + 1) * 128, :], r=[m2x_fm], w=[x])
                k.ts("dve", a[:], x[:], cw[:, ct, 3:4], cb[:, ct:ct + 1], ALU.mult, ALU.add, r=[x, cw, cb], w=[a])
                for sh in (1, 2, 3):
                    k.stt(a[:, sh:S], x[:, 0:S - sh], cw[:, ct, 3 - sh:4 - sh], a[:, sh:S], ALU.mult, ALU.add, r=[x, cw, a], w=[a])
                k.act(xcb[:, ct, :], a[:], AF.Silu, r=[a], w=[xcb])
        with k.phase() as st:
            f = lambda n, sh, dt=F32: k.sb(f"m{n}{l}", sh, dt, st)
            dtb = f("dtb", [128, 8]); aneg = f("aneg", [128, 8]); dskb = f("dskb", [128, 8]); nrmb = f("nrmb", [128, 512])
            ST = f("ST", [128, 8, 64]); STb = f("STb", [128, 8, 64], BF16)
            xs_t = f("xst", [128, 8, 64]); B_t = f("Bt", [128, 2, 128], BF16)
            dtt = f("dtt", [128, 8]); adt = f("adt", [128, 8]); acs = f("acs", [128, 8]); nacs = f("nacs", [128, 8])
            lastb = f("lastb", [128, 8]); elast = f("elast", [128, 8]); eac = f("eac", [128, 8]); dcs = f("dcs", [128, 8])
            rhsh = [f(f"rhsh{i}", [128, 128]) for i in range(2)]
            Ld = [f(f"Ld{i}", [128, 128]) for i in range(2)]
            Mh = [f(f"Mh{i}", [128, 128], BF16) for i in range(2)]
            xdt = f("xdt", [128, 8, 64], BF16); xdw = f("xdw", [128, 8, 64], BF16)
            ysb = f("ysb", [128, 512]); zt = f("zt", [128, 512]); sq = f("sq", [128, 512]); ynb = f("ynb", [128, 512], BF16)
            ss = f("ss", [128, 4])
            pT = k.psum(f"mpT{l}", [128, 8, 128], BF16, st)
            pac = k.psum(f"mpac{l}", [128, 512], F32, st)
            prow = [k.psum(f"mprow{l}{i}", [128, 512], F32, st) for i in range(2)]
            pcb = k.psum(f"mpcb{l}", [128, 4, 128], F32, st)
            pyd = k.psum(f"mpyd{l}", [128, 512], F32, st)
            pyo = k.psum(f"mpyo{l}", [128, 512], F32, st)
            pst = k.psum(f"mpst{l}", [128, 512], F32, st)
            k.dma("sp", dtb[:], dtb_i[l].partition_broadcast(128), w=[dtb])
            k.dma("sp", aneg[:], alog_i[l].partition_broadcast(128), w=[aneg])
            k.dma("sp", dskb[:], dsk_i[l].partition_broadcast(128), w=[dskb])
            k.dma("sp", nrmb[:], nrm_i[l].partition_broadcast(128), w=[nrmb])
            k.act(aneg[:], aneg[:], AF.Exp, r=[aneg], w=[aneg])
            k.ts("dve", aneg[:], aneg[:], -1.0, None, ALU.mult, r=[aneg], w=[aneg])
            k.memset("dve", ST[:], 0.0, w=[ST])
            k.memset("dve", STb[:], 0.0, w=[STb])
            idb = c["idb"]
            for ch in range(k.cfg.get("ssd_ch0", 0), k.cfg.get("ssd_ch0", 0) + k.cfg.get("ssd_chunks", 16)):
                tok = slice(ch * 128, (ch + 1) * 128)
                for j in range(4):
                    k.tr(pT[:, j, :], xcb[:, j, tok], idb[:], r=[xcb, idb], w=[pT])
                for g in range(2):
                    k.tr(pT[:, 4 + g, :], xcb[:, 4 + g, tok], idb[:], r=[xcb, idb], w=[pT])
                k.cp("act", xs_t[:].rearrange("p h d -> p (h d)"), pT[:, 0:4, :].rearrange("p a b -> p (a b)"), r=[pT], w=[xs_t])
                k.cp("act", B_t[:].rearrange("p g n -> p (g n)"), pT[:, 4:6, :].rearrange("p a b -> p (a b)"), r=[pT], w=[B_t])
                lvl = k.cfg.get("ssd_lvl", 9)
                if lvl >= 2:
                    k.dma("sp", dtt[:], m2dt_tm[tok, :], r=[m2dt_tm], w=[dtt])
                    k.tt("dve", dtt[:], dtt[:], dtb[:], ALU.add, r=[dtt, dtb], w=[dtt])
                    k.act(dtt[:], dtt[:], AF.Exp, r=[dtt], w=[dtt])
                    k.act(dtt[:], dtt[:], AF.Ln, r=[dtt], w=[dtt], bias=1.0)
                    k.tt("dve", adt[:], dtt[:], aneg[:], ALU.mult, r=[dtt, aneg], w=[adt])
                    k.mm(pac[:, 0:8], triu[:], adt[:], True, True, r=[triu, adt], w=[pac])
                    k.mm(pac[:, 8:16], c["ones"][:], adt[:], True, True, r=[c["ones"], adt], w=[pac])
                    k.cp("dve", acs[:], pac[:, 0:8], r=[pac], w=[acs])
                    k.ts("dve", nacs[:], pac[:, 0:8], -1.0, None, ALU.mult, r=[pac], w=[nacs])
                    k.cp("dve", lastb[:], pac[:, 8:16], r=[pac], w=[lastb])
                    k.act(elast[:], lastb[:], AF.Exp, r=[lastb], w=[elast])
                    k.act(eac[:], acs[:], AF.Exp, r=[acs], w=[eac])
                    k.tt("dve", dcs[:], lastb[:], acs[:], ALU.subtract, r=[lastb, acs], w=[dcs])
                    k.act(dcs[:], dcs[:], AF.Exp, r=[dcs], w=[dcs])
                if lvl >= 3:
                    for h in range(8):
                        k.ts("dve", xdt[:, h, :], xs_t[:, h, :], dtt[:, h:h + 1], None, ALU.mult, r=[xs_t, dtt], w=[xdt])
                        k.ts("dve", xdw[:, h, :], xs_t[:, h, :], dtt[:, h:h + 1], dcs[:, h:h + 1], ALU.mult, ALU.mult, r=[xs_t, dtt, dcs], w=[xdw])
                    for g in range(2):
                        k.mm(pcb[:, g, :], xcb[:, 4 + g, tok], xcb[:, 6 + g, tok], True, True, r=[xcb], w=[pcb])
                if lvl >= 4:
                    for h in range(8):
                        i2 = h % 2
                        k.ts("dve", rhsh[i2][:], triu[:], adt[:, h:h + 1], None, ALU.mult, r=[triu, adt], w=[rhsh[i2]])
                        k.mm(prow[i2][:, 0:128], c["ones"][:], rhsh[i2][:], True, True, r=[c["ones"], rhsh[i2]], w=[prow[i2]])
                        k.ts("dve", Ld[i2][:], prow[i2][:, 0:128], nacs[:, h:h + 1], 0.0, ALU.add, ALU.min, r=[prow[i2], nacs], w=[Ld[i2]])
                        k.act(Ld[i2][:], Ld[i2][:], AF.Exp, r=[Ld[i2]], w=[Ld[i2]])
                        k.tt("pool", Ld[i2][:], Ld[i2][:], triu[:], ALU.mult, r=[Ld[i2], triu], w=[Ld[i2]])
                        k.tt("dve", Mh[i2][:], pcb[:, h // 4, :], Ld[i2][:], ALU.mult, r=[pcb, Ld[i2]], w=[Mh[i2]])
                        k.mm(pyd[:, h * 64:(h + 1) * 64], Mh[i2][:], xdt[:, h, :], True, True, r=[Mh[i2], xdt], w=[pyd])
                        k.mm(pyo[:, h * 64:(h + 1) * 64], xcb[:, 6 + h // 4, tok], STb[:, h, :], True, True, r=[xcb, STb], w=[pyo])
                        k.mm(pst[:, h * 64:(h + 1) * 64], B_t[:, h // 4, :], xdw[:, h, :], True, True, r=[B_t, xdw], w=[pst])
                if lvl >= 5:
                    if ch == 0:
                        k.dump("d_xcb", xcb, [128, 8, S], BF16)
                        k.dump("d_dtt", dtt, [128, 8]); k.dump("d_acs", acs, [128, 8]); k.dump("d_lastb", lastb, [128, 8])
                        k.dump("d_Ld", Ld[1], [128, 128]); k.dump("d_xdt", xdt, [128, 8, 64], BF16); k.dump("d_xst", xs_t, [128, 8, 64])
                    k.cp("act", ysb[:], pyd[:], r=[pyd], w=[ysb])
                    if ch == 0:
                        k.dump("d_yd", ysb, [128, 512])
                    for h in range(8):
                        hs = slice(h * 64, (h + 1) * 64)
                        k.stt(ysb[:, hs], pyo[:, hs], eac[:, h:h + 1], ysb[:, hs], ALU.mult, ALU.add, r=[pyo, eac, ysb], w=[ysb])
                        k.stt(ysb[:, hs], xs_t[:, h, :], dskb[:, h:h + 1], ysb[:, hs], ALU.mult, ALU.add, r=[xs_t, dskb, ysb], w=[ysb])
                        k.stt(ST[:, h, :], ST[:, h, :], elast[:, h:h + 1], pst[:, hs], ALU.mult, ALU.add, r=[ST, elast, pst], w=[ST])
                    k.cp("pool", STb[:], ST[:], r=[ST], w=[STb])
                if lvl >= 6:
                    k.dma("act", zt[:], m2z_tm[tok, :], r=[m2z_tm], w=[zt])
                    k.act(zt[:], zt[:], AF.Silu, r=[zt], w=[zt])
                    k.tt("dve", ysb[:], ysb[:], zt[:], ALU.mult, r=[ysb, zt], w=[ysb])
                    if ch == 0:
                        k.dump("d_yg", ysb, [128, 512])
                    k.tt("pool", sq[:], ysb[:], ysb[:], ALU.mult, r=[ysb], w=[sq])
                    k.red("dve", ss[:, 0:2], sq[:].rearrange("p (g d) -> p g d", g=2), ALU.add, r=[sq], w=[ss])
                    k.ts("dve", ss[:, 0:2], ss[:, 0:2], 1.0 / 256, EPS, ALU.mult, ALU.add, r=[ss], w=[ss])
                    k.act(ss[:, 0:2], ss[:, 0:2], AF.Sqrt, r=[ss], w=[ss])
                    k.recip(ss[:, 2:4], ss[:, 0:2], r=[ss], w=[ss])
                    for g in range(2):
                        gs_ = slice(g * 256, (g + 1) * 256)
                        k.stt(ynb[:, gs_], ysb[:, gs_], ss[:, 2 + g:3 + g], nrmb[:, gs_], ALU.mult, ALU.mult, r=[ysb, ss, nrmb], w=[ynb])
                    if ch == 0:
                        k.dump("d_ss", ss, [128, 4]); k.dump("d_ynb", ynb, [128, 512], BF16)
                    for j in range(4):
                        k.tr(pT[:, j, :], ynb[:, j * 128:(j + 1) * 128], idb[:], r=[ynb, idb], w=[pT])
                    k.cp("act", ysc[:, :, tok], pT[:, 0:4, :], r=[pT], w=[ysc])
                if k.cfg.get("ssd_barrier", True):
                    k.P.barrier()
        for j in range(4):
            k.dma(k.dq(), ys_fm[2, j * 128:(j + 1) * 128, :], ysc[:, j, :], r=[ysc], w=[ys_fm])


def mix_hgrn2(k, c, l, hg_tm, ys_fm):
    nl = k.nl
    lb_i = k.inp("hg_lower_bound", [L, 512])
    on_i = k.inp("hg_onorm", [nl, 128])
    triu = c["triu"]
    T_ = 64
    with k.phase() as st0:
        ysc = k.sb(f"hysc{l}", [128, 4, S], BF16, st0)
        lbb = k.sb(f"hlbb{l}", [T_, 512], F32, st0)
        omlb = k.sb(f"homlb{l}", [T_, 512], F32, st0)
        onb = k.sb(f"honb{l}", [T_, 512], F32, st0)
        mmid = k.sb(f"hmmid{l}", [T_, T_], F32, st0)
        mask4 = k.sb(f"hmask4{l}", [T_, 4, T_], F32, st0)
        with k.phase() as st:
            raw = k.sb(f"hraw{l}", [T_, L, 512], F32, st)
            mx = k.sb(f"hmx{l}", [T_, 512], F32, st)
            sm = k.sb(f"hsm{l}", [T_, 512], F32, st)
            for j in range(L):
                k.dma(k.dq(), raw[:, j, :], lb_i[j].partition_broadcast(T_), w=[raw])
            k.tt("dve", mx[:], raw[:, 0, :], raw[:, 1, :], ALU.max, r=[raw], w=[mx])
            for j in range(2, L):
                k.tt("dve", mx[:], mx[:], raw[:, j, :], ALU.max, r=[raw, mx], w=[mx])
            for j in range(L):
                k.tt("dve", raw[:, j, :], raw[:, j, :], mx[:], ALU.subtract, r=[raw, mx], w=[raw])
                k.act(raw[:, j, :], raw[:, j, :], AF.Exp, r=[raw], w=[raw])
            k.tt("dve", sm[:], raw[:, 0, :], raw[:, 1, :], ALU.add, r=[raw], w=[sm])
            for j in range(2, L):
                k.tt("dve", sm[:], sm[:], raw[:, j, :], ALU.add, r=[raw, sm], w=[sm])
            k.recip(sm[:], sm[:], r=[sm], w=[sm])
            k.memset("dve", lbb[:], 0.0, w=[lbb])
            for j in range(1, l + 1):
                k.tt("dve", lbb[:], lbb[:], raw[:, j, :], ALU.add, r=[raw, lbb], w=[lbb])
            k.tt("dve", lbb[:], lbb[:], sm[:], ALU.mult, r=[lbb, sm], w=[lbb])
            k.ts("dve", omlb[:], lbb[:], -1.0, 1.0, ALU.mult, ALU.add, r=[lbb], w=[omlb])
            for h in range(4):
                k.dma(k.dq(), onb[:, h * 128:(h + 1) * 128], on_i[l].partition_broadcast(T_), w=[onb])
            k.memset("pool", mmid[:], 1.0, w=[mmid])
            k.P.op("pool", lambda e: e.affine_select(out=mmid[:], in_=mmid[:], pattern=[[0, T_]], compare_op=ALU.is_ge,
                                                      fill=0.0, base=T_ // 2 - 1, channel_multiplier=-1), reads=[mmid], writes=[mmid])
            for h in range(4):
                k.cp("pool", mask4[:, h, :], triu[0:T_, 0:T_], r=[triu], w=[mask4])
        with k.phase() as st:
            f = lambda n, sh, dt=F32: k.sb(f"h{n}{l}", sh, dt, st)
            pin = [f(f"pin{i}", [T_, 2048]) for i in range(2)]
            sf = f("sf", [T_, 512]); fg = f("fg", [T_, 512]); lf = f("lf", [T_, 512]); kk = f("kk", [T_, 512]); qs = f("qs", [T_, 512])
            cums = f("cums", [T_, 512]); d1 = f("d1", [T_, 512]); d4 = f("d4", [T_, 512]); ex = [f(f"ex{i}", [T_, 512]) for i in range(2)]
            qkb = f("qkb", [T_, 3, 512], BF16)
            ksb = f("ksb", [T_, 512], BF16); vb = f("vb", [T_, 512], BF16)
            qkT = f("qkT", [128, 12, T_], BF16)
            scb = f("scb", [T_, 4, T_], BF16); sct = f("sct", [T_, 4 * T_])
            state = f("state", [128, 4, 128]); stb = f("stb", [128, 4, 128], BF16)
            eL = f("eL", [128, 4])
            osb = f("osb", [T_, 512]); sq = f("sq", [T_, 512]); ss = f("ss", [T_, 8]); sg = f("sg", [T_, 512]); yb = f("yb", [T_, 512], BF16)
            pc = k.psum(f"hpc{l}", [128, 512], F32, st); pm = k.psum(f"hpm{l}", [128, 512], F32, st); pl = k.psum(f"hpl{l}", [128, 512], F32, st)
            pT = k.psum(f"hpT{l}", [128, 16, T_], BF16, st)
            psc = k.psum(f"hpsc{l}", [128, 512], F32, st); po = k.psum(f"hpo{l}", [128, 512], F32, st)
            pst = k.psum(f"hpst{l}", [128, 512], F32, st); pL = k.psum(f"hpL{l}", [128, 512], F32, st)
            idb = c["idb"]
            ones = c["ones"]
            k.memset("dve", state[:], 0.0, w=[state])
            k.memset("dve", stb[:], 0.0, w=[stb])
            nch = k.cfg.get("hg_chunks", S // T_)
            for ch in range(nch):
                tok = slice(ch * T_, (ch + 1) * T_)
                p = pin[ch % 2]
                k.dma(k.dq(), p[:], hg_tm[tok, :], r=[hg_tm], w=[p])
                q_ = p[:, 0:512]; f_ = p[:, 512:1024]; i_ = p[:, 1024:1536]; g_ = p[:, 1536:2048]
                k.act(sf[:], f_, AF.Sigmoid, r=[p], w=[sf])
                k.tt("dve", fg[:], sf[:], omlb[:], ALU.mult, r=[sf, omlb], w=[fg])
                k.tt("dve", fg[:], fg[:], lbb[:], ALU.add, r=[fg, lbb], w=[fg])
                k.act(lf[:], fg[:], AF.Ln, r=[fg], w=[lf])
                k.ts("pool", kk[:], fg[:], -1.0, 1.0, ALU.mult, ALU.add, r=[fg], w=[kk])
                k.act(qs[:], q_, AF.Silu, r=[p], w=[qs])
                k.cp("pool", vb[:], i_, r=[p], w=[vb])
                k.mm(pc[0:T_, :], triu[0:T_, 0:T_], lf[:], True, True, r=[triu, lf], w=[pc])
                k.mm(pm[0:T_, :], mmid[:], lf[:], True, True, r=[mmid, lf], w=[pm])
                k.mm(pl[0:T_, :], ones[0:T_, 0:T_], lf[:], True, True, r=[ones, lf], w=[pl])
                for h in range(4):
                    k.mm(pL[:, h:h + 1], lf[:, h * 128:(h + 1) * 128], ones[0:T_, 0:1], True, True, r=[lf, ones], w=[pL])
                k.cp("act", cums[:], pc[0:T_, :], r=[pc], w=[cums])
                k.tt("dve", d1[:], cums[:], pm[0:T_, :], ALU.subtract, r=[cums, pm], w=[d1])
                k.ts("dve", d1[:], d1[:], 85.0, -85.0, ALU.min, ALU.max, r=[d1], w=[d1])
                k.tt("dve", d4[:], pl[0:T_, :], cums[:], ALU.subtract, r=[pl, cums], w=[d4])
                k.act(ex[0][:], d1[:], AF.Exp, r=[d1], w=[ex[0]])
                k.tt("dve", qkb[:, 0, :], qs[:], ex[0][:], ALU.mult, r=[qs, ex[0]], w=[qkb])
                k.act(ex[1][:], d1[:], AF.Exp, r=[d1], w=[ex[1]], scale=-1.0)
                k.tt("pool", qkb[:, 1, :], kk[:], ex[1][:], ALU.mult, r=[kk, ex[1]], w=[qkb])
                k.act(ex[0][:], cums[:], AF.Exp, r=[cums], w=[ex[0]])
                k.tt("dve", qkb[:, 2, :], qs[:], ex[0][:], ALU.mult, r=[qs, ex[0]], w=[qkb])
                k.act(ex[1][:], d4[:], AF.Exp, r=[d4], w=[ex[1]])
                k.tt("pool", ksb[:], kk[:], ex[1][:], ALU.mult, r=[kk, ex[1]], w=[ksb])
                k.act(eL[:], pL[:, 0:4], AF.Exp, r=[pL], w=[eL])
                for a in range(3):
                    for h in range(4):
                        k.tr(pT[:, a * 4 + h, :], qkb[:, a, h * 128:(h + 1) * 128], idb[0:T_, 0:T_], r=[qkb, idb], w=[pT])
                k.cp("act", qkT[:], pT[:, 0:12, :], r=[pT], w=[qkT])
                for h in range(4):
                    k.mm(psc[0:T_, h * T_:(h + 1) * T_], qkT[:, 4 + h, :], qkT[:, h, :], True, True, r=[qkT], w=[psc])
                k.ts("dve", sct[:], psc[0:T_, 0:4 * T_], -3.0e38, 3.0e38, ALU.max, ALU.min, r=[psc], w=[sct])
                k.tt("dve", scb[:].rearrange("p h t -> p (h t)"), sct[:], mask4[:].rearrange("p h t -> p (h t)"), ALU.mult,
                     r=[sct, mask4], w=[scb])
                for h in range(4):
                    hs = slice(h * 128, (h + 1) * 128)
                    k.mm(po[0:T_, hs], scb[:, h, :], vb[:, hs], True, False, r=[scb, vb], w=[po])
                    k.mm(po[0:T_, hs], qkT[:, 8 + h, :], stb[:, h, :], False, True, r=[qkT, stb], w=[po])
                for h in range(4):
                    hs = slice(h * 128, (h + 1) * 128)
                    k.mm(pst[:, hs], ksb[:, hs], vb[:, hs], True, True, r=[ksb, vb], w=[pst])
                for h in range(4):
                    hs = slice(h * 128, (h + 1) * 128)
                    k.stt(state[:, h, :], state[:, h, :], eL[:, h:h + 1], pst[:, hs], ALU.mult, ALU.add, r=[state, eL, pst], w=[state])
                k.cp("pool", stb[:], state[:], r=[state], w=[stb])
                k.cp("act", osb[:], po[0:T_, :], r=[po], w=[osb])
                if ch == 1:
                    k.dump("h_lf", lf, [T_, 512]); k.dump("h_cums", cums, [T_, 512]); k.dump("h_d1", d1, [T_, 512]); k.dump("h_d4", d4, [T_, 512])
                    k.dump("h_qkb", qkb, [T_, 3, 512], BF16); k.dump("h_qkT", qkT, [128, 12, T_], BF16); k.dump("h_scb", scb, [T_, 4, T_], BF16)
                    k.dump("h_osb", osb, [T_, 512]); k.dump("h_eL", eL, [128, 4]); k.dump("h_state", state, [128, 4, 128]); k.dump("h_ksb", ksb, [T_, 512], BF16)
                k.tt("pool", sq[:], osb[:], osb[:], ALU.mult, r=[osb], w=[sq])
                k.red("dve", ss[:, 0:4], sq[:].rearrange("p (h d) -> p h d", h=4), ALU.add, r=[sq], w=[ss])
                k.ts("dve", ss[:, 0:4], ss[:, 0:4], 1.0 / 128, EPS, ALU.mult, ALU.add, r=[ss], w=[ss])
                k.act(ss[:, 0:4], ss[:, 0:4], AF.Sqrt, r=[ss], w=[ss])
                k.recip(ss[:, 4:8], ss[:, 0:4], r=[ss], w=[ss])
                k.act(sg[:], g_, AF.Silu, r=[p], w=[sg])
                k.tt("pool", sg[:], sg[:], onb[:], ALU.mult, r=[sg, onb], w=[sg])
                for h in range(4):
                    hs = slice(h * 128, (h + 1) * 128)
                    k.stt(yb[:, hs], osb[:, hs], ss[:, 4 + h:5 + h], sg[:, hs], ALU.mult, ALU.mult, r=[osb, ss, sg], w=[yb])
                if ch == 1:
                    k.dump("h_yb", yb, [T_, 512], BF16); k.dump("h_ss", ss, [T_, 8]); k.dump("h_sg", sg, [T_, 512])
                for j in range(4):
                    k.tr(pT[:, j, :], yb[:, j * 128:(j + 1) * 128], idb[0:T_, 0:T_], r=[yb, idb], w=[pT])
                k.cp("act", ysc[:, :, tok], pT[:, 0:4, :], r=[pT], w=[ysc])
        for j in range(4):
            k.dma(k.dq(), ys_fm[0, j * 128:(j + 1) * 128, :], ysc[:, j, :], r=[ysc], w=[ys_fm])


def mix_rwkv7(k, c, l, rk_tm, ys_fm):
    nl = k.nl
    mu_i = k.inp("rk_mu", [nl, 1792])
    w0_i = k.inp("rk_w0", [nl, 512]); a0_i = k.inp("rk_a0", [nl, 512])
    w2_i = k.inp("rk_w2pad", [nl, 128, 512]); a2_i = k.inp("rk_a2pad", [nl, 128, 512]); g2_i = k.inp("rk_g2", [nl, 128, 512])
    kk_i = k.inp("rk_k_k", [nl, 512]); ka_i = k.inp("rk_k_a", [nl, 512]); rk_i = k.inp("rk_r_k_flat", [nl, 512])
    lnw_i = k.inp("rk_ln_w", [nl, 512]); lnb_i = k.inp("rk_ln_b", [nl, 512])
    qW = k.dram(f"rkq_w", [8, S, 64], F32) if l == 0 else k.rkq["w"]
    if l == 0:
        k.rkq = dict(w=qW, nkk=k.dram("rkq_nkk", [8, S, 64], BF16), ka=k.dram("rkq_ka", [8, S, 64], BF16),
                     k2=k.dram("rkq_k2", [8, S, 64], BF16), r=k.dram("rkq_r", [8, S, 64], BF16),
                     g=k.dram("rkq_g", [S, 512], F32), bonus=k.dram("rkq_bonus", [S, 512], F32))
    Q = k.rkq
    idf = c["idf"]; idb = c["idb"]
    nsteps = k.cfg.get("rk_steps", S)
    with k.phase() as st0:
        vT = k.sb(f"rvT{l}", [128, 4, S], F32, st0)
        with k.phase() as st:
            f = lambda n, sh, dt=F32: k.sb(f"r{n}{l}", sh, dt, st)
            mub = f("mub", [128, 1792]); w0b = f("w0b", [128, 512]); a0b = f("a0b", [128, 512])
            kkb = f("kkb", [128, 512]); kab = f("kab", [128, 512]); rkb = f("rkb", [128, 512])
            w2b = f("w2b", [128, 512], BF16); a2b = f("a2b", [128, 512], BF16); g2b = f("g2b", [128, 512], BF16)
            pc = [f(f"pc{i}", [128, 1792]) for i in range(2)]
            pv = [f(f"pv{i}", [128, 1792]) for i in range(2)]
            lob = f("lob", [128, 256], BF16); loT = f("loT", [128, 2, 128], BF16)
            xw = f("xw", [128, 512]); dec = f("dec", [128, 512]); av = f("av", [128, 512]); gsb = f("gsb", [128, 512])
            kkt = f("kkt", [128, 512]); sq = f("sq", [128, 512]); k2 = f("k2", [128, 512]); bon = f("bon", [128, 512])
            ss = f("ss", [128, 24])
            o_nkk = f("onkk", [128, 512], BF16); o_ka = f("oka", [128, 512], BF16); o_k2 = f("ok2", [128, 512], BF16); o_r = f("or", [128, 512], BF16)
            pT = k.psum(f"rpT{l}", [128, 8, 128], BF16, st)
            pw = k.psum(f"rpw{l}", [128, 512], F32, st); pa = k.psum(f"rpa{l}", [128, 512], F32, st); pg = k.psum(f"rpg{l}", [128, 512], F32, st)
            pvt = k.psum(f"rpvt{l}", [128, 512], F32, st)
            for (t_, src_, n_) in ((mub, mu_i, 1792), (w0b, w0_i, 512), (a0b, a0_i, 512), (kkb, kk_i, 512), (kab, ka_i, 512), (rkb, rk_i, 512)):
                k.dma(k.dq(), t_[:], src_[l].partition_broadcast(128), w=[t_])
            k.dma("pool", w2b[:], w2_i[l], w=[w2b])
            k.dma("pool", a2b[:], a2_i[l], w=[a2b])
            k.dma("pool", g2b[:], g2_i[l], w=[g2b])
            for tt in range(16):
                tok = slice(tt * 128, (tt + 1) * 128)
                p = pc[tt % 2]; pr = pv[tt % 2]
                k.dma("sp", p[:], rk_tm[tok, :], r=[rk_tm], w=[p])
                if tt == 0:
                    k.memset("pool", pr[0:1, :], 0.0, w=[pr])
                    k.dma("act", pr[1:128, :], rk_tm[0:127, :], r=[rk_tm], w=[pr])
                else:
                    k.dma("act", pr[:], rk_tm[tt * 128 - 1:tt * 128 + 127, :], r=[rk_tm], w=[pr])
                k.tt("dve", pr[:], pr[:], p[:], ALU.subtract, r=[pr, p], w=[pr])
                k.tt("pool", pr[:], pr[:], mub[:], ALU.mult, r=[pr, mub], w=[pr])
                k.tt("dve", p[:], p[:], pr[:], ALU.add, r=[p, pr], w=[p])
                r_ = p[:, 0:512]; k_ = p[:, 512:1024]; v_ = p[:, 1024:1536]
                k.act(lob[:, 0:64], p[:, 1536:1600], AF.Tanh, r=[p], w=[lob])
                k.cp("act", lob[:, 64:128], p[:, 1600:1664], r=[p], w=[lob])
                k.act(lob[:, 128:256], p[:, 1664:1792], AF.Sigmoid, r=[p], w=[lob])
                k.tr(pT[:, 0, :], lob[:, 0:128], idb[:], r=[lob, idb], w=[pT])
                k.tr(pT[:, 1, :], lob[:, 128:256], idb[:], r=[lob, idb], w=[pT])
                k.cp("act", loT[:], pT[:, 0:2, :], r=[pT], w=[loT])
                k.mm(pw[:], loT[:, 0, :], w2b[:], True, True, r=[loT, w2b], w=[pw])
                k.mm(pa[:], loT[:, 0, :], a2b[:], True, True, r=[loT, a2b], w=[pa])
                k.mm(pg[:], loT[:, 1, :], g2b[:], True, True, r=[loT, g2b], w=[pg])
                k.tt("dve", xw[:], pw[:], w0b[:], ALU.add, r=[pw, w0b], w=[xw])
                k.act(xw[:], xw[:], AF.Exp, r=[xw], w=[xw], scale=-1.0)
                k.act(xw[:], xw[:], AF.Ln, r=[xw], w=[xw], bias=1.0)
                k.act(xw[:], xw[:], AF.Exp, r=[xw], w=[xw], scale=-1.0, bias=-0.5)
                k.act(dec[:], xw[:], AF.Exp, r=[xw], w=[dec], scale=-1.0)
                k.tt("dve", av[:], pa[:], a0b[:], ALU.add, r=[pa, a0b], w=[av])
                k.act(av[:], av[:], AF.Sigmoid, r=[av], w=[av])
                k.cp("act", gsb[:], pg[:], r=[pg], w=[gsb])
                k.tt("pool", kkt[:], k_, kkb[:], ALU.mult, r=[p, kkb], w=[kkt])
                k.tt("pool", sq[:], kkt[:], kkt[:], ALU.mult, r=[kkt], w=[sq])
                k.red("dve", ss[:, 0:8], sq[:].rearrange("p (h d) -> p h d", h=8), ALU.add, r=[sq], w=[ss])
                k.act(ss[:, 0:8], ss[:, 0:8], AF.Sqrt, r=[ss], w=[ss])
                k.ts("dve", ss[:, 0:8], ss[:, 0:8], 1e-12, None, ALU.max, r=[ss], w=[ss])
                k.recip(ss[:, 8:16], ss[:, 0:8], r=[ss], w=[ss])
                for h in range(8):
                    hs = slice(h * 64, (h + 1) * 64)
                    k.ts("dve", kkt[:, hs], kkt[:, hs], ss[:, 8 + h:9 + h], None, ALU.mult, r=[kkt, ss], w=[kkt])
                k.stt(k2[:], av[:], -1.0, kab[:], ALU.add, ALU.mult, r=[av, kab], w=[k2])
                k.stt(k2[:], k2[:], 1.0, k_, ALU.add, ALU.mult, r=[k2, p], w=[k2])
                k.ts("dve", o_nkk[:], kkt[:], -1.0, None, ALU.mult, r=[kkt], w=[o_nkk])
                k.tt("pool", o_ka[:], kkt[:], av[:], ALU.mult, r=[kkt, av], w=[o_ka])
                k.cp("pool", o_k2[:], k2[:], r=[k2], w=[o_k2])
                k.cp("act", o_r[:], r_, r=[p], w=[o_r])
                k.tt("pool", sq[:], r_, k2[:], ALU.mult, r=[p, k2], w=[sq])
                k.tt("pool", sq[:], sq[:], rkb[:], ALU.mult, r=[sq, rkb], w=[sq])
                k.red("dve", ss[:, 16:24], sq[:].rearrange("p (h d) -> p h d", h=8), ALU.add, r=[sq], w=[ss])
                for h in range(8):
                    hs = slice(h * 64, (h + 1) * 64)
                    k.ts("dve", bon[:, hs], p[:, 1024 + h * 64:1024 + (h + 1) * 64], ss[:, 16 + h:17 + h], None, ALU.mult, r=[p, ss], w=[bon])
                for j in range(4):
                    k.tr(pvt[:, j * 128:(j + 1) * 128], p[:, 1024 + j * 128:1024 + (j + 1) * 128], idf[:], r=[p, idf], w=[pvt])
                k.cp("act", vT[:, :, tok], pvt[:].rearrange("p (j t) -> p j t", j=4), r=[pvt], w=[vT])
                hm = lambda q: q.t.rearrange("h t k -> t h k")[tok]
                k.dma("sp", hm(Q["w"]), dec[:].rearrange("p (h d) -> p h d", h=8), r=[dec], w=[Q["w"]])
                k.dma("act", hm(Q["nkk"]), o_nkk[:].rearrange("p (h d) -> p h d", h=8), r=[o_nkk], w=[Q["nkk"]])
                k.dma("sp", hm(Q["ka"]), o_ka[:].rearrange("p (h d) -> p h d", h=8), r=[o_ka], w=[Q["ka"]])
                k.dma("act", hm(Q["k2"]), o_k2[:].rearrange("p (h d) -> p h d", h=8), r=[o_k2], w=[Q["k2"]])
                k.dma("sp", hm(Q["r"]), o_r[:].rearrange("p (h d) -> p h d", h=8), r=[o_r], w=[Q["r"]])
                k.dma("act", Q["g"][tok, :], gsb[:], r=[gsb], w=[Q["g"]])
                k.dma("sp", Q["bonus"][tok, :], bon[:], r=[bon], w=[Q["bonus"]])
        with k.phase() as st1:
            yT = k.sb(f"ryT{l}", [128, 4, S], F32, st1)
            with k.phase() as st:
                TS = 8
                Wb = [k.sb(f"rWb{l}{i}", [128, 4, TS, 64], F32, st) for i in range(2)]
                Nb = [k.sb(f"rNb{l}{i}", [128, 4, TS, 64], BF16, st) for i in range(2)]
                Ab = [k.sb(f"rAb{l}{i}", [128, 4, TS, 64], BF16, st) for i in range(2)]
                Kb = [k.sb(f"rKb{l}{i}", [128, 4, TS, 64], BF16, st) for i in range(2)]
                Rb = [k.sb(f"rRb{l}{i}", [128, 4, TS, 64], BF16, st) for i in range(2)]
                KV = [k.sb(f"rKV{l}{i}", [128, 4, TS, 64], F32, st) for i in range(2)]
                St = k.sb(f"rSt{l}", [128, 4, 64], F32, st)
                tmpA = k.sb(f"rtmpA{l}", [128, 4, 64], F32, st)
                tmpB = k.sb(f"rtmpB{l}", [128, 4, 64], F32, st)
                tmpC = k.sb(f"rtmpC{l}", [128, 4, 64], F32, st)
                pend = None
                sa = k.sb(f"rsa{l}", [128, 4], F32, st)
                k.memset("dve", St[:], 0.0, w=[St])
                if nsteps < S:
                    k.memset("pool", yT[:], 0.0, w=[yT])
                for cch in range(nsteps // TS):
                    t0 = cch * TS
                    b = cch % 2
                    for (buf, qn) in ((Wb, "w"), (Nb, "nkk"), (Ab, "ka"), (Kb, "k2"), (Rb, "r")):
                        if k.cfg.get("rk_nodma") and cch >= 2:
                            continue
                        for hp in range(2):
                            srcap = Q[qn].t.rearrange("(j hp) t k -> hp j t k", hp=2)[hp, :, t0:t0 + TS, :]
                            k.dma(k.dq(), buf[b][hp * 64:(hp + 1) * 64], srcap.partition_broadcast(64), r=[Q[qn]], w=[buf[b]])
                    k.tt("pool", KV[b][:], Kb[b][:], vT[:, :, t0:t0 + TS].unsqueeze(3).to_broadcast([128, 4, TS, 64]), ALU.mult,
                         r=[Kb[b], vT], w=[KV[b]])
                    for ti in range(TS):
                        t = t0 + ti
                        N_ = Nb[b]; W_ = Wb[b]; A_ = Ab[b]; KV_ = KV[b]; R_ = Rb[b]
                        RX = dict(relaxed=True)
                        opa = lambda N_=N_, ti=ti: k.P.op("dve", lambda e: e.tensor_tensor(out=tmpA[:], in0=St[:], in1=N_[:, :, ti, :], op=ALU.mult),
                                                           reads=[St.b, N_.b], writes=[tmpA.b], **RX)
                        opb = lambda W_=W_, ti=ti: k.P.op("dve", lambda e: e.tensor_tensor(out=St[:], in0=St[:], in1=W_[:, :, ti, :], op=ALU.mult),
                                                           reads=[St.b, W_.b], writes=[St.b], **RX)
                        opc = lambda: k.P.op("dve", lambda e: e.tensor_reduce(out=sa[:], in_=tmpA[:], axis=AX.X, op=ALU.add),
                                             reads=[tmpA.b], writes=[sa.b], **RX)
                        opd = lambda KV_=KV_, ti=ti: k.P.op("dve", lambda e: e.tensor_tensor(out=St[:], in0=St[:], in1=KV_[:, :, ti, :], op=ALU.add),
                                                             reads=[St.b, KV_.b], writes=[St.b], **RX)
                        ope = lambda A_=A_, ti=ti: k.P.op("dve", lambda e: e.tensor_tensor(out=tmpB[:], in0=A_[:, :, ti, :],
                                                                                          in1=sa[:].unsqueeze(2).to_broadcast([128, 4, 64]), op=ALU.mult),
                                                           reads=[A_.b, sa.b], writes=[tmpB.b], **RX)
                        opf = lambda: k.P.op("dve", lambda e: e.tensor_tensor(out=St[:], in0=St[:], in1=tmpB[:], op=ALU.add),
                                             reads=[St.b, tmpB.b], writes=[St.b], **RX)
                        opg = lambda R_=R_, ti=ti: k.P.op("dve", lambda e: e.tensor_tensor(out=tmpC[:], in0=St[:], in1=R_[:, :, ti, :], op=ALU.mult),
                                                           reads=[St.b, R_.b], writes=[tmpC.b], **RX)
                        oph = lambda t=t: k.P.op("dve", lambda e: e.tensor_reduce(out=yT[:, :, t], in_=tmpC[:], axis=AX.X, op=ALU.add),
                                                 reads=[tmpC.b], writes=[yT.b], **RX)
                        opa()
                        if pend is not None:
                            pend[0]()
                        opb(); opc(); opd(); ope()
                        if pend is not None:
                            pend[1]()
                        opf()
                        pend = (opg, oph)
                if pend is not None:
                    pend[0]()
                    pend[1]()
            with k.phase() as st:
                f = lambda n, sh, dt=F32: k.sb(f"q{n}{l}", sh, dt, st)
                lnw = f("lnw", [128, 512]); lnb = f("lnb", [128, 512])
                ysb = f("ysb", [128, 512]); sq = f("sq", [128, 512]); ss = f("ss", [128, 24]); gt = f("gt", [128, 512]); bt = f("bt", [128, 512])
                ynb = f("ynb", [128, 512], BF16)
                ysc = f("ysc", [128, 4, S], BF16)
                py = k.psum(f"qpy{l}", [128, 512], F32, st)
                pT = k.psum(f"qpT{l}", [128, 8, 128], BF16, st)
                k.dma("sp", lnw[:], lnw_i[l].partition_broadcast(128), w=[lnw])
                k.dma("act", lnb[:], lnb_i[l].partition_broadcast(128), w=[lnb])
                for tt in range(16):
                    tok = slice(tt * 128, (tt + 1) * 128)
                    for j in range(4):
                        k.tr(py[:, j * 128:(j + 1) * 128], yT[:, j, tok], idf[:], r=[yT, idf], w=[py])
                    k.cp("act", ysb[:], py[:], r=[py], w=[ysb])
                    k.dma("sp", gt[:], Q["g"][tok, :], r=[Q["g"]], w=[gt])
                    k.dma("act", bt[:], Q["bonus"][tok, :], r=[Q["bonus"]], w=[bt])
                    k.red("dve", ss[:, 0:8], ysb[:].rearrange("p (h d) -> p h d", h=8), ALU.add, r=[ysb], w=[ss])
                    k.ts("dve", ss[:, 0:8], ss[:, 0:8], -1.0 / 64, None, ALU.mult, r=[ss], w=[ss])
                    for h in range(8):
                        hs = slice(h * 64, (h + 1) * 64)
                        k.ts("dve", ysb[:, hs], ysb[:, hs], ss[:, h:h + 1], None, ALU.add, r=[ysb, ss], w=[ysb])
                    k.tt("pool", sq[:], ysb[:], ysb[:], ALU.mult, r=[ysb], w=[sq])
                    k.red("dve", ss[:, 8:16], sq[:].rearrange("p (h d) -> p h d", h=8), ALU.add, r=[sq], w=[ss])
                    k.ts("dve", ss[:, 8:16], ss[:, 8:16], 1.0 / 64, 64e-5, ALU.mult, ALU.add, r=[ss], w=[ss])
                    k.act(ss[:, 8:16], ss[:, 8:16], AF.Sqrt, r=[ss], w=[ss])
                    k.recip(ss[:, 16:24], ss[:, 8:16], r=[ss], w=[ss])
                    for h in range(8):
                        hs = slice(h * 64, (h + 1) * 64)
                        k.stt(ysb[:, hs], ysb[:, hs], ss[:, 16 + h:17 + h], lnw[:, hs], ALU.mult, ALU.mult, r=[ysb, ss, lnw], w=[ysb])
                    k.tt("pool", bt[:], bt[:], lnb[:], ALU.add, r=[bt, lnb], w=[bt])
                    k.tt("dve", ysb[:], ysb[:], bt[:], ALU.add, r=[ysb, bt], w=[ysb])
                    k.tt("dve", ynb[:], ysb[:], gt[:], ALU.mult, r=[ysb, gt], w=[ynb])
                    for j in range(4):
                        k.tr(pT[:, j, :], ynb[:, j * 128:(j + 1) * 128], idb[:], r=[ynb, idb], w=[pT])
                    k.cp("act", ysc[:, :, tok], pT[:, 0:4, :], r=[pT], w=[ysc])
                for j in range(4):
                    k.dma(k.dq(), ys_fm[3, j * 128:(j + 1) * 128, :], ysc[:, j, :], r=[ysc], w=[ys_fm])


def mixers(k, c, l, src, ys_fm):
    which = k.cfg.get("mixers", "abcd")
    if "a" in which:
        mix_hgrn2(k, c, l, src["hg_tm"], ys_fm)
    if "b" in which:
        mix_s5(k, c, l, src["s5_fm"], ys_fm)
    if "d" in which:
        mix_rwkv7(k, c, l, src["rk_tm"], ys_fm)
    if "c" in which:
        mix_ssd(k, c, l, src["m2z_tm"], src["m2x_fm"], src["m2dt_tm"], ys_fm)


def col_layout(v, ncol):
    sh = v.shape[:-1]
    return np.ascontiguousarray(v.reshape(*sh, ncol, 128).swapaxes(-1, -2))


def host_inputs(inp, b):
    m = {}
    m["x"] = np.ascontiguousarray(inp["x"][b])
    m["c_col"] = col_layout(inp["c"][b], 16)
    if "w_mod" in inp:
        m["w_mod"] = inp["w_mod"]
        m["b_mod_col"] = col_layout(inp["b_mod"], 96)
        m["g_mix_col"] = col_layout(inp["g_norm_mix"], 16)
        m["g_ffn_col"] = col_layout(inp["g_norm_ffn"], 16)
    if "w_in" in inp:
        m["w_in"] = inp["w_in"]
    for n in ("w_branch", "w_out", "w_router", "b_router", "w_gu", "w_down", "b_down", "g_final"):
        if n in inp:
            m[n] = inp[n]
    if "b_gu" in inp:
        bg = inp["b_gu"]
        m["b_gu_col"] = np.ascontiguousarray(bg.reshape(bg.shape[0], NE, 12, 128).transpose(0, 3, 1, 2).reshape(bg.shape[0], 128, NE * 12))
    if "ys_fm" in inp:
        m["ys_fm"] = inp["ys_fm"]
    if "rk_mu" in inp:
        nl_ = inp["rk_mu"].shape[0]
        for n in ("rk_mu", "rk_w0", "rk_a0", "rk_g2", "rk_k_k", "rk_k_a", "rk_ln_w", "rk_ln_b"):
            m[n] = inp[n]
        m["rk_r_k_flat"] = np.ascontiguousarray(inp["rk_r_k"].reshape(nl_, 512))
        w2p = np.zeros((nl_, 128, 512), np.float32); w2p[:, 0:64] = inp["rk_w2"]
        a2p = np.zeros((nl_, 128, 512), np.float32); a2p[:, 64:128] = inp["rk_a2"]
        m["rk_w2pad"] = w2p; m["rk_a2pad"] = a2p
    if "hg_lower_bound" in inp:
        m["hg_lower_bound"] = inp["hg_lower_bound"]
        m["hg_onorm"] = inp["hg_onorm"]
    if "m2_conv_w" in inp:
        cw = inp["m2_conv_w"]
        nl_ = cw.shape[0]
        m["m2_convw_col"] = np.ascontiguousarray(cw.reshape(nl_, 4, 8, 128).transpose(0, 3, 2, 1))
        m["m2_convb_col"] = col_layout(inp["m2_conv_b"], 8)
        for n in ("m2_dt_bias", "m2_a_log", "m2_d", "m2_norm"):
            m[n] = inp[n]
    if "s5_lambda_re" in inp:
        nl = inp["s5_lambda_re"].shape[0]
        def stcol(a):
            return np.ascontiguousarray(a.reshape(nl, 16, 2, 64).transpose(0, 2, 3, 1).reshape(nl, 128, 16))
        m["s5_lr_col"] = stcol(inp["s5_lambda_re"])
        m["s5_li_col"] = stcol(inp["s5_lambda_im"])
        m["s5_ldt_col"] = stcol(np.broadcast_to(inp["s5_log_dt"][:, :, None], (nl, 32, 64)))
        def bT(b):
            o = np.zeros((nl, 16, 128, 128), np.float32)
            for g in range(32):
                st_, g2 = g // 2, g % 2
                gl = g % 8
                o[:, st_, gl * 16:(gl + 1) * 16, g2 * 64:(g2 + 1) * 64] = b[:, g].transpose(0, 2, 1)
            return o
        def cT(cc):
            o = np.zeros((nl, 16, 128, 128), np.float32)
            for g in range(32):
                st_, g2 = g // 2, g % 2
                gl = g % 8
                o[:, st_, g2 * 64:(g2 + 1) * 64, gl * 16:(gl + 1) * 16] = cc[:, g].transpose(0, 2, 1)
            return o
        m["s5_bT_re"] = bT(inp["s5_b_re"]); m["s5_bT_im"] = bT(inp["s5_b_im"])
        m["s5_cT_re"] = cT(inp["s5_c_re"]); m["s5_cT_im"] = cT(inp["s5_c_im"])
        m["s5_d_col"] = col_layout(inp["s5_d"], 4)
        m["s5_w_glu"] = inp["s5_w_glu"]
        m["s5_bglu_col"] = col_layout(inp["s5_b_glu"], 4)
    return m


def run(inputs, cfg, cores):
    nc = bass.Bass("TRN2", target_bir_lowering=False)
    with contextlib.ExitStack() as stack:
        k = build(nc, stack, cfg)
    maps = []
    for ci in range(cores):
        full = host_inputs(inputs, ci % 4)
        nl = cfg.get("nl", L)
        maps.append({n: np.ascontiguousarray(full[n]) for n in k.inputs})
    res = run_bass_kernel_spmd(nc, maps, core_ids=list(range(cores)))
    return res, k


def kernel(**inputs):
    inputs = {n: np.asarray(v) for n, v in inputs.items()}
    res, k = run(inputs, {}, 4)
    out = np.stack([res.results[b]["y"] for b in range(4)], axis=0)
    return out.astype(np.float32)
```

```python
import contextlib
import numpy as np
import concourse.bass as bass
import concourse.mybir as mybir
from concourse.bass_utils import run_bass_kernel_spmd

F32 = mybir.dt.float32
BF16 = mybir.dt.bfloat16
U32 = mybir.dt.uint32
I32 = mybir.dt.int32
ALU = mybir.AluOpType
AF = mybir.ActivationFunctionType
AX = mybir.AxisListType

D = 2048
S = 2048
L = 4
NB = 4
BR = 512
N_IN = 14088
O1 = 8192
O_HG = 8192
O_S5 = 10240
O_M2 = 10752
O_RK = 12296
NE = 32
DE = 768
EPS = 1e-6


class Buf:
    __slots__ = ("w", "r", "name")

    def __init__(self, name=""):
        self.w = None
        self.r = {}
        self.name = name


class T:
    def __init__(self, t, name=""):
        self.t = t
        self.b = Buf(name)

    def __getitem__(self, k):
        return self.t[k]


def _bufs(lst):
    out = []
    for x in lst:
        if x is None:
            continue
        out.append(x.b if isinstance(x, T) else x)
    return out


class Prog:
    CE = ("pe", "act", "dve", "pool")
    ENG = ("pe", "act", "dve", "pool", "sp")

    def __init__(self, nc, stack, ndma=20):
        self.nc = nc
        self.stack = stack
        self.q = {e: [] for e in self.ENG}
        self.cnt = {e: 0 for e in self.CE}
        self.sems = []
        self.own = {}
        for e in self.CE:
            self.own[e] = self._newsem("own_" + e)
        self.known = {e: {} for e in self.ENG}
        self.dpool = {}
        self.drr = {}
        for qn in ("sp", "act", "pool"):
            self.dpool[qn] = [[self._newsem(f"d_{qn}{i}"), 0] for i in range(ndma)]
            self.drr[qn] = 0
        self.n_ops = 0

    def _newsem(self, name):
        s = self.stack.enter_context(self.nc.semaphore(name))
        self.sems.append(s)
        return len(self.sems) - 1

    def _deps(self, eng, reads, writes, relaxed=False):
        deps = {}
        own = self.own.get(eng, -1)

        def add(s, v):
            if deps.get(s, 0) < v:
                deps[s] = v
        for b in reads:
            if b.w is not None:
                add(*b.w)
        for b in writes:
            if b.w is not None:
                add(*b.w)
            for s, v in b.r.items():
                if relaxed and s == own:
                    continue
                add(s, v)
        waits = []
        kn = self.known[eng]
        for s, v in deps.items():
            if eng in self.own and s == self.own[eng]:
                if eng == "pe":
                    continue
                if relaxed and v < self.cnt[eng]:
                    continue
                if v < self.cnt[eng] - 1:
                    continue
            if kn.get(s, 0) >= v:
                continue
            kn[s] = v
            waits.append((s, v))
        return waits

    def _mark(self, ev, reads, writes):
        s, v = ev
        for b in reads:
            if b.r.get(s, 0) < v:
                b.r[s] = v
        for b in writes:
            b.w = ev
            b.r = {}

    def op(self, eng, fn, reads=(), writes=(), relaxed=False):
        reads = _bufs(reads)
        writes = _bufs(writes)
        waits = self._deps(eng, reads, writes, relaxed)
        self.cnt[eng] += 1
        ev = (self.own[eng], self.cnt[eng])
        self.q[eng].append((waits, fn, ev[0], 1))
        self._mark(ev, reads, writes)
        self.n_ops += 1
        return ev

    def _dmaev(self, qn, waits):
        pool = self.dpool[qn]
        i = self.drr[qn]
        self.drr[qn] = (i + 1) % len(pool)
        s, tot = pool[i]
        kn = self.known[qn]
        if kn.get(s, 0) < tot:
            kn[s] = tot
            waits.append((s, tot))
        pool[i][1] = tot + 16
        return (s, tot + 16)

    def dma(self, qn, out, in_, reads=(), writes=(), **kw):
        reads = _bufs(reads)
        writes = _bufs(writes)
        waits = self._deps(qn, reads, writes)
        ev = self._dmaev(qn, waits)

        def fn(e, out=out, in_=in_, kw=kw):
            return e.dma_start(out=out, in_=in_, **kw)
        self.q[qn].append((waits, fn, ev[0], 16))
        self._mark(ev, reads, writes)
        self.n_ops += 1
        return ev

    def custom(self, qn, fn, reads=(), writes=()):
        reads = _bufs(reads)
        writes = _bufs(writes)
        waits = self._deps(qn, reads, writes)
        ev = self._dmaev(qn, waits)
        self.q[qn].append((waits, fn, ev[0], 16))
        self._mark(ev, reads, writes)
        return ev

    def barrier(self):
        tot = []
        for qn, pool in self.dpool.items():
            for s, t in pool:
                if t > 0:
                    tot.append((s, t))
        for e in self.ENG:
            waits = []
            kn = self.known[e]
            for e2 in self.CE:
                if e2 != e and self.cnt[e2] > 0:
                    s, v = self.own[e2], self.cnt[e2]
                    if kn.get(s, 0) < v:
                        kn[s] = v
                        waits.append((s, v))
            for s, v in tot:
                if kn.get(s, 0) < v:
                    kn[s] = v
                    waits.append((s, v))
            if waits:
                self.q[e].append((waits, None, None, 0))

    def finish(self):
        waits = []
        for qn, pool in self.dpool.items():
            for s, tot in pool:
                if tot > 0:
                    waits.append((s, tot))
        self.q["sp"].append((waits, None, None, 0))
        w2 = [(self.own[e], self.cnt[e]) for e in self.CE if self.cnt[e] > 0]
        self.q["sp"].append((w2, None, None, 0))

    def emit(self):
        nc = self.nc
        sems = self.sems
        q = self.q
        with nc.Block() as block:
            def run(e, lst):
                for waits, fn, s, inc in lst:
                    for ws, wv in waits:
                        e.wait_ge(sems[ws], wv)
                    if fn is None:
                        continue
                    ins = fn(e)
                    ins.then_inc(sems[s], inc)

            @block.tensor
            def _(e):
                run(e, q["pe"])

            @block.scalar
            def _(e):
                run(e, q["act"])

            @block.vector
            def _(e):
                run(e, q["dve"])

            @block.gpsimd
            def _(e):
                run(e, q["pool"])

            @block.sync
            def _(e):
                run(e, q["sp"])


class KB:
    def __init__(self, nc, stack, cfg):
        self.nc = nc
        self.st = stack
        self.cfg = cfg
        self.P = Prog(nc, stack)
        self.inputs = {}
        self.outs = {}
        self.rr = 0
        self.dbg = cfg.get("dbg", ())

    def inp(self, name, shape, dtype=F32):
        if name not in self.inputs:
            self.inputs[name] = T(self.nc.dram_tensor(name, list(shape), dtype, kind="ExternalInput").ap(), name)
        return self.inputs[name]

    def dram(self, name, shape, dtype=F32):
        if name in self.cfg.get("scratch_in", ()):
            return self.inp(name, shape, dtype)
        kind = "ExternalOutput" if name in self.dbg else "Internal"
        t = T(self.nc.dram_tensor(name, list(shape), dtype, kind=kind).ap(), name)
        if name in self.dbg:
            self.outs[name] = t
        return t

    def sb(self, name, shape, dtype=F32, stack=None):
        st = stack or self.st
        return T(st.enter_context(self.nc.sbuf_tensor(name, list(shape), dtype)), name)

    def psum(self, name, shape, dtype=F32, stack=None):
        st = stack or self.st
        return T(st.enter_context(self.nc.psum_tensor(name, list(shape), dtype)), name)

    @contextlib.contextmanager
    def phase(self):
        with contextlib.ExitStack() as st:
            yield st
        self.P.barrier()

    def dump(self, name, tile, shape, dtype=F32, ap=None):
        if name not in self.dbg:
            return
        if name not in self.outs:
            self.outs[name] = T(self.nc.dram_tensor(name, list(shape), dtype, kind="ExternalOutput").ap(), name)
        d = self.outs[name]
        self.dma("sp", d[:], tile[:] if ap is None else ap, r=[tile], w=[d])

    def dq(self):
        self.rr += 1
        return ("sp", "act")[self.rr % 2]

    def mm(self, out, lhsT, rhs, start, stop, r=(), w=()):
        return self.P.op("pe", lambda e: e.matmul(out, lhsT=lhsT, rhs=rhs, start=start, stop=stop), reads=r, writes=w)

    def tr(self, out, in_, ident, r=(), w=()):
        return self.P.op("pe", lambda e: e.transpose(out=out, in_=in_, identity=ident), reads=r, writes=w)

    def act(self, out, in_, func, r=(), w=(), bias=None, scale=None, accum=None, eng="act"):
        kw = {}
        if bias is not None:
            kw["bias"] = bias
        if scale is not None:
            kw["scale"] = scale
        if accum is not None:
            kw["accum_out"] = accum
        return self.P.op("act", lambda e: e.activation(out=out, in_=in_, func=func, **kw), reads=r, writes=w)

    def ts(self, eng, out, in0, s1, s2, op0, op1=None, r=(), w=(), accum=None):
        kw = {}
        if accum is not None:
            kw["accum_out"] = accum
        if op1 is None:
            return self.P.op(eng, lambda e: e.tensor_scalar(out=out, in0=in0, scalar1=s1, scalar2=None, op0=op0, **kw), reads=r, writes=w)
        return self.P.op(eng, lambda e: e.tensor_scalar(out=out, in0=in0, scalar1=s1, scalar2=s2, op0=op0, op1=op1, **kw), reads=r, writes=w)

    def tt(self, eng, out, in0, in1, op, r=(), w=()):
        return self.P.op(eng, lambda e: e.tensor_tensor(out=out, in0=in0, in1=in1, op=op), reads=r, writes=w)

    def stt(self, out, in0, scalar, in1, op0, op1, r=(), w=(), accum=None):
        kw = {}
        if accum is not None:
            kw["accum_out"] = accum
        return self.P.op("dve", lambda e: e.scalar_tensor_tensor(out=out, in0=in0, scalar=scalar, in1=in1, op0=op0, op1=op1, **kw), reads=r, writes=w)

    def cp(self, eng, out, in_, r=(), w=()):
        if eng == "act":
            return self.P.op("act", lambda e: e.copy(out=out, in_=in_), reads=r, writes=w)
        return self.P.op(eng, lambda e: e.tensor_copy(out=out, in_=in_), reads=r, writes=w)

    def memset(self, eng, ap, val, w=()):
        return self.P.op(eng, lambda e: e.memset(ap, val), writes=w)

    def red(self, eng, out, in_, op, r=(), w=(), axis=AX.X):
        return self.P.op(eng, lambda e: e.tensor_reduce(out=out, in_=in_, axis=axis, op=op), reads=r, writes=w)

    def recip(self, out, in_, r=(), w=()):
        return self.P.op("dve", lambda e: e.reciprocal(out=out, in_=in_), reads=r, writes=w)

    def dma(self, q, out, in_, r=(), w=(), **kw):
        return self.P.dma(q, out, in_, reads=r, writes=w, **kw)


def build_consts(k):
    c = {}
    idf = k.sb("idf", [128, 128], F32)
    k.memset("pool", idf[:], 1.0, w=[idf])
    k.P.op("pool", lambda e: e.affine_select(out=idf[:], in_=idf[:], pattern=[[-1, 128]], compare_op=ALU.is_equal,
                                              fill=0.0, base=0, channel_multiplier=1), reads=[idf], writes=[idf])
    idb = k.sb("idb", [128, 128], BF16)
    k.cp("dve", idb[:], idf[:], r=[idf], w=[idb])
    c["idf"] = idf
    c["idb"] = idb
    triu = k.sb("triu", [128, 128], F32)
    k.memset("pool", triu[:], 1.0, w=[triu])
    k.P.op("pool", lambda e: e.affine_select(out=triu[:], in_=triu[:], pattern=[[1, 128]], compare_op=ALU.is_ge,
                                              fill=0.0, base=0, channel_multiplier=-1), reads=[triu], writes=[triu])
    c["triu"] = triu
    ones = k.sb("onesf", [128, 128], F32)
    k.memset("pool", ones[:], 1.0, w=[ones])
    c["ones"] = ones
    onesb = k.sb("onesb", [128, 128], BF16)
    k.memset("pool", onesb[:], 1.0, w=[onesb])
    c["onesb"] = onesb
    return c


def stage_mod(k, c, nl):
    nc = k.nc
    cin = k.inp("c_col", [128, 16])
    w_mod = k.inp("w_mod", [nl, D, 6 * D])
    b_mod = k.inp("b_mod_col", [nl, 128, 96])
    gmix = k.inp("g_mix_col", [nl, 128, 16])
    gffn = k.inp("g_ffn_col", [nl, 128, 16])
    condc = k.sb("condc", [128, 16], F32)
    k.dma("sp", condc[:], cin[:], w=[condc])
    k.act(condc[:], condc[:], AF.Silu, r=[condc], w=[condc])
    modc = k.sb("modc", [128, L, 96], F32)
    gsa = k.sb("gsa", [128, L, 16], F32)
    gsm = k.sb("gsm", [128, L, 16], F32)
    gt_scr = k.dram("gt_scr", [L, 2, 16, 128])
    with k.phase() as st:
        wb = [k.sb(f"wmodbuf{i}", [128, 16, 768], F32, st) for i in range(2)]
        pm = k.psum("ps_mod", [128, 512], F32, st)
        pt = k.psum("ps_modT", [128, 512], F32, st)
        bt = k.sb("bmodt", [128, 96], F32, st)
        gtmp = k.sb("gtmp", [128, 16], F32, st)
        rowt = k.sb("rowt", [16, 128], F32, st)
        it = 0
        for l in range(nl):
            for cb in range(16):
                w = wb[it % 2]
                it += 1
                k.dma(k.dq(), w[:], w_mod[l, :, cb * 768:(cb + 1) * 768].rearrange("(kc p) n -> p kc n", p=128), w=[w])
                for m in range(6):
                    col = cb * 6 + m
                    for kc in range(16):
                        k.mm(pm[:, col:col + 1], w[:, kc, m * 128:(m + 1) * 128], condc[:, kc:kc + 1],
                             kc == 0, kc == 15, r=[w, condc], w=[pm])
            k.dma("sp", bt[:], b_mod[l], w=[bt])
            k.tt("dve", modc[:, l, :], pm[:, 0:96], bt[:], ALU.add, r=[pm, bt], w=[modc])
            for (dst, gsrc, off) in ((gsa, gmix, 16), (gsm, gffn, 64)):
                k.dma("sp", gtmp[:], gsrc[l], w=[gtmp])
                k.stt(dst[:, l, :], modc[:, l, off:off + 16], 1.0, gtmp[:], ALU.add, ALU.mult, r=[modc, gtmp], w=[dst])
            for gi, off in enumerate((32, 80)):
                k.tr(pt[0:16, 0:128], modc[:, l, off:off + 16], c["idf"][:], r=[modc, c["idf"]], w=[pt])
                k.cp("dve", rowt[:], pt[0:16, 0:128], r=[pt], w=[rowt])
                k.dma("sp", gt_scr[l, gi], rowt[:], r=[rowt], w=[gt_scr])
    return dict(modc=modc, gsa=gsa, gsm=gsm, gt_scr=gt_scr)


def stage_norm(k, c, xres, hT, gsT, l, modc, shoff, tag):
    with k.phase() as st:
        xt = [k.sb(f"nx{tag}{i}", [128, D], F32, st) for i in range(2)]
        xn = [k.sb(f"nxn{tag}{i}", [128, D], BF16, st) for i in range(2)]
        sq = k.sb(f"nsq{tag}", [128, D], BF16, st)
        ss = [k.sb(f"nss{tag}{i}", [128, 4], F32, st) for i in range(2)]
        pt = [k.psum(f"npt{tag}{i}", [128, 4, 128], BF16, st) for i in range(2)]
        for tt in range(S // 128):
            x = xt[tt % 2]
            n = xn[tt % 2]
            s = ss[tt % 2]
            k.dma(k.dq(), x[:], xres[tt * 128:(tt + 1) * 128, :], r=[xres], w=[x])
            k.act(sq[:], x[:], AF.Square, r=[x], w=[sq, s], accum=s[:, 0:1])
            k.ts("dve", s[:, 1:2], s[:, 0:1], 1.0 / D, EPS, ALU.mult, ALU.add, r=[s], w=[s])
            k.act(s[:, 2:3], s[:, 1:2], AF.Sqrt, r=[s], w=[s])
            k.recip(s[:, 3:4], s[:, 2:3], r=[s], w=[s])
            k.act(n[:], x[:], AF.Copy, r=[x, s], w=[n], scale=s[:, 3:4])
            for j4 in range(4):
                p = pt[j4 % 2]
                for jj in range(4):
                    j = j4 * 4 + jj
                    k.tr(p[:, jj, :], n[:, j * 128:(j + 1) * 128], c["idb"][:], r=[n, c["idb"]], w=[p])
                for jj in range(4):
                    j = j4 * 4 + jj
                    eng = "dve" if jj % 2 == 0 else "pool"
                    if eng == "pool":
                        k.act(hT[:, j, tt * 128:(tt + 1) * 128], p[:, jj, :], AF.Identity, r=[p, gsT, modc], w=[hT],
                              scale=gsT[:, l, j:j + 1], bias=modc[:, l, shoff + j:shoff + j + 1])
                    else:
                        k.ts("dve", hT[:, j, tt * 128:(tt + 1) * 128], p[:, jj, :], gsT[:, l, j:j + 1],
                             modc[:, l, shoff + j:shoff + j + 1], ALU.mult, ALU.add, r=[p, gsT, modc], w=[hT])


def proj_tm(k, hT, wsrc, c0, ncols, dst, st, tag):
    wb = [k.sb(f"pw{tag}{i}", [128, 16, 512], BF16, st) for i in range(2)]
    ob = [k.sb(f"po{tag}{i}", [128, 512], F32, st) for i in range(3)]
    ps = [k.psum(f"pp{tag}{i}", [128, 512], F32, st) for i in range(2)]
    it = 0
    io = 0
    for b0 in range(0, ncols, 512):
        nb = min(512, ncols - b0)
        w = wb[it % 2]
        it += 1
        k.dma("pool", w[:, :, 0:nb], wsrc[:, c0 + b0:c0 + b0 + nb].rearrange("(kc p) n -> p kc n", p=128), w=[w])
        for tt in range(S // 128):
            p = ps[tt % 2]
            for kc in range(16):
                k.mm(p[:, 0:nb], hT[:, kc, tt * 128:(tt + 1) * 128], w[:, kc, 0:nb], kc == 0, kc == 15, r=[hT, w], w=[p])
            o = ob[io % 3]
            io += 1
            if io % 2 == 0:
                k.cp("dve", o[:, 0:nb], p[:, 0:nb], r=[p], w=[o])
            else:
                k.cp("act", o[:, 0:nb], p[:, 0:nb], r=[p], w=[o])
            k.dma(k.dq(), dst[tt * 128:(tt + 1) * 128, b0:b0 + nb], o[:, 0:nb], r=[o], w=[dst])


def proj_fm(k, hT, wsrc, c0, ncols, dst, st, tag):
    wb = [k.sb(f"fw{tag}{i}", [128, 16, 128], BF16, st) for i in range(2)]
    ob = [k.sb(f"fo{tag}{i}", [128, S], F32, st) for i in range(2)]
    ps = [k.psum(f"fp{tag}{i}", [128, 512], F32, st) for i in range(2)]
    assert ncols % 128 == 0
    ip = 0
    for m in range(ncols // 128):
        w = wb[m % 2]
        k.dma("pool", w[:], wsrc[:, c0 + m * 128:c0 + (m + 1) * 128].rearrange("(kc p) n -> p kc n", p=128), w=[w])
        o = ob[m % 2]
        for n in range(4):
            p = ps[ip % 2]
            ip += 1
            for kc in range(16):
                k.mm(p[:], w[:, kc, :], hT[:, kc, n * 512:(n + 1) * 512], kc == 0, kc == 15, r=[hT, w], w=[p])
            if ip % 2 == 0:
                k.cp("dve", o[:, n * 512:(n + 1) * 512], p[:], r=[p], w=[o])
            else:
                k.cp("act", o[:, n * 512:(n + 1) * 512], p[:], r=[p], w=[o])
        k.dma(k.dq(), dst[m * 128:(m + 1) * 128, :], o[:], r=[o], w=[dst])


def stage_merge(k, c, hT, l, w_in, w_branch, w_out, ys_fm, mods, y):
    mergedT = None
    with k.phase() as st0:
        mergedT = k.sb(f"mergedT{l}", [128, 16, S], BF16, st0)
        with k.phase() as st:
            wg = [k.sb(f"mwg{l}{i}", [128, 16, 128], BF16, st) for i in range(2)]
            wbr = [k.sb(f"mwb{l}{i}", [128, 4, 128], BF16, st) for i in range(2)]
            ysb = [k.sb(f"mys{l}{i}", [128, 4, S], BF16, st) for i in range(2)]
            macc = k.sb(f"macc{l}", [128, S], F32, st)
            sg = [k.sb(f"msg{l}{i}", [128, 512], F32, st) for i in range(2)]
            tmp = [k.sb(f"mtmp{l}{i}", [128, 512], F32, st) for i in range(2)]
            pg = [k.psum(f"mpg{l}{i}", [128, 512], F32, st) for i in range(2)]
            pz = [k.psum(f"mpz{l}{i}", [128, 512], F32, st) for i in range(2)]
            it = 0
            for j in range(16):
                for n in range(4):
                    w1 = wg[it % 2]
                    w2 = wbr[it % 2]
                    yb = ysb[it % 2]
                    k.dma("pool", w1[:], w_in[l, :, n * 2048 + j * 128:n * 2048 + (j + 1) * 128].rearrange("(kc p) n -> p kc n", p=128), w=[w1])
                    k.dma("pool", w2[:], w_branch[l, n, :, j * 128:(j + 1) * 128].rearrange("(kc p) n -> p kc n", p=128), w=[w2])
                    k.dma(k.dq(), yb[:], ys_fm[n].rearrange("(kc p) s -> p kc s", p=128), r=[ys_fm], w=[yb])
                    for t in range(4):
                        sl = slice(t * 512, (t + 1) * 512)
                        g = pg[it % 2]
                        z = pz[it % 2]
                        sgt = sg[it % 2]
                        tm = tmp[it % 2]
                        it += 1
                        for kc in range(16):
                            k.mm(g[:], w1[:, kc, :], hT[:, kc, sl], kc == 0, kc == 15, r=[w1, hT], w=[g])
                        for kc in range(4):
                            k.mm(z[:], w2[:, kc, :], yb[:, kc, sl], kc == 0, kc == 3, r=[w2, yb], w=[z])
                        k.act(sgt[:], g[:], AF.Sigmoid, r=[g], w=[sgt])
                        if n == 0:
                            k.tt("dve", macc[:, sl], sgt[:], z[:], ALU.mult, r=[sgt, z], w=[macc])
                        else:
                            k.tt("dve", tm[:], sgt[:], z[:], ALU.mult, r=[sgt, z], w=[tm])
                            k.tt("pool", macc[:, sl], macc[:, sl], tm[:], ALU.add, r=[macc, tm], w=[macc])
                k.cp("act", mergedT[:, j, :], macc[:], r=[macc], w=[mergedT])
        with k.phase() as st:
            wo = [k.sb(f"mwo{l}{i}", [128, 16, 512], BF16, st) for i in range(2)]
            gtb = k.sb(f"mgtb{l}", [128, D], F32, st)
            xt = [k.sb(f"mxt{l}{i}", [128, 512], F32, st) for i in range(3)]
            tm2 = [k.sb(f"mt2{l}{i}", [128, 512], F32, st) for i in range(2)]
            po = [k.psum(f"mpo{l}{i}", [128, 512], F32, st) for i in range(2)]
            k.dma("sp", gtb[:], mods["gt_scr"][l, 0].rearrange("a b -> (a b)").partition_broadcast(128), r=[mods["gt_scr"]], w=[gtb])
            it = 0
            for nd in range(4):
                w = wo[nd % 2]
                k.dma("pool", w[:], w_out[l, :, nd * 512:(nd + 1) * 512].rearrange("(kc p) n -> p kc n", p=128), w=[w])
                for tt in range(16):
                    p = po[it % 2]
                    x = xt[it % 3]
                    t2 = tm2[it % 2]
                    it += 1
                    k.dma(k.dq(), x[:], y[tt * 128:(tt + 1) * 128, nd * 512:(nd + 1) * 512], r=[y], w=[x])
                    for kc in range(16):
                        k.mm(p[:], mergedT[:, kc, tt * 128:(tt + 1) * 128], w[:, kc, :], kc == 0, kc == 15, r=[mergedT, w], w=[p])
                    k.tt("dve", t2[:], p[:], gtb[:, nd * 512:(nd + 1) * 512], ALU.mult, r=[p, gtb], w=[t2])
                    k.tt("pool", x[:], x[:], t2[:], ALU.add, r=[x, t2], w=[x])
                    k.dma(k.dq(), y[tt * 128:(tt + 1) * 128, nd * 512:(nd + 1) * 512], x[:], r=[x], w=[y])


def stage_moe(k, c, hT, l, mods, y, nexp=NE):
    w_router = k.inp("w_router", [k.nl, D, NE])
    b_router = k.inp("b_router", [k.nl, NE])
    w_gu = k.inp("w_gu_r", [k.nl, NE, 6, 128, 2, 2048])
    b_gu = k.inp("b_gu_col", [k.nl, 128, NE * 12])
    w_down = k.inp("w_down", [k.nl, NE, DE, D])
    b_down = k.inp("b_down", [k.nl, NE, D])
    with k.phase() as st0:
        wts = k.sb(f"wts{l}", [128, 16, NE], F32, st0)
        wtsT = k.sb(f"wtsT{l}", [NE, 16, 128], BF16, st0)
        with k.phase() as st:
            wr = k.sb(f"wr{l}", [128, 16, NE], BF16, st)
            brb = k.sb(f"brb{l}", [128, NE], F32, st)
            lg = [k.sb(f"lg{l}{i}", [128, NE], F32, st) for i in range(2)]
            ex = [k.sb(f"ex{l}{i}", [128, NE], F32, st) for i in range(2)]
            mk = [k.sb(f"mk{l}{i}", [128, NE], F32, st) for i in range(2)]
            m8 = [k.sb(f"m8{l}{i}", [128, 8], F32, st) for i in range(2)]
            sm = [k.sb(f"sm{l}{i}", [128, 4], F32, st) for i in range(2)]
            pr = [k.psum(f"pr{l}{i}", [128, 512], F32, st) for i in range(2)]
            ptw = k.psum(f"ptw{l}", [128, 512], F32, st)
            k.dma("pool", wr[:], w_router[l].rearrange("(kc p) n -> p kc n", p=128), w=[wr])
            k.dma("sp", brb[:], b_router[l].partition_broadcast(128), w=[brb])
            for tt in range(16):
                i = tt % 2
                for kc in range(16):
                    k.mm(pr[i][:, 0:NE], hT[:, kc, tt * 128:(tt + 1) * 128], wr[:, kc, :], kc == 0, kc == 15, r=[hT, wr], w=[pr[i]])
                k.tt("dve", lg[i][:], pr[i][:, 0:NE], brb[:], ALU.add, r=[pr[i], brb], w=[lg[i]])
                k.P.op("dve", lambda e, a=m8[i], b=lg[i]: e.max(out=a[:], in_=b[:]), reads=[lg[i]], writes=[m8[i]])
                k.ts("dve", mk[i][:], lg[i][:], m8[i][:, 3:4], None, ALU.is_ge, r=[lg[i], m8[i]], w=[mk[i]])
                k.ts("dve", sm[i][:, 0:1], m8[i][:, 0:1], -1.0, None, ALU.mult, r=[m8[i]], w=[sm[i]])
                k.act(ex[i][:], lg[i][:], AF.Exp, r=[lg[i], sm[i]], w=[ex[i]], bias=sm[i][:, 0:1])
                k.stt(ex[i][:], ex[i][:], 1.0, mk[i][:], ALU.mult, ALU.mult, r=[ex[i], mk[i]], w=[ex[i], sm[i]], accum=sm[i][:, 1:2])
                k.recip(sm[i][:, 2:3], sm[i][:, 1:2], r=[sm[i]], w=[sm[i]])
                k.ts("dve", wts[:, tt, :], ex[i][:], sm[i][:, 2:3], None, ALU.mult, r=[ex[i], sm[i]], w=[wts])
                k.tr(ptw[0:NE, 0:128], wts[:, tt, :], c["idf"][:], r=[wts, c["idf"]], w=[ptw])
                k.cp("act", wtsT[:, tt, :], ptw[0:NE, 0:128], r=[ptw], w=[wtsT])
        if "wtsd" in k.dbg:
            wd_ = k.dram("wtsd", [128, 16, NE])
            k.dma("sp", wd_[:], wts[:], r=[wts], w=[wd_])
        if not hasattr(k, "moe_act"):
            k.moe_act = k.dram("moe_act", [NE, 6, 128, S], BF16)
        act_scr = k.moe_act
        with k.phase() as st:
            acc = k.sb(f"acc{l}", [128, 16, 1024], F32, st)
            actT = k.sb(f"actT{l}", [128, 6, S], BF16, st)
            wgu = [k.sb(f"wgu{l}{i}", [128, 16, 2, 128], BF16, st) for i in range(2)]
            wdh = [k.sb(f"wdh{l}{i}", [128, 6, 512], BF16, st) for i in range(2)]
            bgu = k.sb(f"bgu{l}", [128, NE * 12], F32, st)
            bdall = k.sb(f"bdall{l}", [NE, D], BF16, st)
            gt_ = [k.sb(f"eg{l}{i}", [128, 512], F32, st) for i in range(2)]
            st_ = [k.sb(f"es{l}{i}", [128, 512], F32, st) for i in range(1)]
            ut_ = [k.sb(f"eu{l}{i}", [128, 512], F32, st) for i in range(2)]
            xt = ut_
            gtc = gt_[0]
            pgs = [k.psum(f"epg{l}{i}", [128, 512], F32, st) for i in range(2)]
            pus = [k.psum(f"epu{l}{i}", [128, 512], F32, st) for i in range(2)]
            pys = [k.psum(f"epy{l}{i}", [128, 512], F32, st) for i in range(3)]
            k.dma("sp", bgu[:], b_gu[l], w=[bgu])
            k.dma("pool", bdall[:], b_down[l], w=[bdall])
            iw = 0
            ie = 0
            iy = 0
            ix = 0
            for half in range(2):
                for nd in range(2):
                    c0 = half * 1024 + nd * 512
                    for tt in range(16):
                        p = pys[iy % 3]
                        iy += 1
                        k.mm(p[:], wtsT[:, tt, :], bdall[:, c0:c0 + 512], True, True, r=[wtsT, bdall], w=[p])
                        k.cp("act", acc[:, tt, nd * 512:(nd + 1) * 512], p[:], r=[p], w=[acc])
                for e in range(nexp):
                    for nd in range(2):
                        c0 = half * 1024 + nd * 512
                        k.dma("pool", wdh[nd][:], w_down[l, e, :, c0:c0 + 512].rearrange("(kc p) n -> p kc n", p=128), w=[wdh[nd]])
                    if half == 0:
                        abuf = actT
                        aview = lambda kc, tt, ab=actT: ab[:, kc, tt * 128:(tt + 1) * 128]
                        for m in range(6):
                            w = wgu[iw % 2]
                            iw += 1
                            k.dma("pool", w[:].rearrange("p a b c -> p (a b c)").rearrange("p (h n) -> p h n", h=2), w_gu[l, e, m], w=[w])
                            for n in range(4):
                                tok = slice(n * 512, (n + 1) * 512)
                                g = pgs[ie % 2]
                                u = pus[ie % 2]
                                gs_ = gt_[ie % 2]
                                ss_ = st_[0]
                                us_ = ut_[ie % 2]
                                ie += 1
                                for kc in range(16):
                                    k.mm(g[:], w[:, kc, 0, :], hT[:, kc, tok], kc == 0, kc == 15, r=[w, hT], w=[g])
                                for kc in range(16):
                                    k.mm(u[:], w[:, kc, 1, :], hT[:, kc, tok], kc == 0, kc == 15, r=[w, hT], w=[u])
                                cg = e * 12 + m
                                cu = e * 12 + 6 + m
                                k.ts("dve", gs_[:], g[:], bgu[:, cg:cg + 1], 7.0, ALU.add, ALU.min, r=[g, bgu], w=[gs_])
                                k.act(ss_[:], gs_[:], AF.Sigmoid, r=[gs_], w=[ss_], scale=1.702)
                                k.ts("dve", us_[:], u[:], bgu[:, cu:cu + 1], 7.0, ALU.add, ALU.min, r=[u, bgu], w=[us_])
                                k.ts("pool", us_[:], us_[:], -7.0, 1.0, ALU.max, ALU.add, r=[us_], w=[us_])
                                k.tt("pool", gs_[:], gs_[:], ss_[:], ALU.mult, r=[gs_, ss_], w=[gs_])
                                k.tt("dve", actT[:, m, tok], us_[:], gs_[:], ALU.mult, r=[us_, gs_], w=[actT])
                            k.dma(k.dq(), act_scr[e, m], actT[:, m, :], r=[actT], w=[act_scr])
                    else:
                        if e % 2 == 0:
                            abuf = actT
                            k.dma(k.dq(), actT[:], act_scr[e].rearrange("m p s -> p m s"), r=[act_scr], w=[actT])
                            aview = lambda kc, tt, ab=actT: ab[:, kc, tt * 128:(tt + 1) * 128]
                        else:
                            abuf = hT
                            k.dma(k.dq(), hT[:, 0:6, :], act_scr[e].rearrange("m p s -> p m s"), r=[act_scr], w=[hT])
                            aview = lambda kc, tt, ab=hT: ab[:, kc, tt * 128:(tt + 1) * 128]
                    for nd in range(2):
                        for tt in range(16):
                            p = pys[iy % 3]
                            iy += 1
                            for kc in range(6):
                                k.mm(p[:], aview(kc, tt), wdh[nd][:, kc, :], kc == 0, kc == 5, r=[abuf, wdh[nd]], w=[p])
                            wc = wts[:, tt, e:e + 1]
                            a_ = acc[:, tt, nd * 512:(nd + 1) * 512]
                            k.stt(a_, p[:], wc, a_, ALU.mult, ALU.add, r=[p, wts, acc], w=[acc])
                for nd in range(2):
                    c0 = half * 1024 + nd * 512
                    cs = slice(c0, c0 + 512)
                    k.dma("sp", gtc[:], mods["gt_scr"][l, 1].rearrange("a b -> (a b)")[c0:c0 + 512].partition_broadcast(128),
                          r=[mods["gt_scr"]], w=[gtc])
                    for tt in range(16):
                        x = xt[ix % 2]
                        ix += 1
                        rows = slice(tt * 128, (tt + 1) * 128)
                        a_ = acc[:, tt, nd * 512:(nd + 1) * 512]
                        k.dma(k.dq(), x[:], y[rows, cs], r=[y], w=[x])
                        k.tt("dve", a_, a_, gtc[:], ALU.mult, r=[acc, gtc], w=[acc])
                        k.tt("pool", x[:], x[:], a_, ALU.add, r=[x, acc], w=[x])
                        k.dma(k.dq(), y[rows, cs], x[:], r=[x], w=[y])


def stage_final(k, c, y):
    gf = k.inp("g_final", [D])
    with k.phase() as st:
        gb = k.sb("gfb", [128, D], F32, st)
        xt = [k.sb(f"fx{i}", [128, D], F32, st) for i in range(2)]
        sq = k.sb("fsq", [128, D], BF16, st)
        ss = [k.sb(f"fss{i}", [128, 4], F32, st) for i in range(2)]
        k.dma("sp", gb[:], gf[:].partition_broadcast(128), w=[gb])
        for tt in range(16):
            x = xt[tt % 2]
            s = ss[tt % 2]
            k.dma(k.dq(), x[:], y[tt * 128:(tt + 1) * 128, :], r=[y], w=[x])
            k.act(sq[:], x[:], AF.Square, r=[x], w=[sq, s], accum=s[:, 0:1])
            k.ts("dve", s[:, 1:2], s[:, 0:1], 1.0 / D, EPS, ALU.mult, ALU.add, r=[s], w=[s])
            k.act(s[:, 2:3], s[:, 1:2], AF.Sqrt, r=[s], w=[s])
            k.recip(s[:, 3:4], s[:, 2:3], r=[s], w=[s])
            k.stt(x[:], x[:], s[:, 3:4], gb[:], ALU.mult, ALU.mult, r=[x, s, gb], w=[x])
            k.dma(k.dq(), y[tt * 128:(tt + 1) * 128, :], x[:], r=[x], w=[y])


def build(nc, stack, cfg):
    k = KB(nc, stack, cfg)
    nl = cfg.get("nl", L)
    k.nl = nl
    stages = cfg.get("stages", "all")
    c = build_consts(k)
    x_in = k.inp("x", [S, D])
    y = T(nc.dram_tensor("y", [S, D], F32, kind="ExternalOutput").ap(), "y")
    k.outs["y"] = y
    w_in = k.inp("w_in", [nl, D, N_IN])
    k.dma("sp", y[0:1024, :], x_in[0:1024, :], w=[y])
    k.dma("act", y[1024:2048, :], x_in[1024:2048, :], w=[y])
    mods = stage_mod(k, c, nl) if (stages == "all" or "mod" in stages or "norm" in stages or "merge" in stages or "moe" in stages) else None
    hT = k.sb("hT", [128, 16, S], BF16)
    hg_tm = k.dram("hg_tm", [S, 2048])
    s5_fm = k.dram("s5_fm", [512, S])
    m2z_tm = k.dram("m2z_tm", [S, 512])
    m2x_fm = k.dram("m2x_fm", [1024, S])
    m2dt_tm = k.dram("m2dt_tm", [S, 8])
    rk_tm = k.dram("rk_tm", [S, 1792])
    if cfg.get("ys_input"):
        ys_fm = k.inp("ys_fm", [4, 512, S], BF16)
    else:
        ys_fm = k.dram("ys_fm", [4, 512, S], BF16)
    if not cfg.get("ys_input"):
        with k.phase() as st:
            zt_ = k.sb("yszero", [128, S], BF16, st)
            k.memset("dve", zt_[:], 0.0, w=[zt_])
            for n_ in range(4):
                for j_ in range(4):
                    k.dma(k.dq(), ys_fm[n_, j_ * 128:(j_ + 1) * 128, :], zt_[:], r=[zt_], w=[ys_fm])
    for l in range(nl):
        if mods is not None:
            stage_norm(k, c, y, hT, mods["gsa"], l, mods["modc"], 0, f"a{l}")
        if "hTd" in k.dbg:
            hdbg = k.dram("hTd", [128, 16, S], BF16) if l == 0 else k.outs["hTd"]
            k.dma("sp", hdbg[:], hT[:], r=[hT], w=[hdbg])
        if stages == "norm":
            continue
        if "proj" in stages or stages == "all":
            with k.phase() as st:
                proj_tm(k, hT, w_in[l], O_HG, 2048, hg_tm, st, f"hg{l}")
            with k.phase() as st:
                proj_fm(k, hT, w_in[l], O_S5, 512, s5_fm, st, f"s5{l}")
            with k.phase() as st:
                proj_tm(k, hT, w_in[l], O_M2, 512, m2z_tm, st, f"mz{l}")
            with k.phase() as st:
                proj_fm(k, hT, w_in[l], O_M2 + 512, 1024, m2x_fm, st, f"mx{l}")
            with k.phase() as st:
                proj_tm(k, hT, w_in[l], O_M2 + 1536, 8, m2dt_tm, st, f"md{l}")
            with k.phase() as st:
                proj_tm(k, hT, w_in[l], O_RK, 1792, rk_tm, st, f"rk{l}")
        if "mix" in stages or stages == "all":
            mixers(k, c, l, dict(hg_tm=hg_tm, s5_fm=s5_fm, m2z_tm=m2z_tm, m2x_fm=m2x_fm, m2dt_tm=m2dt_tm, rk_tm=rk_tm), ys_fm)
        if "merge" in stages or stages == "all":
            w_branch = k.inp("w_branch", [nl, 4, BR, D])
            w_out = k.inp("w_out", [nl, D, D])
            stage_merge(k, c, hT, l, w_in, w_branch, w_out, ys_fm, mods, y)
        if "moe" in stages or stages == "all":
            stage_norm(k, c, y, hT, mods["gsm"], l, mods["modc"], 48, f"m{l}")
            stage_moe(k, c, hT, l, mods, y, nexp=cfg.get("nexp", NE))
    if stages == "all" or "final" in stages:
        stage_final(k, c, y)
    k.P.finish()
    k.P.emit()
    return k


TWO_PI = 6.283185307179586
PI = 3.141592653589793


def mix_s5(k, c, l, s5_fm, ys_fm):
    nl = k.nl
    lr_i = k.inp("s5_lr_col", [nl, 128, 16])
    li_i = k.inp("s5_li_col", [nl, 128, 16])
    ldt_i = k.inp("s5_ldt_col", [nl, 128, 16])
    bTr_i = k.inp("s5_bT_re", [nl, 16, 128, 128])
    bTi_i = k.inp("s5_bT_im", [nl, 16, 128, 128])
    cTr_i = k.inp("s5_cT_re", [nl, 16, 128, 128])
    cTi_i = k.inp("s5_cT_im", [nl, 16, 128, 128])
    dsk_i = k.inp("s5_d_col", [nl, 128, 4])
    wgl_i = k.inp("s5_w_glu", [nl, 512, 512])
    bgl_i = k.inp("s5_bglu_col", [nl, 128, 4])
    with k.phase() as st0:
        sm = lambda n, sh=[128, 16]: k.sb(f"s5{n}{l}", sh, F32, st0)
        mag = sm("mag"); cs = sm("cs"); sn = sm("sn"); cre = sm("cre"); cim = sm("cim")
        pwc = k.sb(f"s5pwc{l}", [128, 11, 16], F32, st0)
        pws = k.sb(f"s5pws{l}", [128, 11, 16], F32, st0)
        bTr = k.sb(f"s5bTr{l}", [128, 16, 128], BF16, st0)
        bTi = k.sb(f"s5bTi{l}", [128, 16, 128], BF16, st0)
        cAr = k.sb(f"s5cAr{l}", [128, 16, 128], BF16, st0)
        cAi = k.sb(f"s5cAi{l}", [128, 16, 128], BF16, st0)
        k.dma("pool", bTr[:], bTr_i[l].rearrange("t p m -> p t m"), w=[bTr])
        k.dma("pool", bTi[:], bTi_i[l].rearrange("t p m -> p t m"), w=[bTi])
        with k.phase() as st:
            t_ = lambda n, sh=[128, 16]: k.sb(f"s5t{n}{l}", sh, F32, st)
            lr = t_("lr"); li = t_("li"); dt = t_("dt"); a1 = t_("a1"); a2 = t_("a2"); mk = t_("mk"); den = t_("den")
            nr = t_("nr"); abr = t_("abr"); abi = t_("abi"); t1 = t_("t1"); t2 = t_("t2")
            k.dma("sp", lr[:], lr_i[l], w=[lr])
            k.dma("sp", li[:], li_i[l], w=[li])
            k.dma("sp", dt[:], ldt_i[l], w=[dt])
            k.act(dt[:], dt[:], AF.Exp, r=[dt], w=[dt])
            k.tt("dve", t1[:], lr[:], dt[:], ALU.mult, r=[lr, dt], w=[t1])
            k.act(mag[:], t1[:], AF.Exp, r=[t1], w=[mag])
            k.tt("dve", a1[:], li[:], dt[:], ALU.mult, r=[li, dt], w=[a1])
            k.ts("dve", a2[:], a1[:], PI / 2, None, ALU.add, r=[a1], w=[a2])
            for a in (a1, a2):
                for _ in range(6):
                    k.ts("dve", mk[:], a[:], PI, TWO_PI, ALU.is_gt, ALU.mult, r=[a], w=[mk])
                    k.tt("dve", a[:], a[:], mk[:], ALU.subtract, r=[a, mk], w=[a])
            k.act(sn[:], a1[:], AF.Sin, r=[a1], w=[sn])
            k.act(cs[:], a2[:], AF.Sin, r=[a2], w=[cs])
            k.tt("dve", abr[:], mag[:], cs[:], ALU.mult, r=[mag, cs], w=[abr])
            k.tt("dve", abi[:], mag[:], sn[:], ALU.mult, r=[mag, sn], w=[abi])
            k.tt("dve", den[:], lr[:], lr[:], ALU.mult, r=[lr], w=[den])
            k.tt("dve", t1[:], li[:], li[:], ALU.mult, r=[li], w=[t1])
            k.tt("dve", den[:], den[:], t1[:], ALU.add, r=[den, t1], w=[den])
            k.recip(den[:], den[:], r=[den], w=[den])
            k.ts("dve", nr[:], abr[:], -1.0, None, ALU.add, r=[abr], w=[nr])
            k.tt("dve", t1[:], nr[:], lr[:], ALU.mult, r=[nr, lr], w=[t1])
            k.tt("dve", t2[:], abi[:], li[:], ALU.mult, r=[abi, li], w=[t2])
            k.tt("dve", t1[:], t1[:], t2[:], ALU.add, r=[t1, t2], w=[t1])
            k.tt("dve", cre[:], t1[:], den[:], ALU.mult, r=[t1, den], w=[cre])
            k.tt("dve", t1[:], abi[:], lr[:], ALU.mult, r=[abi, lr], w=[t1])
            k.tt("dve", t2[:], nr[:], li[:], ALU.mult, r=[nr, li], w=[t2])
            k.tt("dve", t1[:], t1[:], t2[:], ALU.subtract, r=[t1, t2], w=[t1])
            k.tt("dve", cim[:], t1[:], den[:], ALU.mult, r=[t1, den], w=[cim])
            k.cp("dve", pwc[:, 0, :], cs[:], r=[cs], w=[pwc])
            k.cp("dve", pws[:, 0, :], sn[:], r=[sn], w=[pws])
            for jj in range(1, 11):
                k.tt("dve", t1[:], pwc[:, jj - 1, :], pwc[:, jj - 1, :], ALU.mult, r=[pwc], w=[t1])
                k.tt("dve", t2[:], pws[:, jj - 1, :], pws[:, jj - 1, :], ALU.mult, r=[pws], w=[t2])
                k.tt("dve", pwc[:, jj, :], t1[:], t2[:], ALU.subtract, r=[t1, t2], w=[pwc])
                k.tt("dve", t1[:], pwc[:, jj - 1, :], pws[:, jj - 1, :], ALU.mult, r=[pwc, pws], w=[t1])
                k.ts("dve", pws[:, jj, :], t1[:], 2.0, None, ALU.mult, r=[t1], w=[pws])
            cr = k.sb(f"s5cr{l}", [128, 16, 128], F32, st)
            ci = k.sb(f"s5ci{l}", [128, 16, 128], F32, st)
            tm = [k.sb(f"s5tm{l}{i}", [128, 128], F32, st) for i in range(2)]
            k.dma("sp", cr[:], cTr_i[l].rearrange("t p m -> p t m"), w=[cr])
            k.dma("act", ci[:], cTi_i[l].rearrange("t p m -> p t m"), w=[ci])
            ncim = t_("ncim")
            k.ts("dve", ncim[:], cim[:], -1.0, None, ALU.mult, r=[cim], w=[ncim])
            for t in range(16):
                a = tm[t % 2]
                k.ts("dve", a[:], cr[:, t, :], cre[:, t:t + 1], None, ALU.mult, r=[cr, cre], w=[a])
                k.stt(cAr[:, t, :], ci[:, t, :], ncim[:, t:t + 1], a[:], ALU.mult, ALU.add, r=[ci, ncim, a], w=[cAr])
                k.ts("dve", a[:], cr[:, t, :], ncim[:, t:t + 1], None, ALU.mult, r=[cr, ncim], w=[a])
                k.ts("dve", ci[:, t, :], ci[:, t, :], cre[:, t:t + 1], -1.0, ALU.mult, ALU.mult, r=[ci, cre], w=[ci])
                k.tt("dve", cAi[:, t, :], ci[:, t, :], a[:], ALU.add, r=[ci, a], w=[cAi])
        with k.phase() as st:
            tabc = k.sb(f"s5tabc{l}", [128, S], F32, st)
            tabs = k.sb(f"s5tabs{l}", [128, S], F32, st)
            bur = k.sb(f"s5bur{l}", [128, S], F32, st)
            bui = k.sb(f"s5bui{l}", [128, S], F32, st)
            tA = k.sb(f"s5tA{l}", [128, S], F32, st)
            tB = k.sb(f"s5tB{l}", [128, S], F32, st)
            tC = k.sb(f"s5tC{l}", [128, S], F32, st)
            Rt = k.sb(f"s5R{l}", [128, S], F32, st)
            xr = k.sb(f"s5xr{l}", [128, S], BF16, st)
            xi = k.sb(f"s5xi{l}", [128, S], BF16, st)
            ub = k.sb(f"s5ub{l}", [128, S], BF16, st)
            uf = k.sb(f"s5uf{l}", [128, S], F32, st)
            ygb = k.sb(f"s5ygb{l}", [128, 4, S], BF16, st)
            dsk = k.sb(f"s5dsk{l}", [128, 4], F32, st)
            bgl = k.sb(f"s5bgl{l}", [128, 4], F32, st)
            wgl = k.sb(f"s5wgl{l}", [128, 4, 512], BF16, st)
            pb = [k.psum(f"s5pb{l}{i}", [128, 512], F32, st) for i in range(4)]
            py = [k.psum(f"s5py{l}{i}", [128, 512], F32, st) for i in range(4)]
            k.dma("sp", dsk[:], dsk_i[l], w=[dsk])
            k.dma("sp", bgl[:], bgl_i[l], w=[bgl])
            k.dma("pool", wgl[:], wgl_i[l].rearrange("(kc p) n -> p kc n", p=128), w=[wgl])
            for ct in range(4):
                k.dma("pool", ub[:], s5_fm[ct * 128:(ct + 1) * 128, :], r=[s5_fm], w=[ub])
                k.dma("sp", uf[:], s5_fm[ct * 128:(ct + 1) * 128, :], r=[s5_fm], w=[uf])
                for s4 in range(4):
                    stt_ = ct * 4 + s4
                    k.memset("pool", tabc[:, 0:1], 1.0, w=[tabc])
                    k.memset("pool", tabs[:, 0:1], 0.0, w=[tabs])
                    for jj in range(11):
                        n = 1 << jj
                        pc = pwc[:, jj, stt_:stt_ + 1]
                        ps_ = pws[:, jj, stt_:stt_ + 1]
                        k.ts("dve", tA[:, 0:n], tabs[:, 0:n], ps_, None, ALU.mult, r=[tabs, pws], w=[tA])
                        k.ts("dve", tB[:, 0:n], tabs[:, 0:n], pc, None, ALU.mult, r=[tabs, pwc], w=[tB])
                        k.stt(tC[:, 0:n], tabc[:, 0:n], pc, tA[:, 0:n], ALU.mult, ALU.subtract, r=[tabc, pwc, tA], w=[tC])
                        k.stt(tabs[:, n:2 * n], tabc[:, 0:n], ps_, tB[:, 0:n], ALU.mult, ALU.add, r=[tabc, pws, tB], w=[tabs])
                        k.cp("pool", tabc[:, n:2 * n], tC[:, 0:n], r=[tC], w=[tabc])
                    k.ts("dve", Rt[:], tabc[:], 0.0, mag[:, stt_:stt_ + 1], ALU.mult, ALU.add, r=[tabc, mag], w=[Rt])
                    for n4 in range(4):
                        sl = slice(n4 * 512, (n4 + 1) * 512)
                        p1 = pb[(n4 % 2) * 2]
                        p2 = pb[(n4 % 2) * 2 + 1]
                        k.mm(p1[:], bTr[:, stt_, :], ub[:, sl], True, True, r=[bTr, ub], w=[p1])
                        k.mm(p2[:], bTi[:, stt_, :], ub[:, sl], True, True, r=[bTi, ub], w=[p2])
                        k.cp("act", bur[:, sl], p1[:], r=[p1], w=[bur])
                        k.cp("act", bui[:, sl], p2[:], r=[p2], w=[bui])
                    k.tt("dve", tA[:], tabc[:], bur[:], ALU.mult, r=[tabc, bur], w=[tA])
                    k.tt("pool", tB[:], tabs[:], bui[:], ALU.mult, r=[tabs, bui], w=[tB])
                    k.tt("dve", tA[:], tA[:], tB[:], ALU.add, r=[tA, tB], w=[tA])
                    k.tt("pool", tB[:], tabc[:], bui[:], ALU.mult, r=[tabc, bui], w=[tB])
                    k.tt("dve", tC[:], tabs[:], bur[:], ALU.mult, r=[tabs, bur], w=[tC])
                    k.tt("pool", tB[:], tB[:], tC[:], ALU.subtract, r=[tB, tC], w=[tB])
                    k.P.op("dve", lambda e: e.tensor_tensor_scan(out=bur[:], data0=Rt[:], data1=tA[:], initial=0.0, op0=ALU.mult, op1=ALU.add),
                           reads=[Rt, tA], writes=[bur])
                    k.P.op("dve", lambda e: e.tensor_tensor_scan(out=bui[:], data0=Rt[:], data1=tB[:], initial=0.0, op0=ALU.mult, op1=ALU.add),
                           reads=[Rt, tB], writes=[bui])
                    k.tt("dve", tA[:], tabc[:], bur[:], ALU.mult, r=[tabc, bur], w=[tA])
                    k.tt("pool", tC[:], tabs[:], bui[:], ALU.mult, r=[tabs, bui], w=[tC])
                    k.tt("dve", xr[:], tA[:], tC[:], ALU.subtract, r=[tA, tC], w=[xr])
                    k.tt("pool", tB[:], tabs[:], bur[:], ALU.mult, r=[tabs, bur], w=[tB])
                    k.tt("dve", tC[:], tabc[:], bui[:], ALU.mult, r=[tabc, bui], w=[tC])
                    k.tt("pool", xi[:], tB[:], tC[:], ALU.add, r=[tB, tC], w=[xi])
                    for n4 in range(4):
                        sl = slice(n4 * 512, (n4 + 1) * 512)
                        k.mm(py[n4][:], cAr[:, stt_, :], xr[:, sl], s4 == 0, False, r=[cAr, xr], w=[py[n4]])
                        k.mm(py[n4][:], cAi[:, stt_, :], xi[:, sl], False, s4 == 3, r=[cAi, xi], w=[py[n4]])
                for n4 in range(4):
                    sl = slice(n4 * 512, (n4 + 1) * 512)
                    k.stt(tA[:, sl], uf[:, sl], dsk[:, ct:ct + 1], py[n4][:], ALU.mult, ALU.add, r=[uf, dsk, py[n4]], w=[tA])
                k.tt("pool", tB[:], tA[:], tA[:], ALU.mult, r=[tA], w=[tB])
                k.ts("dve", tB[:], tB[:], 0.044715, 1.0, ALU.mult, ALU.add, r=[tB], w=[tB])
                k.tt("pool", tB[:], tB[:], tA[:], ALU.mult, r=[tB, tA], w=[tB])
                k.act(tB[:], tB[:], AF.Tanh, r=[tB], w=[tB], scale=0.7978845608028654)
                k.ts("dve", tB[:], tB[:], 1.0, 0.5, ALU.add, ALU.mult, r=[tB], w=[tB])
                k.tt("dve", ygb[:, ct, :], tB[:], tA[:], ALU.mult, r=[tB, tA], w=[ygb])
            for ct in range(4):
                for n4 in range(4):
                    sl = slice(n4 * 512, (n4 + 1) * 512)
                    p = pb[n4]
                    for kc in range(4):
                        k.mm(p[:], wgl[:, kc, ct * 128:(ct + 1) * 128], ygb[:, kc, sl], kc == 0, kc == 3, r=[wgl, ygb], w=[p])
                    k.act(tA[:, sl], p[:], AF.Sigmoid, r=[p, bgl], w=[tA], bias=bgl[:, ct:ct + 1])
                k.tt("dve", xr[:], tA[:], ygb[:, ct, :], ALU.mult, r=[tA, ygb], w=[xr])
                k.dma("sp", ys_fm[1, ct * 128:(ct + 1) * 128, :], xr[:], r=[xr], w=[ys_fm])


def mix_ssd(k, c, l, m2z_tm, m2x_fm, m2dt_tm, ys_fm):
    nl = k.nl
    cw_i = k.inp("m2_convw_col", [nl, 128, 8, 4])
    cb_i = k.inp("m2_convb_col", [nl, 128, 8])
    dtb_i = k.inp("m2_dt_bias", [nl, 8])
    alog_i = k.inp("m2_a_log", [nl, 8])
    dsk_i = k.inp("m2_d", [nl, 8])
    nrm_i = k.inp("m2_norm", [nl, 512])
    triu = c["triu"]
    with k.phase() as st0:
        xcb = k.sb(f"mxcb{l}", [128, 8, S], BF16, st0)
        ysc = k.sb(f"mysc{l}", [128, 4, S], BF16, st0)
        with k.phase() as st:
            cw = k.sb(f"mcw{l}", [128, 8, 4], F32, st)
            cb = k.sb(f"mcb{l}", [128, 8], F32, st)
            xin = [k.sb(f"mxin{l}{i}", [128, S], F32, st) for i in range(2)]
            ac = [k.sb(f"mac{l}{i}", [128, S], F32, st) for i in range(2)]
            k.dma("sp", cw[:], cw_i[l], w=[cw])
            k.dma("sp", cb[:], cb_i[l], w=[cb])
            for ct in range(8):
                x = xin[ct % 2]
                a = ac[ct % 2]
                k.dma(k.dq(), x[:], m2x_fm[ct * 128:(ct + 1) * 128, :], r=[m2x_fm], w=[x])
                k.ts("dve", a[:], x[:], cw[:, ct, 3:4], cb[:, ct:ct + 1], ALU.mult, ALU.add, r=[x, cw, cb], w=[a])
                for sh in (1, 2, 3):
                    k.stt(a[:, sh:S], x[:, 0:S - sh], cw[:, ct, 3 - sh:4 - sh], a[:, sh:S], ALU.mult, ALU.add, r=[x, cw, a], w=[a])
                k.act(xcb[:, ct, :], a[:], AF.Silu, r=[a], w=[xcb])
        with k.phase() as st:
            f = lambda n, sh, dt=F32: k.sb(f"m{n}{l}", sh, dt, st)
            dtb = f("dtb", [128, 8]); aneg = f("aneg", [128, 8]); dskb = f("dskb", [128, 8]); nrmb = f("nrmb", [128, 512])
            ST = f("ST", [128, 8, 64]); STb = f("STb", [128, 8, 64], BF16)
            xs_t = f("xst", [128, 8, 64]); B_t = f("Bt", [128, 2, 128], BF16)
            dtt = f("dtt", [128, 8]); adt = f("adt", [128, 8]); acs = f("acs", [128, 8]); nacs = f("nacs", [128, 8])
            lastb = f("lastb", [128, 8]); elast = f("elast", [128, 8]); eac = f("eac", [128, 8]); dcs = f("dcs", [128, 8])
            rhsh = [f(f"rhsh{i}", [128, 128]) for i in range(2)]
            Ld = [f(f"Ld{i}", [128, 128]) for i in range(2)]
            Mh = [f(f"Mh{i}", [128, 128], BF16) for i in range(2)]
            xdt = f("xdt", [128, 8, 64], BF16); xdw = f("xdw", [128, 8, 64], BF16)
            ysb = f("ysb", [128, 512]); zt = f("zt", [128, 512]); sq = f("sq", [128, 512]); ynb = f("ynb", [128, 512], BF16)
            ss = f("ss", [128, 4])
            pT = k.psum(f"mpT{l}", [128, 8, 128], BF16, st)
            pac = k.psum(f"mpac{l}", [128, 512], F32, st)
            prow = [k.psum(f"mprow{l}{i}", [128, 512], F32, st) for i in range(2)]
            pcb = k.psum(f"mpcb{l}", [128, 4, 128], F32, st)
            pyd = k.psum(f"mpyd{l}", [128, 512], F32, st)
            pyo = k.psum(f"mpyo{l}", [128, 512], F32, st)
            pst = k.psum(f"mpst{l}", [128, 512], F32, st)
            k.dma("sp", dtb[:], dtb_i[l].partition_broadcast(128), w=[dtb])
            k.dma("sp", aneg[:], alog_i[l].partition_broadcast(128), w=[aneg])
            k.dma("sp", dskb[:], dsk_i[l].partition_broadcast(128), w=[dskb])
            k.dma("sp", nrmb[:], nrm_i[l].partition_broadcast(128), w=[nrmb])
            k.act(aneg[:], aneg[:], AF.Exp, r=[aneg], w=[aneg])
            k.ts("dve", aneg[:], aneg[:], -1.0, None, ALU.mult, r=[aneg], w=[aneg])
            k.memset("dve", ST[:], 0.0, w=[ST])
            k.memset("dve", STb[:], 0.0, w=[STb])
            idb = c["idb"]
            for ch in range(k.cfg.get("ssd_ch0", 0), k.cfg.get("ssd_ch0", 0) + k.cfg.get("ssd_chunks", 16)):
                tok = slice(ch * 128, (ch + 1) * 128)
                for j in range(4):
                    k.tr(pT[:, j, :], xcb[:, j, tok], idb[:], r=[xcb, idb], w=[pT])
                for g in range(2):
                    k.tr(pT[:, 4 + g, :], xcb[:, 4 + g, tok], idb[:], r=[xcb, idb], w=[pT])
                k.cp("act", xs_t[:].rearrange("p h d -> p (h d)"), pT[:, 0:4, :].rearrange("p a b -> p (a b)"), r=[pT], w=[xs_t])
                k.cp("act", B_t[:].rearrange("p g n -> p (g n)"), pT[:, 4:6, :].rearrange("p a b -> p (a b)"), r=[pT], w=[B_t])
                lvl = k.cfg.get("ssd_lvl", 9)
                if lvl >= 2:
                    k.dma("sp", dtt[:], m2dt_tm[tok, :], r=[m2dt_tm], w=[dtt])
                    k.tt("dve", dtt[:], dtt[:], dtb[:], ALU.add, r=[dtt, dtb], w=[dtt])
                    k.act(dtt[:], dtt[:], AF.Exp, r=[dtt], w=[dtt])
                    k.act(dtt[:], dtt[:], AF.Ln, r=[dtt], w=[dtt], bias=1.0)
                    k.tt("dve", adt[:], dtt[:], aneg[:], ALU.mult, r=[dtt, aneg], w=[adt])
                    k.mm(pac[:, 0:8], triu[:], adt[:], True, True, r=[triu, adt], w=[pac])
                    k.mm(pac[:, 8:16], c["ones"][:], adt[:], True, True, r=[c["ones"], adt], w=[pac])
                    k.cp("dve", acs[:], pac[:, 0:8], r=[pac], w=[acs])
                    k.ts("dve", nacs[:], pac[:, 0:8], -1.0, None, ALU.mult, r=[pac], w=[nacs])
                    k.cp("dve", lastb[:], pac[:, 8:16], r=[pac], w=[lastb])
                    k.act(elast[:], lastb[:], AF.Exp, r=[lastb], w=[elast])
                    k.act(eac[:], acs[:], AF.Exp, r=[acs], w=[eac])
                    k.tt("dve", dcs[:], lastb[:], acs[:], ALU.subtract, r=[lastb, acs], w=[dcs])
                    k.act(dcs[:], dcs[:], AF.Exp, r=[dcs], w=[dcs])
                if lvl >= 3:
                    for h in range(8):
                        k.ts("dve", xdt[:, h, :], xs_t[:, h, :], dtt[:, h:h + 1], None, ALU.mult, r=[xs_t, dtt], w=[xdt])
                        k.ts("dve", xdw[:, h, :], xs_t[:, h, :], dtt[:, h:h + 1], dcs[:, h:h + 1], ALU.mult, ALU.mult, r=[xs_t, dtt, dcs], w=[xdw])
                    for g in range(2):
                        k.mm(pcb[:, g, :], xcb[:, 4 + g, tok], xcb[:, 6 + g, tok], True, True, r=[xcb], w=[pcb])
                if lvl >= 4:
                    for h in range(8):
                        i2 = h % 2
                        k.ts("dve", rhsh[i2][:], triu[:], adt[:, h:h + 1], None, ALU.mult, r=[triu, adt], w=[rhsh[i2]])
                        k.mm(prow[i2][:, 0:128], c["ones"][:], rhsh[i2][:], True, True, r=[c["ones"], rhsh[i2]], w=[prow[i2]])
                        k.ts("dve", Ld[i2][:], prow[i2][:, 0:128], nacs[:, h:h + 1], 0.0, ALU.add, ALU.min, r=[prow[i2], nacs], w=[Ld[i2]])
                        k.act(Ld[i2][:], Ld[i2][:], AF.Exp, r=[Ld[i2]], w=[Ld[i2]])
                        k.tt("pool", Ld[i2][:], Ld[i2][:], triu[:], ALU.mult, r=[Ld[i2], triu], w=[Ld[i2]])
                        k.tt("dve", Mh[i2][:], pcb[:, h // 4, :], Ld[i2][:], ALU.mult, r=[pcb, Ld[i2]], w=[Mh[i2]])
                        k.mm(pyd[:, h * 64:(h + 1) * 64], Mh[i2][:], xdt[:, h, :], True, True, r=[Mh[i2], xdt], w=[pyd])
                        k.mm(pyo[:, h * 64:(h + 1) * 64], xcb[:, 6 + h // 4, tok], STb[:, h, :], True, True, r=[xcb, STb], w=[pyo])
                        k.mm(pst[:, h * 64:(h + 1) * 64], B_t[:, h // 4, :], xdw[:, h, :], True, True, r=[B_t, xdw], w=[pst])
                if lvl >= 5:
                    if ch == 0:
                        k.dump("d_xcb", xcb, [128, 8, S], BF16)
                        k.dump("d_dtt", dtt, [128, 8]); k.dump("d_acs", acs, [128, 8]); k.dump("d_lastb", lastb, [128, 8])
                        k.dump("d_Ld", Ld[1], [128, 128]); k.dump("d_xdt", xdt, [128, 8, 64], BF16); k.dump("d_xst", xs_t, [128, 8, 64])
                    k.cp("act", ysb[:], pyd[:], r=[pyd], w=[ysb])
                    if ch == 0:
                        k.dump("d_yd", ysb, [128, 512])
                    for h in range(8):
                        hs = slice(h * 64, (h + 1) * 64)
                        k.stt(ysb[:, hs], pyo[:, hs], eac[:, h:h + 1], ysb[:, hs], ALU.mult, ALU.add, r=[pyo, eac, ysb], w=[ysb])
                        k.stt(ysb[:, hs], xs_t[:, h, :], dskb[:, h:h + 1], ysb[:, hs], ALU.mult, ALU.add, r=[xs_t, dskb, ysb], w=[ysb])
                        k.stt(ST[:, h, :], ST[:, h, :], elast[:, h:h + 1], pst[:, hs], ALU.mult, ALU.add, r=[ST, elast, pst], w=[ST])
                    k.cp("pool", STb[:], ST[:], r=[ST], w=[STb])
                if lvl >= 6:
                    k.dma("act", zt[:], m2z_tm[tok, :], r=[m2z_tm], w=[zt])
                    k.act(zt[:], zt[:], AF.Silu, r=[zt], w=[zt])
                    k.tt("dve", ysb[:], ysb[:], zt[:], ALU.mult, r=[ysb, zt], w=[ysb])
                    if ch == 0:
                        k.dump("d_yg", ysb, [128, 512])
                    k.tt("pool", sq[:], ysb[:], ysb[:], ALU.mult, r=[ysb], w=[sq])
                    k.red("dve", ss[:, 0:2], sq[:].rearrange("p (g d) -> p g d", g=2), ALU.add, r=[sq], w=[ss])
                    k.ts("dve", ss[:, 0:2], ss[:, 0:2], 1.0 / 256, EPS, ALU.mult, ALU.add, r=[ss], w=[ss])
                    k.act(ss[:, 0:2], ss[:, 0:2], AF.Sqrt, r=[ss], w=[ss])
                    k.recip(ss[:, 2:4], ss[:, 0:2], r=[ss], w=[ss])
                    for g in range(2):
                        gs_ = slice(g * 256, (g + 1) * 256)
                        k.stt(ynb[:, gs_], ysb[:, gs_], ss[:, 2 + g:3 + g], nrmb[:, gs_], ALU.mult, ALU.mult, r=[ysb, ss, nrmb], w=[ynb])
                    if ch == 0:
                        k.dump("d_ss", ss, [128, 4]); k.dump("d_ynb", ynb, [128, 512], BF16)
                    for j in range(4):
                        k.tr(pT[:, j, :], ynb[:, j * 128:(j + 1) * 128], idb[:], r=[ynb, idb], w=[pT])
                    k.cp("act", ysc[:, :, tok], pT[:, 0:4, :], r=[pT], w=[ysc])
                if k.cfg.get("ssd_barrier", True):
                    k.P.barrier()
        for j in range(4):
            k.dma(k.dq(), ys_fm[2, j * 128:(j + 1) * 128, :], ysc[:, j, :], r=[ysc], w=[ys_fm])


def mix_hgrn2(k, c, l, hg_tm, ys_fm):
    nl = k.nl
    lb_i = k.inp("hg_lower_bound", [L, 512])
    on_i = k.inp("hg_onorm", [nl, 128])
    triu = c["triu"]
    T_ = 64
    with k.phase() as st0:
        ysc = k.sb(f"hysc{l}", [128, 4, S], BF16, st0)
        lbb = k.sb(f"hlbb{l}", [T_, 512], F32, st0)
        omlb = k.sb(f"homlb{l}", [T_, 512], F32, st0)
        onb = k.sb(f"honb{l}", [T_, 512], F32, st0)
        mmid = k.sb(f"hmmid{l}", [T_, T_], F32, st0)
        mask4 = k.sb(f"hmask4{l}", [T_, 4, T_], F32, st0)
        with k.phase() as st:
            raw = k.sb(f"hraw{l}", [T_, L, 512], F32, st)
            mx = k.sb(f"hmx{l}", [T_, 512], F32, st)
            sm = k.sb(f"hsm{l}", [T_, 512], F32, st)
            for j in range(L):
                k.dma(k.dq(), raw[:, j, :], lb_i[j].partition_broadcast(T_), w=[raw])
            k.tt("dve", mx[:], raw[:, 0, :], raw[:, 1, :], ALU.max, r=[raw], w=[mx])
            for j in range(2, L):
                k.tt("dve", mx[:], mx[:], raw[:, j, :], ALU.max, r=[raw, mx], w=[mx])
            for j in range(L):
                k.tt("dve", raw[:, j, :], raw[:, j, :], mx[:], ALU.subtract, r=[raw, mx], w=[raw])
                k.act(raw[:, j, :], raw[:, j, :], AF.Exp, r=[raw], w=[raw])
            k.tt("dve", sm[:], raw[:, 0, :], raw[:, 1, :], ALU.add, r=[raw], w=[sm])
            for j in range(2, L):
                k.tt("dve", sm[:], sm[:], raw[:, j, :], ALU.add, r=[raw, sm], w=[sm])
            k.recip(sm[:], sm[:], r=[sm], w=[sm])
            k.memset("dve", lbb[:], 0.0, w=[lbb])
            for j in range(1, l + 1):
                k.tt("dve", lbb[:], lbb[:], raw[:, j, :], ALU.add, r=[raw, lbb], w=[lbb])
            k.tt("dve", lbb[:], lbb[:], sm[:], ALU.mult, r=[lbb, sm], w=[lbb])
            k.ts("dve", omlb[:], lbb[:], -1.0, 1.0, ALU.mult, ALU.add, r=[lbb], w=[omlb])
            for h in range(4):
                k.dma(k.dq(), onb[:, h * 128:(h + 1) * 128], on_i[l].partition_broadcast(T_), w=[onb])
            k.memset("pool", mmid[:], 1.0, w=[mmid])
            k.P.op("pool", lambda e: e.affine_select(out=mmid[:], in_=mmid[:], pattern=[[0, T_]], compare_op=ALU.is_ge,
                                                      fill=0.0, base=T_ // 2 - 1, channel_multiplier=-1), reads=[mmid], writes=[mmid])
            for h in range(4):
                k.cp("pool", mask4[:, h, :], triu[0:T_, 0:T_], r=[triu], w=[mask4])
        with k.phase() as st:
            f = lambda n, sh, dt=F32: k.sb(f"h{n}{l}", sh, dt, st)
            pin = [f(f"pin{i}", [T_, 2048]) for i in range(2)]
            sf = f("sf", [T_, 512]); fg = f("fg", [T_, 512]); lf = f("lf", [T_, 512]); kk = f("kk", [T_, 512]); qs = f("qs", [T_, 512])
            cums = f("cums", [T_, 512]); d1 = f("d1", [T_, 512]); d4 = f("d4", [T_, 512]); ex = [f(f"ex{i}", [T_, 512]) for i in range(2)]
            qkb = f("qkb", [T_, 3, 512], BF16)
            ksb = f("ksb", [T_, 512], BF16); vb = f("vb", [T_, 512], BF16)
            qkT = f("qkT", [128, 12, T_], BF16)
            scb = f("scb", [T_, 4, T_], BF16); sct = f("sct", [T_, 4 * T_])
            state = f("state", [128, 4, 128]); stb = f("stb", [128, 4, 128], BF16)
            eL = f("eL", [128, 4])
            osb = f("osb", [T_, 512]); sq = f("sq", [T_, 512]); ss = f("ss", [T_, 8]); sg = f("sg", [T_, 512]); yb = f("yb", [T_, 512], BF16)
            pc = k.psum(f"hpc{l}", [128, 512], F32, st); pm = k.psum(f"hpm{l}", [128, 512], F32, st); pl = k.psum(f"hpl{l}", [128, 512], F32, st)
            pT = k.psum(f"hpT{l}", [128, 16, T_], BF16, st)
            psc = k.psum(f"hpsc{l}", [128, 512], F32, st); po = k.psum(f"hpo{l}", [128, 512], F32, st)
            pst = k.psum(f"hpst{l}", [128, 512], F32, st); pL = k.psum(f"hpL{l}", [128, 512], F32, st)
            idb = c["idb"]
            ones = c["ones"]
            k.memset("dve", state[:], 0.0, w=[state])
            k.memset("dve", stb[:], 0.0, w=[stb])
            nch = k.cfg.get("hg_chunks", S // T_)
            for ch in range(nch):
                tok = slice(ch * T_, (ch + 1) * T_)
                p = pin[ch % 2]
                k.dma(k.dq(), p[:], hg_tm[tok, :], r=[hg_tm], w=[p])
                q_ = p[:, 0:512]; f_ = p[:, 512:1024]; i_ = p[:, 1024:1536]; g_ = p[:, 1536:2048]
                k.act(sf[:], f_, AF.Sigmoid, r=[p], w=[sf])
                k.tt("dve", fg[:], sf[:], omlb[:], ALU.mult, r=[sf, omlb], w=[fg])
                k.tt("dve", fg[:], fg[:], lbb[:], ALU.add, r=[fg, lbb], w=[fg])
                k.act(lf[:], fg[:], AF.Ln, r=[fg], w=[lf])
                k.ts("pool", kk[:], fg[:], -1.0, 1.0, ALU.mult, ALU.add, r=[fg], w=[kk])
                k.act(qs[:], q_, AF.Silu, r=[p], w=[qs])
                k.cp("pool", vb[:], i_, r=[p], w=[vb])
                k.mm(pc[0:T_, :], triu[0:T_, 0:T_], lf[:], True, True, r=[triu, lf], w=[pc])
                k.mm(pm[0:T_, :], mmid[:], lf[:], True, True, r=[mmid, lf], w=[pm])
                k.mm(pl[0:T_, :], ones[0:T_, 0:T_], lf[:], True, True, r=[ones, lf], w=[pl])
                for h in range(4):
                    k.mm(pL[:, h:h + 1], lf[:, h * 128:(h + 1) * 128], ones[0:T_, 0:1], True, True, r=[lf, ones], w=[pL])
                k.cp("act", cums[:], pc[0:T_, :], r=[pc], w=[cums])
                k.tt("dve", d1[:], cums[:], pm[0:T_, :], ALU.subtract, r=[cums, pm], w=[d1])
                k.ts("dve", d1[:], d1[:], 85.0, -85.0, ALU.min, ALU.max, r=[d1], w=[d1])
                k.tt("dve", d4[:], pl[0:T_, :], cums[:], ALU.subtract, r=[pl, cums], w=[d4])
                k.act(ex[0][:], d1[:], AF.Exp, r=[d1], w=[ex[0]])
                k.tt("dve", qkb[:, 0, :], qs[:], ex[0][:], ALU.mult, r=[qs, ex[0]], w=[qkb])
                k.act(ex[1][:], d1[:], AF.Exp, r=[d1], w=[ex[1]], scale=-1.0)
                k.tt("pool", qkb[:, 1, :], kk[:], ex[1][:], ALU.mult, r=[kk, ex[1]], w=[qkb])
                k.act(ex[0][:], cums[:], AF.Exp, r=[cums], w=[ex[0]])
                k.tt("dve", qkb[:, 2, :], qs[:], ex[0][:], ALU.mult, r=[qs, ex[0]], w=[qkb])
                k.act(ex[1][:], d4[:], AF.Exp, r=[d4], w=[ex[1]])
                k.tt("pool", ksb[:], kk[:], ex[1][:], ALU.mult, r=[kk, ex[1]], w=[ksb])
                k.act(eL[:], pL[:, 0:4], AF.Exp, r=[pL], w=[eL])
                for a in range(3):
                    for h in range(4):
                        k.tr(pT[:, a * 4 + h, :], qkb[:, a, h * 128:(h + 1) * 128], idb[0:T_, 0:T_], r=[qkb, idb], w=[pT])
                k.cp("act", qkT[:], pT[:, 0:12, :], r=[pT], w=[qkT])
                for h in range(4):
                    k.mm(psc[0:T_, h * T_:(h + 1) * T_], qkT[:, 4 + h, :], qkT[:, h, :], True, True, r=[qkT], w=[psc])
                k.ts("dve", sct[:], psc[0:T_, 0:4 * T_], -3.0e38, 3.0e38, ALU.max, ALU.min, r=[psc], w=[sct])
                k.tt("dve", scb[:].rearrange("p h t -> p (h t)"), sct[:], mask4[:].rearrange("p h t -> p (h t)"), ALU.mult,
                     r=[sct, mask4], w=[scb])
                for h in range(4):
                    hs = slice(h * 128, (h + 1) * 128)
                    k.mm(po[0:T_, hs], scb[:, h, :], vb[:, hs], True, False, r=[scb, vb], w=[po])
                    k.mm(po[0:T_, hs], qkT[:, 8 + h, :], stb[:, h, :], False, True, r=[qkT, stb], w=[po])
                for h in range(4):
                    hs = slice(h * 128, (h + 1) * 128)
                    k.mm(pst[:, hs], ksb[:, hs], vb[:, hs], True, True, r=[ksb, vb], w=[pst])
                for h in range(4):
                    hs = slice(h * 128, (h + 1) * 128)
                    k.stt(state[:, h, :], state[:, h, :], eL[:, h:h + 1], pst[:, hs], ALU.mult, ALU.add, r=[state, eL, pst], w=[state])
                k.cp("pool", stb[:], state[:], r=[state], w=[stb])
                k.cp("act", osb[:], po[0:T_, :], r=[po], w=[osb])
                if ch == 1:
                    k.dump("h_lf", lf, [T_, 512]); k.dump("h_cums", cums, [T_, 512]); k.dump("h_d1", d1, [T_, 512]); k.dump("h_d4", d4, [T_, 512])
                    k.dump("h_qkb", qkb, [T_, 3, 512], BF16); k.dump("h_qkT", qkT, [128, 12, T_], BF16); k.dump("h_scb", scb, [T_, 4, T_], BF16)
                    k.dump("h_osb", osb, [T_, 512]); k.dump("h_eL", eL, [128, 4]); k.dump("h_state", state, [128, 4, 128]); k.dump("h_ksb", ksb, [T_, 512], BF16)
                k.tt("pool", sq[:], osb[:], osb[:], ALU.mult, r=[osb], w=[sq])
                k.red("dve", ss[:, 0:4], sq[:].rearrange("p (h d) -> p h d", h=4), ALU.add, r=[sq], w=[ss])
                k.ts("dve", ss[:, 0:4], ss[:, 0:4], 1.0 / 128, EPS, ALU.mult, ALU.add, r=[ss], w=[ss])
                k.act(ss[:, 0:4], ss[:, 0:4], AF.Sqrt, r=[ss], w=[ss])
                k.recip(ss[:, 4:8], ss[:, 0:4], r=[ss], w=[ss])
                k.act(sg[:], g_, AF.Silu, r=[p], w=[sg])
                k.tt("pool", sg[:], sg[:], onb[:], ALU.mult, r=[sg, onb], w=[sg])
                for h in range(4):
                    hs = slice(h * 128, (h + 1) * 128)
                    k.stt(yb[:, hs], osb[:, hs], ss[:, 4 + h:5 + h], sg[:, hs], ALU.mult, ALU.mult, r=[osb, ss, sg], w=[yb])
                if ch == 1:
                    k.dump("h_yb", yb, [T_, 512], BF16); k.dump("h_ss", ss, [T_, 8]); k.dump("h_sg", sg, [T_, 512])
                for j in range(4):
                    k.tr(pT[:, j, :], yb[:, j * 128:(j + 1) * 128], idb[0:T_, 0:T_], r=[yb, idb], w=[pT])
                k.cp("act", ysc[:, :, tok], pT[:, 0:4, :], r=[pT], w=[ysc])
        for j in range(4):
            k.dma(k.dq(), ys_fm[0, j * 128:(j + 1) * 128, :], ysc[:, j, :], r=[ysc], w=[ys_fm])


def mix_rwkv7(k, c, l, rk_tm, ys_fm):
    nl = k.nl
    mu_i = k.inp("rk_mu", [nl, 1792])
    w0_i = k.inp("rk_w0", [nl, 512]); a0_i = k.inp("rk_a0", [nl, 512])
    w2_i = k.inp("rk_w2pad", [nl, 128, 512]); a2_i = k.inp("rk_a2pad", [nl, 128, 512]); g2_i = k.inp("rk_g2", [nl, 128, 512])
    kk_i = k.inp("rk_k_k", [nl, 512]); ka_i = k.inp("rk_k_a", [nl, 512]); rk_i = k.inp("rk_r_k_flat", [nl, 512])
    lnw_i = k.inp("rk_ln_w", [nl, 512]); lnb_i = k.inp("rk_ln_b", [nl, 512])
    qW = k.dram(f"rkq_w", [8, S, 64], F32) if l == 0 else k.rkq["w"]
    if l == 0:
        k.rkq = dict(w=qW, nkk=k.dram("rkq_nkk", [8, S, 64], BF16), ka=k.dram("rkq_ka", [8, S, 64], BF16),
                     k2=k.dram("rkq_k2", [8, S, 64], BF16), r=k.dram("rkq_r", [8, S, 64], BF16),
                     g=k.dram("rkq_g", [S, 512], F32), bonus=k.dram("rkq_bonus", [S, 512], F32))
    Q = k.rkq
    idf = c["idf"]; idb = c["idb"]
    nsteps = k.cfg.get("rk_steps", S)
    with k.phase() as st0:
        vT = k.sb(f"rvT{l}", [128, 4, S], F32, st0)
        with k.phase() as st:
            f = lambda n, sh, dt=F32: k.sb(f"r{n}{l}", sh, dt, st)
            mub = f("mub", [128, 1792]); w0b = f("w0b", [128, 512]); a0b = f("a0b", [128, 512])
            kkb = f("kkb", [128, 512]); kab = f("kab", [128, 512]); rkb = f("rkb", [128, 512])
            w2b = f("w2b", [128, 512], BF16); a2b = f("a2b", [128, 512], BF16); g2b = f("g2b", [128, 512], BF16)
            pc = [f(f"pc{i}", [128, 1792]) for i in range(2)]
            pv = [f(f"pv{i}", [128, 1792]) for i in range(2)]
            lob = f("lob", [128, 256], BF16); loT = f("loT", [128, 2, 128], BF16)
            xw = f("xw", [128, 512]); dec = f("dec", [128, 512]); av = f("av", [128, 512]); gsb = f("gsb", [128, 512])
            kkt = f("kkt", [128, 512]); sq = f("sq", [128, 512]); k2 = f("k2", [128, 512]); bon = f("bon", [128, 512])
            ss = f("ss", [128, 24])
            o_nkk = f("onkk", [128, 512], BF16); o_ka = f("oka", [128, 512], BF16); o_k2 = f("ok2", [128, 512], BF16); o_r = f("or", [128, 512], BF16)
            pT = k.psum(f"rpT{l}", [128, 8, 128], BF16, st)
            pw = k.psum(f"rpw{l}", [128, 512], F32, st); pa = k.psum(f"rpa{l}", [128, 512], F32, st); pg = k.psum(f"rpg{l}", [128, 512], F32, st)
            pvt = k.psum(f"rpvt{l}", [128, 512], F32, st)
            for (t_, src_, n_) in ((mub, mu_i, 1792), (w0b, w0_i, 512), (a0b, a0_i, 512), (kkb, kk_i, 512), (kab, ka_i, 512), (rkb, rk_i, 512)):
                k.dma(k.dq(), t_[:], src_[l].partition_broadcast(128), w=[t_])
            k.dma("pool", w2b[:], w2_i[l], w=[w2b])
            k.dma("pool", a2b[:], a2_i[l], w=[a2b])
            k.dma("pool", g2b[:], g2_i[l], w=[g2b])
            for tt in range(16):
                tok = slice(tt * 128, (tt + 1) * 128)
                p = pc[tt % 2]; pr = pv[tt % 2]
                k.dma("sp", p[:], rk_tm[tok, :], r=[rk_tm], w=[p])
                if tt == 0:
                    k.memset("pool", pr[0:1, :], 0.0, w=[pr])
                    k.dma("act", pr[1:128, :], rk_tm[0:127, :], r=[rk_tm], w=[pr])
                else:
                    k.dma("act", pr[:], rk_tm[tt * 128 - 1:tt * 128 + 127, :], r=[rk_tm], w=[pr])
                k.tt("dve", pr[:], pr[:], p[:], ALU.subtract, r=[pr, p], w=[pr])
                k.tt("pool", pr[:], pr[:], mub[:], ALU.mult, r=[pr, mub], w=[pr])
                k.tt("dve", p[:], p[:], pr[:], ALU.add, r=[p, pr], w=[p])
                r_ = p[:, 0:512]; k_ = p[:, 512:1024]; v_ = p[:, 1024:1536]
                k.act(lob[:, 0:64], p[:, 1536:1600], AF.Tanh, r=[p], w=[lob])
                k.cp("act", lob[:, 64:128], p[:, 1600:1664], r=[p], w=[lob])
                k.act(lob[:, 128:256], p[:, 1664:1792], AF.Sigmoid, r=[p], w=[lob])
                k.tr(pT[:, 0, :], lob[:, 0:128], idb[:], r=[lob, idb], w=[pT])
                k.tr(pT[:, 1, :], lob[:, 128:256], idb[:], r=[lob, idb], w=[pT])
                k.cp("act", loT[:], pT[:, 0:2, :], r=[pT], w=[loT])
                k.mm(pw[:], loT[:, 0, :], w2b[:], True, True, r=[loT, w2b], w=[pw])
                k.mm(pa[:], loT[:, 0, :], a2b[:], True, True, r=[loT, a2b], w=[pa])
                k.mm(pg[:], loT[:, 1, :], g2b[:], True, True, r=[loT, g2b], w=[pg])
                k.tt("dve", xw[:], pw[:], w0b[:], ALU.add, r=[pw, w0b], w=[xw])
                k.act(xw[:], xw[:], AF.Exp, r=[xw], w=[xw], scale=-1.0)
                k.act(xw[:], xw[:], AF.Ln, r=[xw], w=[xw], bias=1.0)
                k.act(xw[:], xw[:], AF.Exp, r=[xw], w=[xw], scale=-1.0, bias=-0.5)
                k.act(dec[:], xw[:], AF.Exp, r=[xw], w=[dec], scale=-1.0)
                k.tt("dve", av[:], pa[:], a0b[:], ALU.add, r=[pa, a0b], w=[av])
                k.act(av[:], av[:], AF.Sigmoid, r=[av], w=[av])
                k.cp("act", gsb[:], pg[:], r=[pg], w=[gsb])
                k.tt("pool", kkt[:], k_, kkb[:], ALU.mult, r=[p, kkb], w=[kkt])
                k.tt("pool", sq[:], kkt[:], kkt[:], ALU.mult, r=[kkt], w=[sq])
                k.red("dve", ss[:, 0:8], sq[:].rearrange("p (h d) -> p h d", h=8), ALU.add, r=[sq], w=[ss])
                k.act(ss[:, 0:8], ss[:, 0:8], AF.Sqrt, r=[ss], w=[ss])
                k.ts("dve", ss[:, 0:8], ss[:, 0:8], 1e-12, None, ALU.max, r=[ss], w=[ss])
                k.recip(ss[:, 8:16], ss[:, 0:8], r=[ss], w=[ss])
                for h in range(8):
                    hs = slice(h * 64, (h + 1) * 64)
                    k.ts("dve", kkt[:, hs], kkt[:, hs], ss[:, 8 + h:9 + h], None, ALU.mult, r=[kkt, ss], w=[kkt])
                k.stt(k2[:], av[:], -1.0, kab[:], ALU.add, ALU.mult, r=[av, kab], w=[k2])
                k.stt(k2[:], k2[:], 1.0, k_, ALU.add, ALU.mult, r=[k2, p], w=[k2])
                k.ts("dve", o_nkk[:], kkt[:], -1.0, None, ALU.mult, r=[kkt], w=[o_nkk])
                k.tt("pool", o_ka[:], kkt[:], av[:], ALU.mult, r=[kkt, av], w=[o_ka])
                k.cp("pool", o_k2[:], k2[:], r=[k2], w=[o_k2])
                k.cp("act", o_r[:], r_, r=[p], w=[o_r])
                k.tt("pool", sq[:], r_, k2[:], ALU.mult, r=[p, k2], w=[sq])
                k.tt("pool", sq[:], sq[:], rkb[:], ALU.mult, r=[sq, rkb], w=[sq])
                k.red("dve", ss[:, 16:24], sq[:].rearrange("p (h d) -> p h d", h=8), ALU.add, r=[sq], w=[ss])
                for h in range(8):
                    hs = slice(h * 64, (h + 1) * 64)
                    k.ts("dve", bon[:, hs], p[:, 1024 + h * 64:1024 + (h + 1) * 64], ss[:, 16 + h:17 + h], None, ALU.mult, r=[p, ss], w=[bon])
                for j in range(4):
                    k.tr(pvt[:, j * 128:(j + 1) * 128], p[:, 1024 + j * 128:1024 + (j + 1) * 128], idf[:], r=[p, idf], w=[pvt])
                k.cp("act", vT[:, :, tok], pvt[:].rearrange("p (j t) -> p j t", j=4), r=[pvt], w=[vT])
                hm = lambda q: q.t.rearrange("h t k -> t h k")[tok]
                k.dma("sp", hm(Q["w"]), dec[:].rearrange("p (h d) -> p h d", h=8), r=[dec], w=[Q["w"]])
                k.dma("act", hm(Q["nkk"]), o_nkk[:].rearrange("p (h d) -> p h d", h=8), r=[o_nkk], w=[Q["nkk"]])
                k.dma("sp", hm(Q["ka"]), o_ka[:].rearrange("p (h d) -> p h d", h=8), r=[o_ka], w=[Q["ka"]])
                k.dma("act", hm(Q["k2"]), o_k2[:].rearrange("p (h d) -> p h d", h=8), r=[o_k2], w=[Q["k2"]])
                k.dma("sp", hm(Q["r"]), o_r[:].rearrange("p (h d) -> p h d", h=8), r=[o_r], w=[Q["r"]])
                k.dma("act", Q["g"][tok, :], gsb[:], r=[gsb], w=[Q["g"]])
                k.dma("sp", Q["bonus"][tok, :], bon[:], r=[bon], w=[Q["bonus"]])
        with k.phase() as st1:
            yT = k.sb(f"ryT{l}", [128, 4, S], F32, st1)
            with k.phase() as st:
                TS = 8
                Wb = [k.sb(f"rWb{l}{i}", [128, 4, TS, 64], F32, st) for i in range(2)]
                Nb = [k.sb(f"rNb{l}{i}", [128, 4, TS, 64], BF16, st) for i in range(2)]
                Ab = [k.sb(f"rAb{l}{i}", [128, 4, TS, 64], BF16, st) for i in range(2)]
                Kb = [k.sb(f"rKb{l}{i}", [128, 4, TS, 64], BF16, st) for i in range(2)]
                Rb = [k.sb(f"rRb{l}{i}", [128, 4, TS, 64], BF16, st) for i in range(2)]
                KV = [k.sb(f"rKV{l}{i}", [128, 4, TS, 64], F32, st) for i in range(2)]
                St = k.sb(f"rSt{l}", [128, 4, 64], F32, st)
                tmpA = k.sb(f"rtmpA{l}", [128, 4, 64], F32, st)
                tmpB = k.sb(f"rtmpB{l}", [128, 4, 64], F32, st)
                tmpC = k.sb(f"rtmpC{l}", [128, 4, 64], F32, st)
                pend = None
                sa = k.sb(f"rsa{l}", [128, 4], F32, st)
                k.memset("dve", St[:], 0.0, w=[St])
                if nsteps < S:
                    k.memset("pool", yT[:], 0.0, w=[yT])
                for cch in range(nsteps // TS):
                    t0 = cch * TS
                    b = cch % 2
                    for (buf, qn) in ((Wb, "w"), (Nb, "nkk"), (Ab, "ka"), (Kb, "k2"), (Rb, "r")):
                        if k.cfg.get("rk_nodma") and cch >= 2:
                            continue
                        for hp in range(2):
                            srcap = Q[qn].t.rearrange("(j hp) t k -> hp j t k", hp=2)[hp, :, t0:t0 + TS, :]
                            k.dma(k.dq(), buf[b][hp * 64:(hp + 1) * 64], srcap.partition_broadcast(64), r=[Q[qn]], w=[buf[b]])
                    k.tt("pool", KV[b][:], Kb[b][:], vT[:, :, t0:t0 + TS].unsqueeze(3).to_broadcast([128, 4, TS, 64]), ALU.mult,
                         r=[Kb[b], vT], w=[KV[b]])
                    for ti in range(TS):
                        t = t0 + ti
                        N_ = Nb[b]; W_ = Wb[b]; A_ = Ab[b]; KV_ = KV[b]; R_ = Rb[b]
                        RX = dict(relaxed=True)
                        opa = lambda N_=N_, ti=ti: k.P.op("dve", lambda e: e.tensor_tensor(out=tmpA[:], in0=St[:], in1=N_[:, :, ti, :], op=ALU.mult),
                                                           reads=[St.b, N_.b], writes=[tmpA.b], **RX)
                        opb = lambda W_=W_, ti=ti: k.P.op("dve", lambda e: e.tensor_tensor(out=St[:], in0=St[:], in1=W_[:, :, ti, :], op=ALU.mult),
                                                           reads=[St.b, W_.b], writes=[St.b], **RX)
                        opc = lambda: k.P.op("dve", lambda e: e.tensor_reduce(out=sa[:], in_=tmpA[:], axis=AX.X, op=ALU.add),
                                             reads=[tmpA.b], writes=[sa.b], **RX)
                        opd = lambda KV_=KV_, ti=ti: k.P.op("dve", lambda e: e.tensor_tensor(out=St[:], in0=St[:], in1=KV_[:, :, ti, :], op=ALU.add),
                                                             reads=[St.b, KV_.b], writes=[St.b], **RX)
                        ope = lambda A_=A_, ti=ti: k.P.op("dve", lambda e: e.tensor_tensor(out=tmpB[:], in0=A_[:, :, ti, :],
                                                                                          in1=sa[:].unsqueeze(2).to_broadcast([128, 4, 64]), op=ALU.mult),
                                                           reads=[A_.b, sa.b], writes=[tmpB.b], **RX)
                        opf = lambda: k.P.op("dve", lambda e: e.tensor_tensor(out=St[:], in0=St[:], in1=tmpB[:], op=ALU.add),
                                             reads=[St.b, tmpB.b], writes=[St.b], **RX)
                        opg = lambda R_=R_, ti=ti: k.P.op("dve", lambda e: e.tensor_tensor(out=tmpC[:], in0=St[:], in1=R_[:, :, ti, :], op=ALU.mult),
                                                           reads=[St.b, R_.b], writes=[tmpC.b], **RX)
                        oph = lambda t=t: k.P.op("dve", lambda e: e.tensor_reduce(out=yT[:, :, t], in_=tmpC[:], axis=AX.X, op=ALU.add),
                                                 reads=[tmpC.b], writes=[yT.b], **RX)
                        opa()
                        if pend is not None:
                            pend[0]()
                        opb(); opc(); opd(); ope()
                        if pend is not None:
                            pend[1]()
                        opf()
                        pend = (opg, oph)
                if pend is not None:
                    pend[0]()
                    pend[1]()
            with k.phase() as st:
                f = lambda n, sh, dt=F32: k.sb(f"q{n}{l}", sh, dt, st)
                lnw = f("lnw", [128, 512]); lnb = f("lnb", [128, 512])
                ysb = f("ysb", [128, 512]); sq = f("sq", [128, 512]); ss = f("ss", [128, 24]); gt = f("gt", [128, 512]); bt = f("bt", [128, 512])
                ynb = f("ynb", [128, 512], BF16)
                ysc = f("ysc", [128, 4, S], BF16)
                py = k.psum(f"qpy{l}", [128, 512], F32, st)
                pT = k.psum(f"qpT{l}", [128, 8, 128], BF16, st)
                k.dma("sp", lnw[:], lnw_i[l].partition_broadcast(128), w=[lnw])
                k.dma("act", lnb[:], lnb_i[l].partition_broadcast(128), w=[lnb])
                for tt in range(16):
                    tok = slice(tt * 128, (tt + 1) * 128)
                    for j in range(4):
                        k.tr(py[:, j * 128:(j + 1) * 128], yT[:, j, tok], idf[:], r=[yT, idf], w=[py])
                    k.cp("act", ysb[:], py[:], r=[py], w=[ysb])
                    k.dma("sp", gt[:], Q["g"][tok, :], r=[Q["g"]], w=[gt])
                    k.dma("act", bt[:], Q["bonus"][tok, :], r=[Q["bonus"]], w=[bt])
                    k.red("dve", ss[:, 0:8], ysb[:].rearrange("p (h d) -> p h d", h=8), ALU.add, r=[ysb], w=[ss])
                    k.ts("dve", ss[:, 0:8], ss[:, 0:8], -1.0 / 64, None, ALU.mult, r=[ss], w=[ss])
                    for h in range(8):
                        hs = slice(h * 64, (h + 1) * 64)
                        k.ts("dve", ysb[:, hs], ysb[:, hs], ss[:, h:h + 1], None, ALU.add, r=[ysb, ss], w=[ysb])
                    k.tt("pool", sq[:], ysb[:], ysb[:], ALU.mult, r=[ysb], w=[sq])
                    k.red("dve", ss[:, 8:16], sq[:].rearrange("p (h d) -> p h d", h=8), ALU.add, r=[sq], w=[ss])
                    k.ts("dve", ss[:, 8:16], ss[:, 8:16], 1.0 / 64, 64e-5, ALU.mult, ALU.add, r=[ss], w=[ss])
                    k.act(ss[:, 8:16], ss[:, 8:16], AF.Sqrt, r=[ss], w=[ss])
                    k.recip(ss[:, 16:24], ss[:, 8:16], r=[ss], w=[ss])
                    for h in range(8):
                        hs = slice(h * 64, (h + 1) * 64)
                        k.stt(ysb[:, hs], ysb[:, hs], ss[:, 16 + h:17 + h], lnw[:, hs], ALU.mult, ALU.mult, r=[ysb, ss, lnw], w=[ysb])
                    k.tt("pool", bt[:], bt[:], lnb[:], ALU.add, r=[bt, lnb], w=[bt])
                    k.tt("dve", ysb[:], ysb[:], bt[:], ALU.add, r=[ysb, bt], w=[ysb])
                    k.tt("dve", ynb[:], ysb[:], gt[:], ALU.mult, r=[ysb, gt], w=[ynb])
                    for j in range(4):
                        k.tr(pT[:, j, :], ynb[:, j * 128:(j + 1) * 128], idb[:], r=[ynb, idb], w=[pT])
                    k.cp("act", ysc[:, :, tok], pT[:, 0:4, :], r=[pT], w=[ysc])
                for j in range(4):
                    k.dma(k.dq(), ys_fm[3, j * 128:(j + 1) * 128, :], ysc[:, j, :], r=[ysc], w=[ys_fm])


def mixers(k, c, l, src, ys_fm):
    which = k.cfg.get("mixers", "abcd")
    if "a" in which:
        mix_hgrn2(k, c, l, src["hg_tm"], ys_fm)
    if "b" in which:
        mix_s5(k, c, l, src["s5_fm"], ys_fm)
    if "d" in which:
        mix_rwkv7(k, c, l, src["rk_tm"], ys_fm)
    if "c" in which:
        mix_ssd(k, c, l, src["m2z_tm"], src["m2x_fm"], src["m2dt_tm"], ys_fm)


_WGU_CACHE = {}


def col_layout(v, ncol):
    sh = v.shape[:-1]
    return np.ascontiguousarray(v.reshape(*sh, ncol, 128).swapaxes(-1, -2))


def host_inputs(inp, b):
    m = {}
    m["x"] = np.ascontiguousarray(inp["x"][b])
    m["c_col"] = col_layout(inp["c"][b], 16)
    if "w_mod" in inp:
        m["w_mod"] = inp["w_mod"]
        m["b_mod_col"] = col_layout(inp["b_mod"], 96)
        m["g_mix_col"] = col_layout(inp["g_norm_mix"], 16)
        m["g_ffn_col"] = col_layout(inp["g_norm_ffn"], 16)
    if "w_in" in inp:
        m["w_in"] = inp["w_in"]
    for n in ("w_branch", "w_out", "w_router", "b_router", "w_down", "b_down", "g_final"):
        if n in inp:
            m[n] = inp[n]
    if "w_gu" in inp:
        wg = inp["w_gu"]
        key = id(wg)
        if _WGU_CACHE.get("key") != key:
            nl_ = wg.shape[0]
            r = wg.reshape(nl_, NE, 16, 128, 2, 6, 128).transpose(0, 1, 5, 3, 2, 4, 6)
            _WGU_CACHE["key"] = key
            _WGU_CACHE["val"] = np.ascontiguousarray(r).reshape(nl_, NE, 6, 128, 2, 2048)
        m["w_gu_r"] = _WGU_CACHE["val"]
    if "b_gu" in inp:
        bg = inp["b_gu"]
        m["b_gu_col"] = np.ascontiguousarray(bg.reshape(bg.shape[0], NE, 12, 128).transpose(0, 3, 1, 2).reshape(bg.shape[0], 128, NE * 12))
    if "ys_fm" in inp:
        m["ys_fm"] = inp["ys_fm"]
    if "rk_mu" in inp:
        nl_ = inp["rk_mu"].shape[0]
        for n in ("rk_mu", "rk_w0", "rk_a0", "rk_g2", "rk_k_k", "rk_k_a", "rk_ln_w", "rk_ln_b"):
            m[n] = inp[n]
        m["rk_r_k_flat"] = np.ascontiguousarray(inp["rk_r_k"].reshape(nl_, 512))
        w2p = np.zeros((nl_, 128, 512), np.float32); w2p[:, 0:64] = inp["rk_w2"]
        a2p = np.zeros((nl_, 128, 512), np.float32); a2p[:, 64:128] = inp["rk_a2"]
        m["rk_w2pad"] = w2p; m["rk_a2pad"] = a2p
    if "hg_lower_bound" in inp:
        m["hg_lower_bound"] = inp["hg_lower_bound"]
        m["hg_onorm"] = inp["hg_onorm"]
    if "m2_conv_w" in inp:
        cw = inp["m2_conv_w"]
        nl_ = cw.shape[0]
        m["m2_convw_col"] = np.ascontiguousarray(cw.reshape(nl_, 4, 8, 128).transpose(0, 3, 2, 1))
        m["m2_convb_col"] = col_layout(inp["m2_conv_b"], 8)
        for n in ("m2_dt_bias", "m2_a_log", "m2_d", "m2_norm"):
            m[n] = inp[n]
    if "s5_lambda_re" in inp:
        nl = inp["s5_lambda_re"].shape[0]
        def stcol(a):
            return np.ascontiguousarray(a.reshape(nl, 16, 2, 64).transpose(0, 2, 3, 1).reshape(nl, 128, 16))
        m["s5_lr_col"] = stcol(inp["s5_lambda_re"])
        m["s5_li_col"] = stcol(inp["s5_lambda_im"])
        m["s5_ldt_col"] = stcol(np.broadcast_to(inp["s5_log_dt"][:, :, None], (nl, 32, 64)))
        def bT(b):
            o = np.zeros((nl, 16, 128, 128), np.float32)
            for g in range(32):
                st_, g2 = g // 2, g % 2
                gl = g % 8
                o[:, st_, gl * 16:(gl + 1) * 16, g2 * 64:(g2 + 1) * 64] = b[:, g].transpose(0, 2, 1)
            return o
        def cT(cc):
            o = np.zeros((nl, 16, 128, 128), np.float32)
            for g in range(32):
                st_, g2 = g // 2, g % 2
                gl = g % 8
                o[:, st_, g2 * 64:(g2 + 1) * 64, gl * 16:(gl + 1) * 16] = cc[:, g].transpose(0, 2, 1)
            return o
        m["s5_bT_re"] = bT(inp["s5_b_re"]); m["s5_bT_im"] = bT(inp["s5_b_im"])
        m["s5_cT_re"] = cT(inp["s5_c_re"]); m["s5_cT_im"] = cT(inp["s5_c_im"])
        m["s5_d_col"] = col_layout(inp["s5_d"], 4)
        m["s5_w_glu"] = inp["s5_w_glu"]
        m["s5_bglu_col"] = col_layout(inp["s5_b_glu"], 4)
    return m


def run(inputs, cfg, cores):
    nc = bass.Bass("TRN2", target_bir_lowering=False)
    with contextlib.ExitStack() as stack:
        k = build(nc, stack, cfg)
    maps = []
    for ci in range(cores):
        full = host_inputs(inputs, ci % 4)
        nl = cfg.get("nl", L)
        maps.append({n: np.ascontiguousarray(full[n]) for n in k.inputs})
    res = run_bass_kernel_spmd(nc, maps, core_ids=list(range(cores)))
    return res, k


def kernel(**inputs):
    inputs = {n: np.asarray(v) for n, v in inputs.items()}
    res, k = run(inputs, {}, 4)
    out = np.stack([res.results[b]["y"] for b in range(4)], axis=0)
    return out.astype(np.float32)
```
